# Optimizing a Trainium2 kernel written in Bass

```python
import math
import jax
import jax.numpy as jnp
from jax import lax
import numpy as np

D_MODEL = 1024
BATCH = 16
SEQ = 2048
DEPTH = 2

GRID_W = 64
CTX_LEN = 256
N_EVEN = (DEPTH + 1) // 2
N_ODD = DEPTH // 2
HEAD_DIM = 64
A_WIDTH = D_MODEL // 2
B_Q_HEADS = (D_MODEL - A_WIDTH) // HEAD_DIM
B_KV_HEADS = max(1, B_Q_HEADS // 4)
HY_ORDER = 2
HY_BANDS = 16
HY_EMB = 1 + 2 * HY_BANDS
HY_HIDDEN = 64
HY_TARGET = 1e-2
HY_MAX_DECAY = math.log(1.0 / HY_TARGET) / 0.3
HY_MIN_DECAY = math.log(1.0 / HY_TARGET) / 1.5
SHORT_CONV = 3
MLA_HEADS = 8
MLA_NOPE = 64
MLA_ROPE = 32
MLA_V = 64
MLA_Q_RANK = D_MODEL // 4
MLA_KV_RANK = D_MODEL // 8
S5_WIDTH = D_MODEL - MLA_HEADS * MLA_V
S5_GROUP = 16
S5_GROUPS = S5_WIDTH // S5_GROUP
S5_STATE = 64
S5_DT_MIN = 1e-3
S5_DT_MAX = 1e-1
N_GROUPS = 4
EXPERTS_PER_GROUP = 8
N_EXPERTS = N_GROUPS * EXPERTS_PER_GROUP
TOP_K_INNER = 2
D_EXPERT = D_MODEL // 4
ROPE_THETA = 10000.0
Q_BLOCK = 128
EPS = 1e-6
EVEN_IN = 3 * A_WIDTH + (B_Q_HEADS + 2 * B_KV_HEADS) * HEAD_DIM
ODD_IN = MLA_Q_RANK + MLA_KV_RANK + MLA_ROPE + S5_WIDTH
F32 = jnp.float32

kernel_name = 'hybrid_diffusion_hyena_gqa_mla_s5_hmoe'


def _split(t, sizes):
    return jnp.split(t, [int(s) for s in np.cumsum(sizes)[:-1]], axis=-1)


def _heads(t, n):
    return t.reshape(t.shape[0], t.shape[1], n, -1)


def rmsnorm(t, g):
    tf = t.astype(F32)
    y = tf * lax.rsqrt(jnp.mean(tf * tf, axis=-1, keepdims=True) + EPS)
    return (y * g.astype(F32)).astype(t.dtype)


def modnorm(t, g, shift, scale):
    return rmsnorm(t, g) * (1.0 + scale) + shift


def grid_positions(n_tokens):
    rows_count = n_tokens // GRID_W
    rows = jnp.repeat(jnp.arange(rows_count, dtype=jnp.int32), GRID_W)
    cols = jnp.arange(rows_count * GRID_W, dtype=jnp.int32) % GRID_W
    return rows, cols


def rope_1d(t, pos):
    half = t.shape[-1] // 2
    freqs = ROPE_THETA ** (-jnp.arange(half, dtype=F32) / half)
    ang = pos.astype(F32)[:, None] * freqs
    cos = jnp.cos(ang)[:, None, :]
    sin = jnp.sin(ang)[:, None, :]
    t1 = t[..., :half].astype(F32)
    t2 = t[..., half:].astype(F32)
    return jnp.concatenate([t1 * cos - t2 * sin, t1 * sin + t2 * cos], axis=-1).astype(t.dtype)


def rope_2d(t, rows, cols):
    a = t.shape[-1] // 2
    return jnp.concatenate([rope_1d(t[..., :a], rows), rope_1d(t[..., a:], cols)], axis=-1)


def block_attention(q, k, v):
    bsz, lq, hq, dk = q.shape
    hkv = k.shape[2]
    rep = hq // hkv
    scale = dk ** -0.5
    qb = jnp.moveaxis(q.reshape(bsz, lq // Q_BLOCK, Q_BLOCK, hkv, rep, dk), 1, 0)

    def one_block(qblk):
        s = jnp.einsum('bqgrd,bkgd->bgrqk', qblk, k).astype(F32) * scale
        p = jax.nn.softmax(s, axis=-1).astype(v.dtype)
        return jnp.einsum('bgrqk,bkgd->bqgrd', p, v)

    o = lax.map(one_block, qb)
    return jnp.moveaxis(o, 0, 1).reshape(bsz, lq, hq * v.shape[-1])


def short_conv(u, w, b):
    up = jnp.pad(u, ((0, 0), (1, 1), (0, 0)))
    return up[:, :-2] * w[0] + up[:, 1:-1] * w[1] + up[:, 2:] * w[2] + b


def hyena_filter_rfft(n, w1, b1, w2, b2, w3, freq):
    t = jnp.arange(n, dtype=F32)
    t_norm = t / max(n - 1, 1)
    bands = jnp.linspace(1e-4, HY_BANDS - 1, HY_BANDS, dtype=F32)
    ang = (2.0 * math.pi / n) * t[:, None] * bands
    z = jnp.concatenate([t_norm[:, None], jnp.cos(ang), jnp.sin(ang)], axis=-1)
    fr = freq.astype(F32)
    hdn = jnp.sin(fr * (z @ w1.astype(F32) + b1.astype(F32)))
    hdn = jnp.sin(fr * (hdn @ w2.astype(F32) + b2.astype(F32)))
    taps = (hdn @ w3.astype(F32)).reshape(n, 2, HY_ORDER, A_WIDTH)
    deltas = jnp.linspace(HY_MIN_DECAY, HY_MAX_DECAY, A_WIDTH, dtype=F32)
    taps = taps * jnp.exp(-t_norm[:, None] * deltas)[:, None, None, :]
    fwd, bwd = taps[:, 0], taps[:, 1]
    circ = jnp.concatenate([fwd, jnp.zeros((1, HY_ORDER, A_WIDTH), F32), bwd[:0:-1]], axis=0)
    circ = circ * lax.rsqrt(jnp.sum(circ * circ, axis=0, keepdims=True) + EPS)
    return jnp.fft.rfft(circ, axis=0)


def long_conv(u, filt_f, bias):
    n = u.shape[1]
    uf = jnp.fft.rfft(u, n=2 * n, axis=1)
    y = jnp.fft.irfft(uf * filt_f[None], n=2 * n, axis=1)[:, :n]
    return y + u * bias


def hyena_mixer(u3, conv_w, conv_b, w1, b1, w2, b2, w3, freq, fbias):
    n = u3.shape[1]
    u3 = short_conv(u3, conv_w, conv_b).astype(F32)
    x1, x2, v = _split(u3, [A_WIDTH, A_WIDTH, A_WIDTH])
    filt = hyena_filter_rfft(n, w1, b1, w2, b2, w3, freq)
    fb = fbias.astype(F32)
    z = x1 * long_conv(v, filt[:, 0], fb[0])
    return x2 * long_conv(z, filt[:, 1], fb[1])


def s5_discretize(a_re, a_im, log_dt, b_re, b_im):
    a_re, a_im = a_re.astype(F32), a_im.astype(F32)
    dt = jnp.exp(log_dt.astype(F32))[:, None]
    mag = jnp.exp(a_re * dt)
    ab_re, ab_im = mag * jnp.cos(a_im * dt), mag * jnp.sin(a_im * dt)
    er, ei = ab_re - 1.0, ab_im
    den = a_re * a_re + a_im * a_im
    co_re = (er * a_re + ei * a_im) / den
    co_im = (ei * a_re - er * a_im) / den
    b_re, b_im = b_re.astype(F32), b_im.astype(F32)
    bb_re = co_re[..., None] * b_re - co_im[..., None] * b_im
    bb_im = co_re[..., None] * b_im + co_im[..., None] * b_re
    return ab_re, ab_im, bb_re, bb_im


def _lin_rec_op(e1, e2):
    a1r, a1i, b1r, b1i = e1
    a2r, a2i, b2r, b2i = e2
    return (a2r * a1r - a2i * a1i, a2r * a1i + a2i * a1r,
            a2r * b1r - a2i * b1i + b2r, a2r * b1i + a2i * b1r + b2i)


def s5_scan(u, ab_re, ab_im, bb_re, bb_im, h0, reverse):
    n = u.shape[1]
    bu_re = jnp.einsum('blgh,gph->lbgp', u, bb_re)
    bu_im = jnp.einsum('blgh,gph->lbgp', u, bb_im)
    a_re = jnp.broadcast_to(ab_re[None, None], (n, 1) + ab_re.shape)
    a_im = jnp.broadcast_to(ab_im[None, None], (n, 1) + ab_im.shape)
    pa_re, pa_im, s_re, s_im = lax.associative_scan(_lin_rec_op, (a_re, a_im, bu_re, bu_im), reverse=reverse, axis=0)
    if h0 is not None:
        h_re, h_im = h0
        s_re = s_re + pa_re * h_re - pa_im * h_im
        s_im = s_im + pa_re * h_im + pa_im * h_re
    return s_re, s_im


def s5_readout(s_re, s_im, c_re, c_im):
    return jnp.einsum('lbgp,ghp->blgh', s_re, c_re.astype(F32)) - jnp.einsum('lbgp,ghp->blgh', s_im, c_im.astype(F32))


def s5_mixer(u_lat, u_ctx, a_re, a_im, log_dt, b_re, b_im, c_re, c_im, d_skip, glu_w, glu_b, ctx_needed):
    def grouped(u):
        return u.astype(F32).reshape(u.shape[0], u.shape[1], S5_GROUPS, S5_GROUP)

    ul, uc = grouped(u_lat), grouped(u_ctx)
    dsk = d_skip.astype(F32).reshape(S5_GROUPS, S5_GROUP)
    y_lat = dsk * ul
    y_ctx = dsk * uc if ctx_needed else None
    for direction in range(2):
        reverse = direction == 1
        disc = s5_discretize(a_re[direction], a_im[direction], log_dt[direction], b_re[direction], b_im[direction])
        cr, ci = s5_scan(uc, *disc, None, reverse)
        end = 0 if reverse else -1
        lr, li = s5_scan(ul, *disc, (cr[end], ci[end]), reverse)
        y_lat = y_lat + s5_readout(lr, li, c_re[direction], c_im[direction])
        if ctx_needed:
            y_ctx = y_ctx + s5_readout(cr, ci, c_re[direction], c_im[direction])

    def glu(y):
        g = jax.nn.gelu(y.reshape(y.shape[0], y.shape[1], S5_WIDTH))
        return g * jax.nn.sigmoid(g @ glu_w.astype(F32) + glu_b.astype(F32))

    return glu(y_lat), (glu(y_ctx) if ctx_needed else None)


def even_mixer(p_lat, p_ctx, rows, cols, ctx_needed, conv_w, conv_b, w1, b1, w2, b2, w3, freq, fbias, qk_g):
    sizes = [3 * A_WIDTH, B_Q_HEADS * HEAD_DIM, B_KV_HEADS * HEAD_DIM, B_KV_HEADS * HEAD_DIM]
    hy_l, q_l, k_l, v_l = _split(p_lat, sizes)
    hy_c, q_c, k_c, v_c = _split(p_ctx, sizes)
    filt = (conv_w, conv_b, w1, b1, w2, b2, w3, freq, fbias)
    kc = rmsnorm(_heads(k_c, B_KV_HEADS), qk_g[1])
    vc = _heads(v_c, B_KV_HEADS)
    ql = rope_2d(rmsnorm(_heads(q_l, B_Q_HEADS), qk_g[0]), rows, cols)
    kl = rope_2d(rmsnorm(_heads(k_l, B_KV_HEADS), qk_g[1]), rows, cols)
    vl = _heads(v_l, B_KV_HEADS)
    att_l = block_attention(ql, jnp.concatenate([kc, kl], axis=1), jnp.concatenate([vc, vl], axis=1))
    out_l = jnp.concatenate([hyena_mixer(hy_l, *filt).astype(p_lat.dtype), att_l], axis=-1)
    if not ctx_needed:
        return out_l, None
    qc = rmsnorm(_heads(q_c, B_Q_HEADS), qk_g[0])
    att_c = block_attention(qc, kc, vc)
    out_c = jnp.concatenate([hyena_mixer(hy_c, *filt).astype(p_ctx.dtype), att_c], axis=-1)
    return out_l, out_c


def odd_mixer(p_lat, p_ctx, rows, cols, ctx_needed, q_norm_g, w_uq, kv_norm_g, w_ukv,
              a_re, a_im, log_dt, b_re, b_im, c_re, c_im, d_skip, glu_w, glu_b):
    sizes = [MLA_Q_RANK, MLA_KV_RANK, MLA_ROPE, S5_WIDTH]
    cq_l, ckv_l, kr_l, u_l = _split(p_lat, sizes)
    cq_c, ckv_c, kr_c, u_c = _split(p_ctx, sizes)

    def mla_kv(ckv, kr, use_rope):
        bsz, n = ckv.shape[:2]
        kvu = (rmsnorm(ckv, kv_norm_g) @ w_ukv).reshape(bsz, n, MLA_HEADS, MLA_NOPE + MLA_V)
        kr = kr[:, :, None, :]
        if use_rope:
            kr = rope_2d(kr, rows, cols)
        k = jnp.concatenate([kvu[..., :MLA_NOPE], jnp.broadcast_to(kr, (bsz, n, MLA_HEADS, MLA_ROPE))], axis=-1)
        return k, kvu[..., MLA_NOPE:]

    def mla_q(cq, use_rope):
        bsz, n = cq.shape[:2]
        q = (rmsnorm(cq, q_norm_g) @ w_uq).reshape(bsz, n, MLA_HEADS, MLA_NOPE + MLA_ROPE)
        if use_rope:
            q = jnp.concatenate([q[..., :MLA_NOPE], rope_2d(q[..., MLA_NOPE:], rows, cols)], axis=-1)
        return q

    kc, vc = mla_kv(ckv_c, kr_c, False)
    kl, vl = mla_kv(ckv_l, kr_l, True)
    att_l = block_attention(mla_q(cq_l, True), jnp.concatenate([kc, kl], axis=1), jnp.concatenate([vc, vl], axis=1))
    s5_l, s5_c = s5_mixer(u_l, u_c, a_re, a_im, log_dt, b_re, b_im, c_re, c_im, d_skip, glu_w, glu_b, ctx_needed)
    out_l = jnp.concatenate([att_l, s5_l.astype(p_lat.dtype)], axis=-1)
    if not ctx_needed:
        return out_l, None
    att_c = block_attention(mla_q(cq_c, False), kc, vc)
    return out_l, jnp.concatenate([att_c, s5_c.astype(p_ctx.dtype)], axis=-1)


def hier_moe(h, w_rg, w_re, w_gate, w_up, w_down):
    bsz, n, d = h.shape
    t = h.reshape(-1, d)
    pg = jax.nn.softmax((t @ w_rg).astype(F32), axis=-1)
    g_idx = jnp.argmax(pg, axis=-1)
    g_p = jnp.max(pg, axis=-1)
    le = (t @ w_re).astype(F32).reshape(-1, N_GROUPS, EXPERTS_PER_GROUP)
    le_sel = jnp.take_along_axis(le, g_idx[:, None, None], axis=1)[:, 0]
    top_p, top_i = lax.top_k(jax.nn.softmax(le_sel, axis=-1), TOP_K_INNER)
    top_p = top_p / jnp.sum(top_p, axis=-1, keepdims=True)
    inner = jnp.sum(jax.nn.one_hot(top_i, EXPERTS_PER_GROUP, dtype=F32) * top_p[..., None], axis=1)
    gates = (jax.nn.one_hot(g_idx, N_GROUPS, dtype=F32)[:, :, None]
             * (g_p[:, None] * inner)[:, None, :]).reshape(-1, N_EXPERTS).astype(t.dtype)
    y = jnp.zeros_like(t)
    for e in range(N_EXPERTS):
        hid = jax.nn.silu(t @ w_gate[e]) * (t @ w_up[e])
        y = y + gates[:, e:e + 1] * (hid @ w_down[e])
    return y.reshape(bsz, n, d)


def setup_inputs(seed: int = 0) -> dict:
    key = jax.random.key(seed)
    ks = iter(jax.random.split(key, 64))

    def nrm(shape, scale):
        return jax.random.normal(next(ks), shape, F32) * scale

    d = D_MODEL
    n_idx = jnp.arange(S5_STATE, dtype=F32)
    s5_shape = (N_ODD, 2, S5_GROUPS, S5_STATE)
    return {
        'x': nrm((BATCH, SEQ, d), 1.0),
        'c': nrm((BATCH, d), 1.0),
        'ctx': nrm((BATCH, CTX_LEN, d), 1.0),
        'c_ctx': nrm((d,), 1.0),
        'w_mod': nrm((DEPTH, d, 6 * d), 0.5 * d ** -0.5),
        'b_mod': nrm((DEPTH, 6 * d), 0.02),
        'norm_g': 1.0 + nrm((DEPTH, 2, d), 0.02),
        'e_w_in': nrm((N_EVEN, d, EVEN_IN), d ** -0.5),
        'e_hy_conv_w': nrm((N_EVEN, SHORT_CONV, 3 * A_WIDTH), SHORT_CONV ** -0.5),
        'e_hy_conv_b': nrm((N_EVEN, 3 * A_WIDTH), 0.02),
        'e_hy_w1': nrm((N_EVEN, HY_EMB, HY_HIDDEN), HY_EMB ** -0.5),
        'e_hy_b1': nrm((N_EVEN, HY_HIDDEN), 0.1),
        'e_hy_w2': nrm((N_EVEN, HY_HIDDEN, HY_HIDDEN), HY_HIDDEN ** -0.5),
        'e_hy_b2': nrm((N_EVEN, HY_HIDDEN), 0.1),
        'e_hy_w3': nrm((N_EVEN, HY_HIDDEN, 2 * HY_ORDER * A_WIDTH), HY_HIDDEN ** -0.5),
        'e_hy_freq': 1.0 + nrm((N_EVEN, HY_HIDDEN), 0.1),
        'e_hy_fbias': nrm((N_EVEN, HY_ORDER, A_WIDTH), 0.5),
        'e_qk_g': 1.0 + nrm((N_EVEN, 2, HEAD_DIM), 0.02),
        'e_w_out': nrm((N_EVEN, d, d), d ** -0.5),
        'o_w_in': nrm((N_ODD, d, ODD_IN), d ** -0.5),
        'o_q_norm_g': 1.0 + nrm((N_ODD, MLA_Q_RANK), 0.02),
        'o_w_uq': nrm((N_ODD, MLA_Q_RANK, MLA_HEADS * (MLA_NOPE + MLA_ROPE)), MLA_Q_RANK ** -0.5),
        'o_kv_norm_g': 1.0 + nrm((N_ODD, MLA_KV_RANK), 0.02),
        'o_w_ukv': nrm((N_ODD, MLA_KV_RANK, MLA_HEADS * (MLA_NOPE + MLA_V)), MLA_KV_RANK ** -0.5),
        'o_s5_a_re': -0.5 + nrm(s5_shape, 0.01),
        'o_s5_a_im': math.pi * n_idx + nrm(s5_shape, 0.01),
        'o_s5_log_dt': jax.random.uniform(next(ks), (N_ODD, 2, S5_GROUPS), F32, math.log(S5_DT_MIN), math.log(S5_DT_MAX)),
        'o_s5_b_re': nrm((N_ODD, 2, S5_GROUPS, S5_STATE, S5_GROUP), (2 * S5_GROUP) ** -0.5),
        'o_s5_b_im': nrm((N_ODD, 2, S5_GROUPS, S5_STATE, S5_GROUP), (2 * S5_GROUP) ** -0.5),
        'o_s5_c_re': nrm((N_ODD, 2, S5_GROUPS, S5_GROUP, S5_STATE), 0.5),
        'o_s5_c_im': nrm((N_ODD, 2, S5_GROUPS, S5_GROUP, S5_STATE), 0.5),
        'o_s5_d': nrm((N_ODD, S5_WIDTH), 0.5),
        'o_glu_w': nrm((N_ODD, S5_WIDTH, S5_WIDTH), S5_WIDTH ** -0.5),
        'o_glu_b': nrm((N_ODD, S5_WIDTH), 0.02),
        'o_w_out': nrm((N_ODD, d, d), d ** -0.5),
        'moe_w_rg': nrm((DEPTH, d, N_GROUPS), d ** -0.5),
        'moe_w_re': nrm((DEPTH, d, N_EXPERTS), d ** -0.5),
        'moe_w_gate': nrm((DEPTH, N_EXPERTS, d, D_EXPERT), d ** -0.5),
        'moe_w_up': nrm((DEPTH, N_EXPERTS, d, D_EXPERT), d ** -0.5),
        'moe_w_down': nrm((DEPTH, N_EXPERTS, D_EXPERT, d), D_EXPERT ** -0.5),
        'final_g': 1.0 + nrm((d,), 0.02),
    }


def reference(x, c, ctx, c_ctx, w_mod, b_mod, norm_g,
              e_w_in, e_hy_conv_w, e_hy_conv_b, e_hy_w1, e_hy_b1, e_hy_w2, e_hy_b2, e_hy_w3, e_hy_freq,
              e_hy_fbias, e_qk_g, e_w_out,
              o_w_in, o_q_norm_g, o_w_uq, o_kv_norm_g, o_w_ukv, o_s5_a_re, o_s5_a_im, o_s5_log_dt,
              o_s5_b_re, o_s5_b_im, o_s5_c_re, o_s5_c_im, o_s5_d, o_glu_w, o_glu_b, o_w_out,
              moe_w_rg, moe_w_re, moe_w_gate, moe_w_up, moe_w_down, final_g):
    n_ctx = ctx.shape[1]
    rows, cols = grid_positions(x.shape[1])
    xc = ctx
    for i in range(DEPTH):
        j = i // 2
        ctx_needed = i < DEPTH - 1
        sh1, sc1, g1, sh2, sc2, g2 = [m[:, None, :] for m in jnp.split(jax.nn.silu(c) @ w_mod[i] + b_mod[i], 6, axis=-1)]
        csh1, csc1, cg1, csh2, csc2, cg2 = jnp.split(jax.nn.silu(c_ctx) @ w_mod[i] + b_mod[i], 6, axis=-1)
        h_all = jnp.concatenate([modnorm(xc, norm_g[i, 0], csh1, csc1), modnorm(x, norm_g[i, 0], sh1, sc1)], axis=1)
        if i % 2 == 0:
            proj = h_all @ e_w_in[j]
            mix_l, mix_c = even_mixer(proj[:, n_ctx:], proj[:, :n_ctx], rows, cols, ctx_needed,
                                      e_hy_conv_w[j], e_hy_conv_b[j], e_hy_w1[j], e_hy_b1[j], e_hy_w2[j], e_hy_b2[j],
                                      e_hy_w3[j], e_hy_freq[j], e_hy_fbias[j], e_qk_g[j])
            w_out = e_w_out[j]
        else:
            proj = h_all @ o_w_in[j]
            mix_l, mix_c = odd_mixer(proj[:, n_ctx:], proj[:, :n_ctx], rows, cols, ctx_needed,
                                     o_q_norm_g[j], o_w_uq[j], o_kv_norm_g[j], o_w_ukv[j],
                                     o_s5_a_re[j], o_s5_a_im[j], o_s5_log_dt[j], o_s5_b_re[j], o_s5_b_im[j],
                                     o_s5_c_re[j], o_s5_c_im[j], o_s5_d[j], o_glu_w[j], o_glu_b[j])
            w_out = o_w_out[j]
        moe_p = (moe_w_rg[i], moe_w_re[i], moe_w_gate[i], moe_w_up[i], moe_w_down[i])
        if ctx_needed:
            out = jnp.concatenate([mix_c, mix_l], axis=1) @ w_out
            xc = xc + cg1 * out[:, :n_ctx]
            x = x + g1 * out[:, n_ctx:]
            f = hier_moe(jnp.concatenate([modnorm(xc, norm_g[i, 1], csh2, csc2),
                                          modnorm(x, norm_g[i, 1], sh2, sc2)], axis=1), *moe_p)
            xc = xc + cg2 * f[:, :n_ctx]
            x = x + g2 * f[:, n_ctx:]
        else:
            x = x + g1 * (mix_l @ w_out)
            x = x + g2 * hier_moe(modnorm(x, norm_g[i, 1], sh2, sc2), *moe_p)
    return rmsnorm(x, final_g)
```

```python
import contextlib
import math
import numpy as np
import ml_dtypes
import concourse.bass as bass
import concourse.mybir as mybir
from concourse.bass_utils import run_bass_kernel_spmd

F32 = mybir.dt.float32
BF16 = mybir.dt.bfloat16
AF = mybir.ActivationFunctionType
ALU = mybir.AluOpType
AX = mybir.AxisListType


class Tile:
    def __init__(self, t, name=""):
        self.t = t
        self.name = name
        self.w = {}
        self.r = {}
        self.dkey = None

    def __getitem__(self, k):
        return self.t[k]


class Sched:
    ENG = ("pe", "act", "dve", "pool", "sp")

    def __init__(self, nc, stack):
        self.nc = nc
        self.stack = stack
        self.streams = {e: [] for e in self.ENG}
        self.cnt = {e: 0 for e in self.ENG}
        self.sems = {}
        self.known = {e: {} for e in self.ENG}
        for e in self.ENG:
            self.sems[e] = stack.enter_context(nc.semaphore("s_" + e))
        self.dfree = []
        self.dtiles = []
        self.dcount = {}
        self.ndsem = 0
        self.n_inst = 0
        self.uid = 0
        self.use_dummy = False

    def sbuf(self, shape, dtype, name, stack=None):
        self.uid += 1
        t = (stack or self.stack).enter_context(self.nc.sbuf_tensor("%s_%d" % (name, self.uid), list(shape), dtype))
        try:
            rem = self.nc.sbuf_bytes_remaining
            if rem < getattr(self, "min_rem", 1 << 60):
                self.min_rem = rem
                self.min_rem_at = name
        except Exception:
            pass
        return Tile(t, name)

    def psum(self, shape, dtype, name, stack=None):
        self.uid += 1
        t = (stack or self.stack).enter_context(self.nc.psum_tensor("%s_%d" % (name, self.uid), list(shape), dtype))
        return Tile(t, name)

    def dram(self, name, shape, dtype, kind="Internal"):
        t = self.nc.dram_tensor(name, list(shape), dtype, kind=kind)
        return Tile(t.ap(), name)

    def _dkey(self, tile):
        if tile.dkey is None:
            if self.dfree:
                tile.dkey = self.dfree.pop()
            else:
                key = "d%d" % self.ndsem
                self.ndsem += 1
                self.sems[key] = self.stack.enter_context(self.nc.semaphore("s_" + key))
                self.dcount[key] = 0
                tile.dkey = key
            self.dtiles.append(tile)
        return tile.dkey

    def release(self, tiles):
        for t in tiles:
            if t.dkey is not None:
                self.dfree.append(t.dkey)
                t.dkey = None

    def _waits(self, eng, reads, writes):
        need = {}

        def add(kv):
            if kv is None:
                return
            k, v = kv
            if k == eng and eng == "pe":
                return
            if need.get(k, 0) < v:
                need[k] = v

        for d in reads:
            for kv in d.w.items():
                add(kv)
        for d in writes:
            for kv in d.w.items():
                add(kv)
            for k, v in d.r.items():
                if k == eng:
                    continue
                add((k, v))
        out = []
        kn = self.known[eng]
        for k, v in need.items():
            if kn.get(k, 0) >= v:
                continue
            kn[k] = v
            out.append((k, v))
        return out

    def op(self, eng, fn, reads=(), writes=(), dummy=None):
        waits = self._waits(eng, reads, writes)
        self.cnt[eng] += 1
        val = self.cnt[eng]
        sem = self.sems[eng]
        sems = self.sems

        def emit(e, waits=waits, fn=fn, sem=sem):
            for k, v in waits:
                e.wait_ge(sems[k], v)
            if waits and dummy is not None and self.use_dummy:
                dummy(e)
                dummy(e)
            fn(e).then_inc(sem, 1)

        self.streams[eng].append(emit)
        for d in reads:
            if d.r.get(eng, 0) < val:
                d.r[eng] = val
        for d in writes:
            d.w[eng] = val
            d.r = {}
        self.n_inst += 1

    def i(self, eng, method, *args, reads=(), writes=(), **kw):
        def fn(e, method=method, args=args, kw=kw):
            return getattr(e, method)(*args, **kw)
        self.op(eng, fn, reads=reads, writes=writes)

    def mm(self, out_t, out_ap, lhs_t, lhs_ap, rhs_t, rhs_ap, start=True, stop=True, extra_reads=()):
        reads = [lhs_t, rhs_t] + list(extra_reads)
        M, N = out_ap.shape[0], out_ap.shape[-1]
        pd = self.pdummy

        def dummy(e):
            e.matmul(pd[0:M, 0:N], lhs_ap, rhs_ap, start=True, stop=True)

        self.op("pe", lambda e: e.matmul(out_ap, lhs_ap, rhs_ap, start=start, stop=stop), reads=reads, writes=[out_t], dummy=dummy)

    def tr(self, out_t, out_ap, in_t, in_ap, ident_t, ident_ap):
        pd = self.pdummy_bf if in_ap.dtype == BF16 else self.pdummy
        M, N = out_ap.shape[0], out_ap.shape[-1]

        def dummy(e):
            e.transpose(pd[0:M, 0:N], in_ap, ident_ap)

        self.op("pe", lambda e: e.transpose(out_ap, in_ap, ident_ap), reads=[in_t, ident_t], writes=[out_t], dummy=dummy)

    def dma(self, q, out_ap, in_ap, reads=(), writes=(), semtile=None, **kw):
        if semtile is None:
            semtile = writes[0]
        key = self._dkey(semtile)
        waits = self._waits(q, reads, writes)
        self.dcount[key] += 16
        val = self.dcount[key]
        sems = self.sems

        def emit(e, waits=waits):
            for k, v in waits:
                e.wait_ge(sems[k], v)
            e.dma_start(out=out_ap, in_=in_ap, **kw).then_inc(sems[key], 16)

        self.streams[q].append(emit)
        for d in reads:
            if d.r.get(key, 0) < val:
                d.r[key] = val
        for d in writes:
            d.w[key] = val
            d.r = {}
        self.n_inst += 1

    def barrier(self):
        targets = {e: self.cnt[e] for e in self.ENG if self.cnt[e] > 0}
        for k, v in self.dcount.items():
            if v > 0:
                targets[k] = v
        sems = self.sems
        for eng in self.ENG:
            kn = self.known[eng]
            ws = []
            for k, v in targets.items():
                if k == eng:
                    continue
                if kn.get(k, 0) >= v:
                    continue
                kn[k] = v
                ws.append((k, v))

            def emit(e, ws=ws):
                for k, v in ws:
                    e.wait_ge(sems[k], v)

            self.streams[eng].append(emit)
        for t in self.dtiles:
            if t.dkey is not None:
                self.dfree.append(t.dkey)
                t.dkey = None
        self.dtiles = []

    def emit(self):
        nc = self.nc
        with nc.Block() as block:
            @block.tensor
            def _(e):
                for f in self.streams["pe"]:
                    f(e)

            @block.scalar
            def _(e):
                for f in self.streams["act"]:
                    f(e)

            @block.vector
            def _(e):
                for f in self.streams["dve"]:
                    f(e)

            @block.gpsimd
            def _(e):
                for f in self.streams["pool"]:
                    f(e)

            @block.sync
            def _(e):
                for f in self.streams["sp"]:
                    f(e)
import os

D = 1024
NCTX = 256
NLAT = 2048
C0 = 2
L0 = 260
W = 2310
EPS = 1e-6
BLKS = [(C0, 256, "c")] + [(L0 + 512 * i, 512, "l") for i in range(4)]
TTILES = [(C0 + 128 * i, "c", i) for i in range(2)] + [(L0 + 128 * i, "l", i) for i in range(16)]
TWO_PI = 2.0 * math.pi


def _bf(a):
    return np.ascontiguousarray(a.astype(ml_dtypes.bfloat16))


def _f(a):
    return np.ascontiguousarray(a, dtype=np.float32)


def host_consts():
    Cn = {}
    Cn["ident"] = np.eye(128, dtype=np.float32)
    Cn["ones"] = np.ones((128, 128), np.float32)
    bo = np.zeros((128, 128), np.float32)
    bo[:64, :64] = 1.0
    bo[64:, 64:] = 1.0
    Cn["blockones"] = bo
    t = np.arange(NLAT)
    rows = (t // 64).astype(np.float32)
    cols = (t % 64).astype(np.float32)

    def rope_tab(dh):
        a = dh // 2
        half = a // 2
        freqs = (10000.0 ** (-np.arange(half, dtype=np.float32) / half)).astype(np.float32)
        cos = np.zeros((dh, NLAT), np.float32)
        sin = np.zeros((dh, NLAT), np.float32)
        R = np.zeros((dh, dh), np.float32)
        for d in range(dh):
            part = d // a
            i = d % a
            fi = i % half
            pos = rows if part == 0 else cols
            ang = (pos * freqs[fi]).astype(np.float32)
            cos[d] = np.cos(ang)
            sin[d] = np.sin(ang)
            if i < half:
                R[d, d + half] = -1.0
            else:
                R[d, d - half] = 1.0
        return cos, sin, R

    cos64, sin64, R64 = rope_tab(64)
    Cn["cos_e"] = np.concatenate([cos64, cos64], 0)
    Cn["sin_e"] = np.concatenate([sin64, sin64], 0)
    Rm = np.zeros((128, 128), np.float32)
    Rm[:64, :64] = R64
    Rm[64:, 64:] = R64
    Cn["rotT_e"] = _bf(Rm.T)
    cos32, sin32, R32 = rope_tab(32)
    co = np.zeros((128, NLAT), np.float32)
    so = np.zeros((128, NLAT), np.float32)
    co[64:96] = cos32
    so[64:96] = sin32
    Cn["cos_o"] = co
    Cn["sin_o"] = so
    Ro = np.zeros((128, 128), np.float32)
    Ro[64:96, 64:96] = R32
    Cn["rotT_o"] = _bf(Ro.T)
    for n, tag in ((NLAT, "l"), (NCTX, "c")):
        NT = n // 128
        N2 = 2 * n
        tt = np.arange(n, dtype=np.float64)
        kk = np.arange(n, dtype=np.float64)
        ang = 2.0 * np.pi * np.outer(tt, kk) / N2
        fre = np.cos(ang)
        fim = -np.sin(ang)
        fim[:, 0] = np.cos(np.pi * tt)
        fwd = np.concatenate([fre, fim], 1)
        fw = fwd.reshape(NT, 128, 2, NT, 128).transpose(3, 2, 1, 0, 4)
        Cn["fwd_" + tag] = _bf(fw)
        ire = (2.0 / N2) * np.cos(ang.T)
        ire[0, :] = 1.0 / N2
        iim = -(2.0 / N2) * np.sin(ang.T)
        iim[0, :] = (1.0 / N2) * np.cos(np.pi * tt)
        inv = np.concatenate([ire, iim], 0)
        iv = inv.reshape(2 * NT, 128, NT, 128).transpose(2, 1, 0, 3)
        Cn["inv_" + tag] = _bf(iv)
        tf = np.arange(n, dtype=np.float32)
        t_norm = tf / max(n - 1, 1)
        bands = np.linspace(1e-4, 15, 16, dtype=np.float32)
        a2 = (np.float32(2.0 * math.pi / n) * tf[:, None] * bands).astype(np.float32)
        z = np.concatenate([t_norm[:, None], np.cos(a2), np.sin(a2)], -1).astype(np.float32)
        Cn["zT_" + tag] = _f(z.T)
        HMAX = math.log(100.0) / 0.3
        HMIN = math.log(100.0) / 1.5
        deltas = np.linspace(HMIN, HMAX, 512, dtype=np.float32)
        dec = np.exp(-t_norm[:, None] * deltas).astype(np.float32)
        Cn["dec_" + tag] = _f(dec.reshape(NT, 128, 512).transpose(1, 0, 2))
    kk_ = np.arange(NCTX + NLAT)
    Cn["s5k1s"] = _f(np.broadcast_to(np.arange(36, dtype=np.float32), (128, 32, 36)))
    Cn["s5k0s"] = _f(np.broadcast_to(np.arange(64, dtype=np.float32), (128, 32, 64)))
    sel = np.zeros((32, 32, 128), np.float32)
    for e in range(32):
        sel[e, e, :] = 1.0
    return Cn


class Prog:
    def __init__(self, stop="all", dbg=()):
        self.stop = stop
        self.dbg = set(dbg)
        self.nc = bass.Bass("TRN2", target_bir_lowering=False)
        self.in_names = []
        self.out_names = []

    def inp(self, name, shape, dtype=F32):
        if self.stop == "modvec" and name not in ("cT", "w_mod", "bmodT", "ngT", "ident", "ones", "blockones"):
            return None
        self.in_names.append(name)
        return self.S.dram(name, shape, dtype, kind="ExternalInput")

    def outp(self, name, shape, dtype=F32):
        self.out_names.append(name)
        return self.S.dram(name, shape, dtype, kind="ExternalOutput")

    def load(self, dst, src_ap, src, q="sp", dst_ap=None):
        self.S.dma(q, dst[:] if dst_ap is None else dst_ap, src_ap, reads=[src], writes=[dst])

    def const_tile(self, name, shape, dtype, val):
        t = self.S.sbuf(shape, dtype, name)
        self.S.i("pool", "memset", t[:], val, writes=[t])
        return t

    def build(self):
        nc = self.nc
        with contextlib.ExitStack() as st:
            S = self.S = Sched(nc, st)
            self.declare_io()
            self.setup_consts()
            self.S.barrier()
            self.modvecs()
            self.S.barrier()
            if self.stop != "modvec":
                if self.stop not in ("mn1", "stage") and not os.environ.get("KSKIP0"):
                    self.hyena_filters()
                self.s5_tables()
                for b in range(2):
                    self.batch(b)
            self.S.barrier()
            S.emit()
        return nc

    def declare_io(self):
        I = self.I = {}
        I["xT"] = self.inp("xT", [2, D, NCTX + NLAT])
        I["cT"] = self.inp("cT", [128, 8, 4])
        I["w_mod"] = self.inp("w_mod", [2, D, 6 * D])
        I["bmodT"] = self.inp("bmodT", [128, 2, 48])
        I["ngT"] = self.inp("ngT", [128, 2, 2, 8])
        I["e_w_in"] = self.inp("e_w_in", [D, 2304])
        I["convw"] = self.inp("convw", [3, 1536])
        I["convb"] = self.inp("convb", [1, 1536])
        I["hy_w1"] = self.inp("hy_w1", [33, 64])
        I["hy_w2"] = self.inp("hy_w2", [64, 64])
        I["hy_w3"] = self.inp("hy_w3", [64, 2048])
        I["hy_vec"] = self.inp("hy_vec", [64, 3])
        I["fbias"] = self.inp("fbias", [2, 512])
        I["qkg"] = self.inp("qkg", [128, 2])
        I["e_w_out"] = self.inp("e_w_out", [D, D])
        I["wr"] = self.inp("wr", [128, 2, 8, 36])
        I["moe_w_gate"] = self.inp("moe_w_gate", [2, 32, D, 256])
        I["moe_w_up"] = self.inp("moe_w_up", [2, 32, D, 256])
        I["moe_w_down"] = self.inp("moe_w_down", [2, 32, 256, D])
        I["finalgT"] = self.inp("finalgT", [128, 8])
        I["o_w_in"] = self.inp("o_w_in", [D, 928])
        I["o_ng"] = self.inp("o_ng", [128, 3])
        I["o_w_uq"] = self.inp("o_w_uq", [256, 768])
        I["o_w_ukv"] = self.inp("o_w_ukv", [128, 1024])
        I["o_w_out"] = self.inp("o_w_out", [D, D])
        I["s5p"] = self.inp("s5p", [128, 3, 32])
        I["s5BD"] = self.inp("s5BD", [2, 2, 16, 128, 128])
        I["s5CD"] = self.inp("s5CD", [2, 2, 16, 128, 128])
        I["dskT"] = self.inp("dskT", [128, 4])
        I["glu_w"] = self.inp("glu_w", [512, 512])
        I["glu_bT"] = self.inp("glu_bT", [128, 4])
        for k, v in HC_SHAPES.items():
            I[k] = self.inp(k, v[0], v[1])
        self.out = self.outp("outT", [2, D, NLAT])
        S = self.S
        self.XT_d = [S.dram("XT_d%d" % b, [D, W], F32) for b in range(2)]
        self.HY_d = S.dram("HY_d", [3, NCTX + NLAT, 512], BF16)
        self.G_d = S.dram("G_d", [32, W], BF16)
        self.H2_d = S.dram("H2_d", [D, W], BF16)
        self.Hf_d = {"l": S.dram("Hf_l", [2, 16, 128, 2, 512], F32), "c": S.dram("Hf_c", [2, 2, 128, 2, 512], F32)}
        self.D = {}
        for name in self.dbg:
            self.D[name] = self.outp("dbg_" + name, DBG_SHAPES[name][0], DBG_SHAPES[name][1])

    def setup_consts(self):
        S, I = self.S, self.I
        self.ident = S.sbuf([128, 128], F32, "ident")
        self.load(self.ident, I["ident"][:], I["ident"])
        self.identb = S.sbuf([128, 128], BF16, "identb")
        self.load(self.identb, I["ident"][:], I["ident"], q="pool")
        self.ones = S.sbuf([128, 128], F32, "ones")
        self.load(self.ones, I["ones"][:], I["ones"])
        self.blockones = S.sbuf([128, 128], F32, "blockones")
        self.load(self.blockones, I["blockones"][:], I["blockones"])
        S.zeros_f = self.const_tile("zeros_f", [128, 128], F32, 0.0)
        S.zeros_bf = self.const_tile("zeros_bf", [128, 128], BF16, 0.0)
        self.eps = self.const_tile("eps", [128, 1], F32, EPS)
        self.negpi = self.const_tile("negpi", [128, 1], F32, -math.pi)
        self.MV = S.sbuf([128, 2, 6, 8, 4], F32, "MV")
        self.PS = [S.psum([128, 512], F32, "ps%d" % i) for i in range(7)]
        pdt = st_psum = S.psum([128, 512], F32, "psdummy")
        S.pdummy = pdt.t
        S.pdummy_bf = pdt[:, :].bitcast(BF16)
        self.psi = 0
        self.ps_res = set()

    def ps(self, reserve=False):
        while True:
            i = self.psi % 7
            self.psi += 1
            if i not in self.ps_res:
                break
        if reserve:
            self.ps_res.add(i)
        return self.PS[i]

    def ps_free(self, p):
        self.ps_res.discard(self.PS.index(p))

    def modvecs(self):
        S, I = self.S, self.I
        with contextlib.ExitStack() as ph:
            cT = S.sbuf([128, 8, 4], F32, "cT", ph)
            self.load(cT, I["cT"][:], I["cT"])
            scT = S.sbuf([128, 8, 4], F32, "scT", ph)
            S.i("act", "activation", scT[:], cT[:], AF.Silu, reads=[cT], writes=[scT])
            bm = S.sbuf([128, 2, 48], F32, "bm", ph)
            self.load(bm, I["bmodT"][:], I["bmodT"])
            ng = S.sbuf([128, 2, 2, 8], F32, "ng", ph)
            self.load(ng, I["ngT"][:], I["ngT"])
            wm = [S.sbuf([128, 8, 512], F32, "wm%d" % i, ph) for i in range(2)]
            modT = S.sbuf([128, 48, 4], F32, "modT", ph)
            MV = self.MV
            for i in range(2):
                pm = self.ps()
                pmv = pm[:, 0:192].rearrange("p (m n) -> p m n", n=4)
                wsrc = I["w_mod"][i].rearrange("(k p) n -> p k n", p=128)
                for cb in range(12):
                    w = wm[cb % 2]
                    self.load(w, wsrc[:, :, cb * 512:(cb + 1) * 512], I["w_mod"])
                    for mm in range(4):
                        m = cb * 4 + mm
                        for k in range(8):
                            S.mm(pm, pmv[:, m, :], w, w[:, k, mm * 128:(mm + 1) * 128], scT, scT[:, k, :], start=(k == 0), stop=(k == 7))
                S.i("dve", "tensor_copy", modT[:].rearrange("p m n -> p (m n)"), pm[:, 0:192], reads=[pm], writes=[modT])
                for n_ in range(4):
                    S.i("dve", "tensor_tensor", modT[:, :, n_], modT[:, :, n_], bm[:, i, :], ALU.add, reads=[modT, bm], writes=[modT])
                for kind, lo in ((0, 0), (2, 16), (3, 24), (5, 40)):
                    S.i("dve", "tensor_copy", MV[:, i, kind], modT[:, lo:lo + 8, :], reads=[modT], writes=[MV])
                for kind, lo, w_ in ((1, 8, 0), (4, 32, 1)):
                    S.i("dve", "tensor_single_scalar", MV[:, i, kind], modT[:, lo:lo + 8, :], 1.0, ALU.add, reads=[modT], writes=[MV])
                    for n_ in range(4):
                        S.i("dve", "tensor_tensor", MV[:, i, kind, :, n_], MV[:, i, kind, :, n_], ng[:, i, w_, :], ALU.mult, reads=[MV, ng], writes=[MV])
            if "MV" in self.D:
                S.dma("sp", self.D["MV"][:], MV[:], reads=[MV], writes=[self.D["MV"]], semtile=MV)
            S.barrier()

    def mv(self, layer, kind, c, n):
        return self.MV[:, layer, kind, c, n:n + 1]

    def modnorm_block(self, xt, n, layer, kindA, kindS, ncol, outs, otiles, h32=None):
        S = self.S
        T = self._mn[self._mni % len(self._mn)]
        self._mni += 1
        sq, rstd, tmp = T["sq"], T["rstd"], T["tmp"]
        ss = self.ps()
        S.i("act", "activation", sq[:, :, :n], xt[:, :, :n], AF.Square, reads=[xt], writes=[sq])
        for c in range(8):
            S.mm(ss, ss[:, :n], self.ones, self.ones[:], sq, sq[:, c, :n], start=(c == 0), stop=(c == 7))
        S.i("act", "activation", rstd[:, :n], ss[:, :n], AF.Sqrt, bias=self.eps[:], scale=1.0 / D, reads=[ss, self.eps], writes=[rstd])
        S.i("dve", "reciprocal", rstd[:, :n], rstd[:, :n], reads=[rstd], writes=[rstd])
        for c in range(8):
            S.i("dve", "scalar_tensor_tensor", tmp[:, c, :n], xt[:, c, :n], self.mv(layer, kindA, c, ncol), rstd[:, :n], ALU.mult, ALU.mult,
                reads=[xt, self.MV, rstd], writes=[tmp])
        for c in range(8):
            S.i("act", "activation", outs[c], tmp[:, c, :n], AF.Identity, bias=self.mv(layer, kindS, c, ncol), reads=[tmp, self.MV], writes=[otiles[c]])
            if h32 is not None:
                S.i("act", "activation", h32[:, c, :n], tmp[:, c, :n], AF.Identity, bias=self.mv(layer, kindS, c, ncol), reads=[tmp, self.MV], writes=[h32])

    def mn_alloc(self, ph, nbuf=1):
        S = self.S
        self._mni = 0
        self._mn = [{"sq": S.sbuf([128, 8, 512], F32, "mn_sq", ph), "rstd": S.sbuf([128, 512], F32, "mn_rstd", ph),
                     "tmp": S.sbuf([128, 8, 512], F32, "mn_tmp", ph)} for _ in range(nbuf)]

    def dump(self, name, src_tile, src_ap, dst_ap=None):
        if name in self.D and getattr(self, "cur_b", 0) == 0:
            d = self.D[name]
            self.S.dma("sp", d[:] if dst_ap is None else dst_ap(d), src_ap, reads=[src_tile], writes=[d], semtile=d)

    def batch(self, b):
        S, I = self.S, self.I
        self.cur_b = b
        XT = self.XT_d[b]
        S.dma("sp", XT[:, C0:C0 + NCTX], I["xT"][b, :, 0:NCTX], reads=[I["xT"]], writes=[XT], semtile=XT)
        S.dma("sp", XT[:, L0:L0 + NLAT], I["xT"][b, :, NCTX:], reads=[I["xT"]], writes=[XT], semtile=XT)
        S.barrier()
        if self.stop == "stage":
            self.dump("XT", XT, XT[:])
            return
        if not os.environ.get("KSKIP0"):
            self.layer0(b)
        S.barrier()
        if self.stop in ("l0", "mn1", "inproj", "mix0", "xa0"):
            return
        self.layer1(b)
        S.barrier()

    def xt_view(self, b):
        return self.XT_d[b][:, :].rearrange("(k p) w -> p k w", p=128)

    def layer0(self, b):
        S, I = self.S, self.I
        XTv = self.xt_view(b)
        XT = self.XT_d[b]
        with contextlib.ExitStack() as LY:
            with contextlib.ExitStack() as L1:
                hT = [S.sbuf([128, W], BF16, "hT%d" % c, L1) for c in range(8)]
                for c in range(8):
                    for (a0, a1) in ((0, C0), (C0 + NCTX, L0), (L0 + NLAT, W)):
                        S.i("pool", "memset", hT[c][:, a0:a1], 0.0, writes=[hT[c]])
                with contextlib.ExitStack() as L2:
                    QT = [S.sbuf([128, W], BF16, "QT%d" % j, L2) for j in range(4)]
                    KK = [S.sbuf([128, W], BF16, "KK%d" % g, L2) for g in range(2)]
                    VA = S.sbuf([128, 18, 2, 128], BF16, "VA", L2)
                    S.i("pool", "memset", VA[:, :, :, 64:128], 1.0, writes=[VA])
                    with contextlib.ExitStack() as ph:
                        self.mn_alloc(ph, nbuf=2)
                        xb = [S.sbuf([128, 8, 512], F32, "xb%d" % i, ph) for i in range(2)]
                        for bi, (s, n, kind) in enumerate(BLKS):
                            xt = xb[bi % 2]
                            self.load(xt, XTv[:, :, s:s + n], XT, dst_ap=xt[:, :, :n])
                            self.modnorm_block(xt, n, 0, 1, 0, 2 if kind == "c" else b, [hT[c][:, s:s + n] for c in range(8)], hT)
                        S.barrier()
                    for c in range(8):
                        self.dump("hT", hT[c], hT[c][:], lambda d, c=c: d[c])
                    if self.stop == "mn1":
                        return
                    with contextlib.ExitStack() as ph:
                        self.inproj_even(b, hT, QT, KK, VA, ph)
                        S.barrier()
                    for j in range(4):
                        self.dump("QT", QT[j], QT[j][:], lambda d, j=j: d[j])
                    for g in range(2):
                        self.dump("KK", KK[g], KK[g][:], lambda d, g=g: d[g])
                    self.dump("VA", VA, VA[:])
                    self.dump("HY", self.HY_d, self.HY_d[:])
                    if self.stop == "inproj":
                        return
                    mixT = hT
                    with contextlib.ExitStack() as ph:
                        self.attention_even(b, QT, KK, VA, mixT, ph)
                        S.barrier()
                with contextlib.ExitStack() as ph:
                    self.hyena(b, mixT, ph)
                    S.barrier()
                for c in range(8):
                    self.dump("mixT", mixT[c], mixT[c][:], lambda d, c=c: d[c])
                if self.stop == "mix0":
                    return
                with contextlib.ExitStack() as ph:
                    self.outproj_norm_router(b, 0, mixT, I["e_w_out"], ph, with_ctx=True)
                    S.barrier()
                self.dump("XT", XT, XT[:])
                self.dump("G", self.G_d, self.G_d[:])
                if self.stop == "xa0":
                    return
            with contextlib.ExitStack() as ph:
                self.moe(b, 0, ph, with_ctx=True)
                S.barrier()
            if b == 0:
                self.dump("XT", XT, XT[:])

    def inproj_even(self, b, hT, QT, KK, VA, ph):
        S, I = self.S, self.I
        wsrc = I["e_w_in"][:, :].rearrange("(k p) n -> p k n", p=128)
        wq = S.sbuf([128, 8, 768], BF16, "wqkv", ph)
        self.load(wq, wsrc[:, :, 1536:2304], I["e_w_in"], q="pool")
        wkk = S.sbuf([128, 8, 2, 128], BF16, "wkk", ph)
        for g in range(2):
            for hf in range(2):
                S.i("dve", "tensor_copy", wkk[:, :, g, hf * 64:(hf + 1) * 64], wq[:, :, 512 + 64 * g:576 + 64 * g], reads=[wq], writes=[wkk])
        qkg = S.sbuf([128, 2], F32, "qkg", ph)
        self.load(qkg, I["qkg"][:], I["qkg"])
        cos = S.sbuf([128, NLAT], F32, "cos", ph)
        sin = S.sbuf([128, NLAT], F32, "sin", ph)
        self.load(cos, I["cos_e"][:], I["cos_e"])
        self.load(sin, I["sin_e"][:], I["sin_e"])
        rotT = S.sbuf([128, 128], BF16, "rotT", ph)
        self.load(rotT, I["rotT_e"][:], I["rotT_e"])
        VT = S.sbuf([128, W], BF16, "VT", ph)
        sqh = S.sbuf([128, 512], F32, "sqh", ph)
        rs = S.sbuf([128, 512], F32, "rs", ph)
        qn = S.sbuf([128, 512], BF16, "qn", ph)
        t1 = S.sbuf([128, 512], F32, "t1", ph)
        t2 = S.sbuf([128, 512], F32, "t2", ph)
        mt = [("q", j) for j in range(4)] + [("k", g) for g in range(2)] + [("v", 0)]
        for (s, n, kind) in BLKS:
            for (typ, j) in mt:
                ps = self.ps()
                for kc in range(8):
                    if typ == "q":
                        lap = wq[:, kc, j * 128:(j + 1) * 128]
                        lt = wq
                    elif typ == "k":
                        lap = wkk[:, kc, j, :]
                        lt = wkk
                    else:
                        lap = wq[:, kc, 640:768]
                        lt = wq
                    S.mm(ps, ps[:, :n], lt, lap, hT[kc], hT[kc][:, s:s + n], start=(kc == 0), stop=(kc == 7))
                if typ == "v":
                    S.i("act", "activation", VT[:, s:s + n], ps[:, :n], AF.Copy, reads=[ps], writes=[VT])
                    continue
                dst_t = QT[j] if typ == "q" else KK[j]
                gi = 0 if typ == "q" else 1
                S.i("act", "activation", sqh[:, :n], ps[:, :n], AF.Square, reads=[ps], writes=[sqh])
                p2 = self.ps()
                S.mm(p2, p2[:, :n], self.blockones, self.blockones[:], sqh, sqh[:, :n])
                S.i("act", "activation", rs[:, :n], p2[:, :n], AF.Sqrt, bias=self.eps[:], scale=1.0 / 64, reads=[p2, self.eps], writes=[rs])
                S.i("dve", "reciprocal", rs[:, :n], rs[:, :n], reads=[rs], writes=[rs])
                if kind == "c":
                    S.i("dve", "scalar_tensor_tensor", dst_t[:, s:s + n], ps[:, :n], qkg[:, gi:gi + 1], rs[:, :n], ALU.mult, ALU.mult, reads=[ps, qkg, rs], writes=[dst_t])
                else:
                    tc0 = s - L0
                    S.i("dve", "scalar_tensor_tensor", qn[:, :n], ps[:, :n], qkg[:, gi:gi + 1], rs[:, :n], ALU.mult, ALU.mult, reads=[ps, qkg, rs], writes=[qn])
                    p3 = self.ps()
                    S.mm(p3, p3[:, :n], rotT, rotT[:], qn, qn[:, :n])
                    S.i("pool", "tensor_tensor", t1[:, :n], qn[:, :n], cos[:, tc0:tc0 + n], ALU.mult, reads=[qn, cos], writes=[t1])
                    S.i("dve", "tensor_tensor", t2[:, :n], p3[:, :n], sin[:, tc0:tc0 + n], ALU.mult, reads=[p3, sin], writes=[t2])
                    S.i("dve", "tensor_tensor", dst_t[:, s:s + n], t1[:, :n], t2[:, :n], ALU.add, reads=[t1, t2], writes=[dst_t])
        for ti, (s, kind, _) in enumerate(TTILES):
            pt = self.ps()
            ptb = pt[:, 0:64].bitcast(BF16)
            S.tr(pt, ptb, VT, VT[:, s:s + 128], self.identb, self.identb[:])
            S.i("act", "activation", VA[:, ti, :, 0:64], ptb.rearrange("p (g d) -> p g d", g=2), AF.Copy, reads=[pt], writes=[VA])
        cw = S.sbuf([128, 3, 512], F32, "cw", ph)
        cb = S.sbuf([128, 512], F32, "cb", ph)
        wh = S.sbuf([128, 8, 512], BF16, "wh", ph)
        whs = [S.sbuf([128, 8, 512], BF16, "whs%d" % t, ph) for t in range(3)]
        ob = [S.sbuf([128, 512], BF16, "hyo%d" % i, ph) for i in range(2)]
        oi = 0
        for nb in range(3):
            self.load(wh, wsrc[:, :, nb * 512:(nb + 1) * 512], I["e_w_in"], q="pool")
            self.load(cw, I["convw"][:, nb * 512:(nb + 1) * 512].partition_broadcast(128), I["convw"])
            self.load(cb, I["convb"][0, nb * 512:(nb + 1) * 512].partition_broadcast(128), I["convb"])
            for tap in range(3):
                for kc in range(8):
                    S.i("dve", "tensor_tensor", whs[tap][:, kc, :], wh[:, kc, :], cw[:, tap, :], ALU.mult, reads=[wh, cw], writes=[whs[tap]])
            for ti, (s, kind, idx) in enumerate(TTILES):
                ps = self.ps()
                first = True
                for tap in range(3):
                    for kc in range(8):
                        S.mm(ps, ps[:, :], hT[kc], hT[kc][:, s + tap - 1:s + tap - 1 + 128], whs[tap], whs[tap][:, kc, :], start=first, stop=(tap == 2 and kc == 7))
                        first = False
                o = ob[oi % 2]
                oi += 1
                S.i("dve", "tensor_tensor", o[:], ps[:, :], cb[:], ALU.add, reads=[ps, cb], writes=[o])
                row = ti * 128
                S.dma("sp", self.HY_d[nb, row:row + 128, :], o[:], reads=[o], writes=[self.HY_d], semtile=o)


    def attn_multi(self, streams, nkt, n, scale, ph_t):
        S = self.S
        ns = len(streams)
        for st_ in streams:
            st_["oacc"] = self.ps(reserve=True)
        npt = len(ph_t["pT"])
        cnt = [0]

        def finish(si, kt_, sT_):
            st_ = streams[si]
            pT = ph_t["pT"][cnt[0] % npt]
            cnt[0] += 1
            oacc = st_["oacc"]
            S.i("act", "activation", pT[:, :n], sT_[:, :n], AF.Exp, scale=scale, reads=[sT_], writes=[pT])
            S.mm(oacc, oacc[:, :n], st_["Vt"], st_["Vap"](kt_), pT, pT[:, :n], start=(kt_ == 0), stop=(kt_ == nkt - 1))

        depth = 2 if ns == 1 else 1
        pend = []
        for kt in range(nkt):
            for si, st_ in enumerate(streams):
                sT = self.ps()
                S.mm(sT, sT[:, :n], st_["KTt"], st_["Kap"](kt), st_["QTt"], st_["Qap"])
                pend.append((si, kt, sT))
            while len(pend) > depth * ns:
                finish(*pend.pop(0))
        while pend:
            finish(*pend.pop(0))
        for si, st_ in enumerate(streams):
            den = ph_t["den"][si]
            oacc = st_["oacc"]
            S.i("act", "activation", den[0:64, :n], oacc[64:128, :n], AF.Copy, reads=[oacc], writes=[den])
            S.i("dve", "reciprocal", den[0:64, :n], den[0:64, :n], reads=[den], writes=[den])
            S.i("dve", "tensor_tensor", st_["out_ap"], oacc[0:64, :n], den[0:64, :n], ALU.mult, reads=[oacc, den], writes=[st_["out_t"]])
            self.ps_free(oacc)

    def attention_even(self, b, QT, KK, VA, mixT, ph):
        S = self.S
        T = {"pT": [S.sbuf([128, 512], BF16, "pT%d" % i, ph) for i in range(6)], "den": [S.sbuf([64, 512], F32, "den%d" % i, ph) for i in range(2)]}

        def kcol(kt):
            return (C0 + 128 * kt) if kt < 2 else (L0 + 128 * (kt - 2))

        for hp in range(4):
            g = hp // 2
            j = hp
            out_t = mixT[4 + j]
            blocks = [(C0, 256, 2)] + [(L0 + 512 * i, 512, 18) for i in range(4)]
            for (s, n, nkt) in blocks:
                streams = []
                for po in (0, 64):
                    streams.append({"QTt": QT[j], "Qap": QT[j][po:po + 64, s:s + n], "KTt": KK[g],
                                    "Kap": (lambda kt, po=po: KK[g][po:po + 64, kcol(kt):kcol(kt) + 128]),
                                    "Vt": VA, "Vap": (lambda kt: VA[:, kt, g, :]), "out_t": out_t, "out_ap": out_t[po:po + 64, s:s + n]})
                self.attn_multi(streams, nkt, n, 0.125, T)

    def hyena_filters(self):
        S, I = self.S, self.I
        with contextlib.ExitStack() as ph:
            w1 = S.sbuf([33, 64], F32, "hw1", ph)
            w2 = S.sbuf([64, 64], F32, "hw2", ph)
            w3 = S.sbuf([64, 2048], F32, "hw3", ph)
            hv = S.sbuf([64, 3], F32, "hv", ph)
            self.load(w1, I["hy_w1"][:], I["hy_w1"])
            self.load(w2, I["hy_w2"][:], I["hy_w2"])
            self.load(w3, I["hy_w3"][:], I["hy_w3"])
            self.load(hv, I["hy_vec"][:], I["hy_vec"])
            fb = S.sbuf([64, 2], F32, "fb", ph)
            for i in range(2):
                S.i("dve", "tensor_tensor", fb[:, i:i + 1], hv[:, i:i + 1], hv[:, 2:3], ALU.mult, reads=[hv], writes=[fb])
            OFF = math.pi + TWO_PI * 16
            for tag, n in (("c", NCTX), ("l", NLAT)):
                NT = n // 128
                with contextlib.ExitStack() as p2:
                    zT = S.sbuf([33, n], F32, "zT", p2)
                    self.load(zT, I["zT_" + tag][:], I["zT_" + tag])
                    h1 = S.sbuf([64, n], F32, "h1", p2)
                    h2 = S.sbuf([64, n], F32, "h2", p2)
                    dec = S.sbuf([128, NT, 512], F32, "dec", p2)
                    self.load(dec, I["dec_" + tag][:], I["dec_" + tag])
                    U4 = S.sbuf([128, NT, 2048], BF16, "U4", p2)
                    a1 = S.sbuf([64, 512], F32, "a1", p2)
                    ki = S.sbuf([64, 512], mybir.dt.int32, "ki", p2)
                    kf = S.sbuf([64, 512], F32, "kf", p2)
                    tp = S.sbuf([128, 512], F32, "tp", p2)
                    sq = S.sbuf([128, 512], F32, "sqf", p2)
                    rn = S.sbuf([128, 2, 512], F32, "rn", p2)
                    nb = min(512, n)
                    for (src, wt, wap, dst, bi) in ((zT, w1, w1[:, :], h1, 0), (h1, w2, w2[:, :], h2, 1)):
                        for c0 in range(0, n, nb):
                            p = self.ps()
                            K = 33 if bi == 0 else 64
                            S.mm(p, p[0:64, :nb], wt, wap, src, src[0:K, c0:c0 + nb])
                            S.i("dve", "tensor_scalar", a1[:, :nb], p[0:64, :nb], hv[:, 2:3], fb[:, bi:bi + 1], ALU.mult, ALU.add, reads=[p, hv, fb], writes=[a1])
                            S.i("dve", "tensor_scalar", a1[:, :nb], a1[:, :nb], 1.0 / TWO_PI, 16.0, ALU.mult, ALU.add, reads=[a1], writes=[a1])
                            S.i("dve", "tensor_copy", ki[:, :nb], a1[:, :nb], reads=[a1], writes=[ki])
                            S.i("dve", "tensor_copy", kf[:, :nb], ki[:, :nb], reads=[ki], writes=[kf])
                            S.i("dve", "tensor_tensor", a1[:, :nb], a1[:, :nb], kf[:, :nb], ALU.subtract, reads=[a1, kf], writes=[a1])
                            S.i("dve", "tensor_single_scalar", kf[:, :nb], a1[:, :nb], 0.5, ALU.is_gt, reads=[a1], writes=[kf])
                            S.i("dve", "tensor_tensor", a1[:, :nb], a1[:, :nb], kf[:, :nb], ALU.subtract, reads=[a1, kf], writes=[a1])
                            S.i("act", "activation", dst[:, c0:c0 + nb], a1[:, :nb], AF.Sin, scale=TWO_PI, reads=[a1], writes=[dst])
                    ssum = [self.ps(reserve=True), self.ps(reserve=True)]
                    tpd = [S.sbuf([128, 512], F32, "tpd%d" % i, p2) for i in range(2)]
                    for tt in range(NT):
                        for o_ in range(2):
                            for d_ in range(2):
                                cbk = d_ * 2 + o_
                                tpx = tpd[d_]
                                p = self.ps()
                                S.mm(p, p[:, :], h2, h2[:, tt * 128:(tt + 1) * 128], w3, w3[:, cbk * 512:(cbk + 1) * 512])
                                S.i("dve", "tensor_tensor", tpx[:], p[:, :], dec[:, tt, :], ALU.mult, reads=[p, dec], writes=[tpx])
                                if d_ == 1 and tt == 0:
                                    S.i("dve", "memset", tpx[0:1, :], 0.0, writes=[tpx])
                                S.i("act", "activation", sq[:], tpx[:], AF.Square, reads=[tpx], writes=[sq])
                                S.mm(ssum[o_], ssum[o_][:, :], self.ones, self.ones[:], sq, sq[:], start=(tt == 0 and d_ == 0), stop=(tt == NT - 1 and d_ == 1))
                            S.i("dve", "tensor_tensor", U4[:, tt, o_ * 512:(o_ + 1) * 512], tpd[0][:], tpd[1][:], ALU.add, reads=tpd, writes=[U4])
                            S.i("dve", "tensor_tensor", U4[:, tt, (2 + o_) * 512:(3 + o_) * 512], tpd[0][:], tpd[1][:], ALU.subtract, reads=tpd, writes=[U4])
                    for o_ in range(2):
                        S.i("act", "activation", rn[:, o_, :], ssum[o_][:, :], AF.Sqrt, bias=self.eps[:], reads=[ssum[o_], self.eps], writes=[rn])
                        S.i("dve", "reciprocal", rn[:, o_, :], rn[:, o_, :], reads=[rn], writes=[rn])
                        self.ps_free(ssum[o_])
                    fw = [S.sbuf([128, 2, NT, 128], BF16, "fw%d" % i, p2) for i in range(2)]
                    Ht = [S.sbuf([128, 2, 512], F32, "Ht%d" % i, p2) for i in range(2)]
                    hi = 0
                    self.load(fw[0], I["fwd_" + tag][0].rearrange("r p k m -> p r k m"), I["fwd_" + tag])
                    for f in range(NT):
                        fwt = fw[f % 2]
                        if f + 1 < NT:
                            self.load(fw[(f + 1) % 2], I["fwd_" + tag][f + 1].rearrange("r p k m -> p r k m"), I["fwd_" + tag])
                        for o_ in range(2):
                            H = Ht[hi % 2]
                            hi += 1
                            for ri in range(2):
                                pf = self.ps()
                                c_lo = (o_ if ri == 0 else 2 + o_) * 512
                                for kt in range(NT):
                                    S.mm(pf, pf[:, :], fwt, fwt[:, ri, kt, :], U4, U4[:, kt, c_lo:c_lo + 512], start=(kt == 0), stop=(kt == NT - 1))
                                S.i("dve", "tensor_tensor", H[:, ri, :], pf[:, :], rn[:, o_, :], ALU.mult, reads=[pf, rn], writes=[H])
                            if f == 0:
                                pn = self.ps()
                                for kt in range(NT):
                                    S.mm(pn, pn[:, :], fwt, fwt[:, 1, kt, :], U4, U4[:, kt, o_ * 512:(o_ + 1) * 512], start=(kt == 0), stop=(kt == NT - 1))
                                S.i("dve", "tensor_tensor", H[0:1, 1, :], pn[0:1, :], rn[0:1, o_, :], ALU.mult, reads=[pn, rn], writes=[H])
                            S.dma("sp", self.Hf_d[tag][o_, f], H[:], reads=[H], writes=[self.Hf_d[tag]], semtile=H)
                    S.barrier()
            for tag in ("l", "c"):
                self.dump("Hf_" + tag, self.Hf_d[tag], self.Hf_d[tag][:])
            S.barrier()

    def hyena(self, b, mixT, ph):
        S, I = self.S, self.I
        fbb = S.sbuf([128, 2, 512], F32, "fbb", ph)
        self.load(fbb, I["fbias"][:, :].partition_broadcast(128), I["fbias"])
        for tag, n, row0, col0 in (("c", NCTX, 0, C0), ("l", NLAT, NCTX, L0)):
            NT = n // 128
            with contextlib.ExitStack() as p2:
                u = S.sbuf([128, NT, 512], BF16, "hu", p2)
                z = S.sbuf([128, NT, 512], BF16, "hz", p2)
                Yf = S.sbuf([128, 2 * NT, 512], BF16, "Yf", p2)
                fw = [S.sbuf([128, 2, NT, 128], BF16, "cfw%d" % i, p2) for i in range(2)]
                iv = [S.sbuf([128, 2 * NT, 128], BF16, "civ%d" % i, p2) for i in range(2)]
                Hl = [S.sbuf([128, 2, 512], F32, "Hl%d" % i, p2) for i in range(2)]
                ta = S.sbuf([128, 512], F32, "ta", p2)
                tb = S.sbuf([128, 512], F32, "tb", p2)
                xg = [S.sbuf([128, 512], BF16, "xg%d" % i, p2) for i in range(2)]
                vg = [S.sbuf([128, 512], BF16, "vg%d" % i, p2) for i in range(2)]
                og = [S.sbuf([128, 512], BF16, "og%d" % i, p2) for i in range(2)]
                self.load(u, self.HY_d[2, row0:row0 + n, :].rearrange("(t p) c -> p t c", p=128), self.HY_d)
                for o_ in range(2):
                    src = u if o_ == 0 else z
                    def ld_f(f_):
                        self.load(fw[f_ % 2], I["fwd_" + tag][f_].rearrange("r p k m -> p r k m"), I["fwd_" + tag])
                        self.load(Hl[f_ % 2], self.Hf_d[tag][o_, f_], self.Hf_d[tag])

                    def ld_t(t_):
                        self.load(iv[t_ % 2], I["inv_" + tag][t_], I["inv_" + tag])
                        r0_ = row0 + t_ * 128
                        self.load(xg[t_ % 2], self.HY_d[o_, r0_:r0_ + 128, :], self.HY_d)

                    ld_f(0)
                    for f in range(NT):
                        fwt = fw[f % 2]
                        H = Hl[f % 2]
                        if f + 1 < NT:
                            ld_f(f + 1)
                        else:
                            ld_t(0)
                        pr = self.ps()
                        pi = self.ps()
                        for kt in range(NT):
                            S.mm(pr, pr[:, :], fwt, fwt[:, 0, kt, :], src, src[:, kt, :], start=(kt == 0), stop=(kt == NT - 1))
                        for kt in range(NT):
                            S.mm(pi, pi[:, :], fwt, fwt[:, 1, kt, :], src, src[:, kt, :], start=(kt == 0), stop=(kt == NT - 1))
                        S.i("dve", "tensor_tensor", ta[:], pr[:, :], H[:, 1, :], ALU.mult, reads=[pr, H], writes=[ta])
                        S.i("dve", "tensor_tensor", tb[:], pi[:, :], H[:, 0, :], ALU.mult, reads=[pi, H], writes=[tb])
                        S.i("dve", "tensor_tensor", Yf[:, NT + f, :], ta[:], tb[:], ALU.add, reads=[ta, tb], writes=[Yf])
                        S.i("dve", "tensor_tensor", ta[:], pr[:, :], H[:, 0, :], ALU.mult, reads=[pr, H], writes=[ta])
                        S.i("dve", "tensor_tensor", tb[:], pi[:, :], H[:, 1, :], ALU.mult, reads=[pi, H], writes=[tb])
                        S.i("dve", "tensor_tensor", Yf[:, f, :], ta[:], tb[:], ALU.subtract, reads=[ta, tb], writes=[Yf])
                        if f == 0:
                            S.i("dve", "tensor_copy", Yf[0:1, 0, :], ta[0:1, :], reads=[ta], writes=[Yf])
                            S.i("dve", "tensor_copy", Yf[0:1, NT, :], tb[0:1, :], reads=[tb], writes=[Yf])
                    for tt in range(NT):
                        ivt = iv[tt % 2]
                        xt_ = xg[tt % 2]
                        if tt + 1 < NT:
                            ld_t(tt + 1)
                        py = self.ps()
                        for jj in range(2 * NT):
                            S.mm(py, py[:, :], ivt, ivt[:, jj, :], Yf, Yf[:, jj, :], start=(jj == 0), stop=(jj == 2 * NT - 1))
                        S.i("pool", "tensor_tensor", ta[:], src[:, tt, :], fbb[:, o_, :], ALU.mult, reads=[src, fbb], writes=[ta])
                        S.i("dve", "tensor_tensor", tb[:], py[:, :], ta[:], ALU.add, reads=[py, ta], writes=[tb])
                        if o_ == 0:
                            S.i("dve", "tensor_tensor", z[:, tt, :], tb[:], xt_[:], ALU.mult, reads=[tb, xt_], writes=[z])
                        else:
                            ot = og[tt % 2]
                            S.i("dve", "tensor_tensor", ot[:], tb[:], xt_[:], ALU.mult, reads=[tb, xt_], writes=[ot])
                            for cc in range(4):
                                pt = self.ps()
                                ptb = pt[:, 0:64].bitcast(BF16)
                                S.tr(pt, ptb, ot, ot[:, cc * 128:(cc + 1) * 128], self.identb, self.identb[:])
                                S.i("act", "activation", mixT[cc][:, col0 + tt * 128:col0 + (tt + 1) * 128], ptb, AF.Copy, reads=[pt], writes=[mixT[cc]])
                S.barrier()


    def outproj_norm_router(self, b, layer, mixT, w_out, ph, with_ctx):
        S, I = self.S, self.I
        XTv = self.xt_view(b)
        XT = self.XT_d[b]
        wo = S.sbuf([128, 8, 1024], BF16, "wo", ph)
        self.load(wo, w_out[:, :].rearrange("(k p) n -> p k n", p=128), w_out, q="pool")
        wr = S.sbuf([128, 8, 36], F32, "wr", ph)
        self.load(wr, I["wr"][:, layer], I["wr"])
        self.mn_alloc(ph, nbuf=2)
        xb = [S.sbuf([128, 8, 512], F32, "xb%d" % i, ph) for i in range(2)]
        h32s = [S.sbuf([128, 8, 512], F32, "h32_%d" % i, ph) for i in range(2)]
        hbs = [S.sbuf([128, 8, 512], BF16, "hb%d" % i, ph) for i in range(2)]
        H2v = self.H2_d[:, :].rearrange("(k p) w -> p k w", p=128)
        L = S.sbuf([128, 36], F32, "rL", ph)
        sm = S.sbuf([128, 16], F32, "rsm", ph)
        mg = S.sbuf([128, 4], F32, "rmg", ph)
        eg = S.sbuf([128, 4], F32, "reg", ph)
        les = S.sbuf([128, 8], F32, "rles", ph)
        m8 = S.sbuf([128, 8], F32, "rm8", ph)
        mk1 = S.sbuf([128, 8], F32, "rmk1", ph)
        mk12 = S.sbuf([128, 8], F32, "rmk12", ph)
        inner = S.sbuf([128, 8], F32, "rinner", ph)
        gates = S.sbuf([128, 32], F32, "rgates", ph)
        gT = S.sbuf([32, 512], BF16, "rgT", ph)
        blks = BLKS if with_ctx else BLKS[1:]
        for bi, (s, n, kind) in enumerate(blks):
            ncol = 2 if kind == "c" else b
            xt = xb[bi % 2]
            self.load(xt, XTv[:, :, s:s + n], XT, dst_ap=xt[:, :, :n])
            for mo in range(8):
                ps = self.ps()
                for kc in range(8):
                    S.mm(ps, ps[:, :n], wo, wo[:, kc, mo * 128:(mo + 1) * 128], mixT[kc], mixT[kc][:, s:s + n], start=(kc == 0), stop=(kc == 7))
                S.i("dve", "scalar_tensor_tensor", xt[:, mo, :n], ps[:, :n], self.mv(layer, 2, mo, ncol), xt[:, mo, :n], ALU.mult, ALU.add, reads=[ps, self.MV, xt], writes=[xt])
            S.dma("sp", XTv[:, :, s:s + n], xt[:, :, :n], reads=[xt], writes=[XT], semtile=xt)
            hb = hbs[bi % 2]
            h32 = h32s[bi % 2]
            self.modnorm_block(xt, n, layer, 4, 3, ncol, [hb[:, c, :n] for c in range(8)], [hb] * 8, h32=h32)
            S.dma("sp", H2v[:, :, s:s + n], hb[:, :, :n], reads=[hb], writes=[self.H2_d], semtile=hb)
            for t0 in range(0, n, 128):
                pl = self.ps()
                for c in range(8):
                    S.mm(pl, pl[:, 0:36], h32, h32[:, c, t0:t0 + 128], wr, wr[:, c, :], start=(c == 0), stop=(c == 7))
                S.i("dve", "tensor_copy", L[:], pl[:, 0:36], reads=[pl], writes=[L])
                S.i("dve", "reduce_max", sm[:, 0:1], L[:, 0:4], AX.X, reads=[L], writes=[sm])
                S.i("dve", "tensor_single_scalar", sm[:, 1:2], sm[:, 0:1], -1.0, ALU.mult, reads=[sm], writes=[sm])
                S.i("dve", "tensor_single_scalar", mg[:], L[:, 0:4], sm[:, 0:1], ALU.is_ge, reads=[L, sm], writes=[mg])
                S.i("act", "activation", eg[:], L[:, 0:4], AF.Exp, bias=sm[:, 1:2], accum_out=sm[:, 2:3], reads=[L, sm], writes=[eg, sm])
                S.i("dve", "reciprocal", sm[:, 3:4], sm[:, 2:3], reads=[sm], writes=[sm])
                S.i("dve", "tensor_single_scalar", les[:], L[:, 4:12], mg[:, 0:1], ALU.mult, reads=[L, mg], writes=[les])
                for g in range(1, 4):
                    S.i("dve", "scalar_tensor_tensor", les[:], L[:, 4 + 8 * g:12 + 8 * g], mg[:, g:g + 1], les[:], ALU.mult, ALU.add, reads=[L, mg, les], writes=[les])
                S.i("dve", "max", m8[:], les[:], reads=[les], writes=[m8])
                S.i("dve", "tensor_single_scalar", mk1[:], les[:], m8[:, 0:1], ALU.is_ge, reads=[les, m8], writes=[mk1])
                S.i("dve", "tensor_single_scalar", mk12[:], les[:], m8[:, 1:2], ALU.is_ge, reads=[les, m8], writes=[mk12])
                S.i("dve", "tensor_single_scalar", sm[:, 4:5], m8[:, 0:1], -1.0, ALU.mult, reads=[m8], writes=[sm])
                S.i("act", "activation", sm[:, 5:6], m8[:, 1:2], AF.Exp, bias=sm[:, 4:5], reads=[m8, sm], writes=[sm])
                S.i("dve", "tensor_single_scalar", sm[:, 6:7], sm[:, 5:6], 1.0, ALU.add, reads=[sm], writes=[sm])
                S.i("dve", "reciprocal", sm[:, 7:8], sm[:, 6:7], reads=[sm], writes=[sm])
                S.i("dve", "tensor_tensor", sm[:, 8:9], sm[:, 5:6], sm[:, 7:8], ALU.mult, reads=[sm], writes=[sm])
                S.i("dve", "tensor_tensor", sm[:, 9:10], sm[:, 7:8], sm[:, 8:9], ALU.subtract, reads=[sm], writes=[sm])
                S.i("dve", "tensor_single_scalar", inner[:], mk12[:], sm[:, 8:9], ALU.mult, reads=[mk12, sm], writes=[inner])
                S.i("dve", "scalar_tensor_tensor", inner[:], mk1[:], sm[:, 9:10], inner[:], ALU.mult, ALU.add, reads=[mk1, sm, inner], writes=[inner])
                S.i("dve", "tensor_single_scalar", mg[:], mg[:], sm[:, 3:4], ALU.mult, reads=[mg, sm], writes=[mg])
                for g in range(4):
                    S.i("dve", "tensor_single_scalar", gates[:, 8 * g:8 * g + 8], inner[:], mg[:, g:g + 1], ALU.mult, reads=[inner, mg], writes=[gates])
                pt = self.ps()
                S.tr(pt, pt[0:32, 0:128], gates, gates[:], self.ident, self.ident[:])
                S.i("act", "activation", gT[:, t0:t0 + 128], pt[0:32, 0:128], AF.Copy, reads=[pt], writes=[gT])
            S.dma("sp", self.G_d[:, s:s + n], gT[:, :n], reads=[gT], writes=[self.G_d], semtile=gT)

    def moe(self, b, layer, ph, with_ctx, final=False):
        S, I = self.S, self.I
        XTv = self.xt_view(b)
        XT = self.XT_d[b]
        h2T = [S.sbuf([128, W], BF16, "h2T%d" % c, ph) for c in range(8)]
        for c in range(8):
            self.load(h2T[c], self.H2_d[c * 128:(c + 1) * 128, :], self.H2_d)
        xT = [S.sbuf([128, W], F32, "xT%d" % c, ph) for c in range(8)]
        for c in range(8):
            self.load(xT[c], XT[c * 128:(c + 1) * 128, :], XT)
        wgu = [S.sbuf([128, 2, 2, 8, 256], BF16, "wgu%d" % i, ph) for i in range(2)]
        wd = [S.sbuf([128, 2, 2, 1024], BF16, "wd%d" % i, ph) for i in range(2)]
        gbc = [S.sbuf([128, 2, W], BF16, "gbc%d" % i, ph) for i in range(2)]
        sg = [S.sbuf([128, 512], BF16, "sg%d" % i, ph) for i in range(2)]
        su = [S.sbuf([128, 512], BF16, "su%d" % i, ph) for i in range(2)]
        hid = [S.sbuf([128, 512], BF16, "hid%d" % i, ph) for i in range(8)]
        blks = BLKS if with_ctx else BLKS[1:]
        self._hcnt = 0

        def load_pair(ep):
            wg_, wd_, gb_ = wgu[ep % 2], wd[ep % 2], gbc[ep % 2]
            for e in range(2):
                E = 2 * ep + e
                self.load(wg_, I["moe_w_gate"][layer, E].rearrange("(k p) n -> p k n", p=128), I["moe_w_gate"], q="pool", dst_ap=wg_[:, e, 0])
                self.load(wg_, I["moe_w_up"][layer, E].rearrange("(k p) n -> p k n", p=128), I["moe_w_up"], q="pool", dst_ap=wg_[:, e, 1])
                self.load(wd_, I["moe_w_down"][layer, E].rearrange("(k p) n -> p k n", p=128), I["moe_w_down"], q="pool", dst_ap=wd_[:, e])
            self.load(gb_, self.G_d[2 * ep:2 * ep + 2, :].partition_broadcast(128), self.G_d)

        def stage_a(ep, s, n):
            wg_, gb_ = wgu[ep % 2], gbc[ep % 2]
            hs = []
            for e in range(2):
                for m in range(2):
                    pg = self.ps()
                    pu = self.ps()
                    for kc in range(8):
                        S.mm(pg, pg[:, :n], wg_, wg_[:, e, 0, kc, m * 128:(m + 1) * 128], h2T[kc], h2T[kc][:, s:s + n], start=(kc == 0), stop=(kc == 7))
                    for kc in range(8):
                        S.mm(pu, pu[:, :n], wg_, wg_[:, e, 1, kc, m * 128:(m + 1) * 128], h2T[kc], h2T[kc][:, s:s + n], start=(kc == 0), stop=(kc == 7))
                    a_, u_ = sg[self._hcnt % 2], su[self._hcnt % 2]
                    hd = hid[self._hcnt % 8]
                    self._hcnt += 1
                    S.i("act", "activation", a_[:, :n], pg[:, :n], AF.Silu, reads=[pg], writes=[a_])
                    S.i("act", "activation", u_[:, :n], pu[:, :n], AF.Copy, reads=[pu], writes=[u_])
                    S.i("dve", "tensor_tensor", a_[:, :n], a_[:, :n], u_[:, :n], ALU.mult, reads=[a_, u_], writes=[a_])
                    S.i("dve", "tensor_tensor", hd[:, :n], a_[:, :n], gb_[:, e, s:s + n], ALU.mult, reads=[a_, gb_], writes=[hd])
                    hs.append((hd, e, m))
            return hs

        def stage_b(ep, s, n, ncol, hs):
            wd_ = wd[ep % 2]
            for mo in range(8):
                py = self.ps()
                for ii, (hd, e, m) in enumerate(hs):
                    S.mm(py, py[:, :n], wd_, wd_[:, e, m, mo * 128:(mo + 1) * 128], hd, hd[:, :n], start=(ii == 0), stop=(ii == 3))
                S.i("dve", "scalar_tensor_tensor", xT[mo][:, s:s + n], py[:, :n], self.mv(layer, 5, mo, ncol), xT[mo][:, s:s + n], ALU.mult, ALU.add,
                    reads=[py, self.MV, xT[mo]], writes=[xT[mo]])

        load_pair(0)
        for ep in range(16):
            if ep + 1 < 16:
                load_pair(ep + 1)
            pend = None
            for (s, n, kind) in blks:
                ncol = 2 if kind == "c" else b
                hs = stage_a(ep, s, n)
                if pend is not None:
                    stage_b(*pend)
                pend = (ep, s, n, ncol, hs)
            stage_b(*pend)
        if not final:
            for c in range(8):
                S.dma("sp", XT[c * 128:(c + 1) * 128, :], xT[c][:], reads=[xT[c]], writes=[XT], semtile=xT[c])
            return
        fg = S.sbuf([128, 8], F32, "fg", ph)
        self.load(fg, I["finalgT"][:], I["finalgT"])
        sq_t, rs_t = wgu[0], gbc[0]
        sq = sq_t[:].rearrange("p a b c d -> p (a b c d)").bitcast(F32).rearrange("p (c n) -> p c n", n=512)
        rsv = rs_t[:].rearrange("p a w -> p (a w)").bitcast(F32)
        for bi in range(4):
            s = L0 + 512 * bi
            rs_ = rsv[:, 512 * (bi % 2):512 * (bi % 2 + 1)]
            for c in range(8):
                S.i("act", "activation", sq[:, c, :], xT[c][:, s:s + 512], AF.Square, reads=[xT[c]], writes=[sq_t])
            ss = self.ps()
            for c in range(8):
                S.mm(ss, ss[:, :], self.ones, self.ones[:], sq_t, sq[:, c, :], start=(c == 0), stop=(c == 7))
            S.i("act", "activation", rs_, ss[:, :], AF.Sqrt, bias=self.eps[:], scale=1.0 / D, reads=[ss, self.eps], writes=[rs_t])
            S.i("dve", "reciprocal", rs_, rs_, reads=[rs_t], writes=[rs_t])
            for c in range(8):
                S.i("dve", "scalar_tensor_tensor", xT[c][:, s:s + 512], xT[c][:, s:s + 512], fg[:, c:c + 1], rs_, ALU.mult, ALU.mult, reads=[xT[c], fg, rs_t], writes=[xT[c]])
        for c in range(8):
            S.dma("sp", self.out[b, c * 128:(c + 1) * 128, :], xT[c][:, L0:L0 + NLAT], reads=[xT[c]], writes=[self.out], semtile=xT[c])

    def layer1(self, b):
        S, I = self.S, self.I
        XTv = self.xt_view(b)
        XT = self.XT_d[b]
        with contextlib.ExitStack() as LY:
            with contextlib.ExitStack() as L1:
                hT = [S.sbuf([128, W], BF16, "hT%d" % c, L1) for c in range(8)]
                with contextlib.ExitStack() as ph:
                    self.mn_alloc(ph, nbuf=2)
                    xb = [S.sbuf([128, 8, 512], F32, "xb%d" % i, ph) for i in range(2)]
                    for bi, (s, n, kind) in enumerate(BLKS):
                        xt = xb[bi % 2]
                        self.load(xt, XTv[:, :, s:s + n], XT, dst_ap=xt[:, :, :n])
                        self.modnorm_block(xt, n, 1, 1, 0, 2 if kind == "c" else b, [hT[c][:, s:s + n] for c in range(8)], hT)
                    S.barrier()
                mixT = hT
                with contextlib.ExitStack() as L2:
                    uS = [S.sbuf([128, 4, NCTX + NLAT], BF16, "uS%d" % d, L2) for d in range(2)]
                    with contextlib.ExitStack() as L3:
                        cqn = S.sbuf([128, 2, W], BF16, "cqn", L3)
                        ckvn = S.sbuf([128, W], BF16, "ckvn", L3)
                        KR = S.sbuf([128, W], BF16, "KR", L3)
                        with contextlib.ExitStack() as ph:
                            self.inproj_odd(b, hT, cqn, ckvn, KR, uS, ph)
                            S.barrier()
                        with contextlib.ExitStack() as ph:
                            self.mla(b, cqn, ckvn, KR, mixT, ph)
                            S.barrier()
                    with contextlib.ExitStack() as ph:
                        self.s5(b, uS, mixT, ph)
                        S.barrier()
                for c in range(8):
                    self.dump("mixT", mixT[c], mixT[c][:], lambda d, c=c: d[c])
                if self.stop == "mix1":
                    return
                with contextlib.ExitStack() as ph:
                    self.outproj_norm_router(b, 1, mixT, I["o_w_out"], ph, with_ctx=False)
                    S.barrier()
                if self.stop == "xa1":
                    self.dump("XT", XT, XT[:])
                    return
            with contextlib.ExitStack() as ph:
                self.moe(b, 1, ph, with_ctx=False, final=True)
                S.barrier()

    def final_norm(self, b, ph):
        S, I = self.S, self.I
        XTv = self.xt_view(b)
        XT = self.XT_d[b]
        fg = S.sbuf([128, 8], F32, "fg", ph)
        self.load(fg, I["finalgT"][:], I["finalgT"])
        xb = [S.sbuf([128, 8, 512], F32, "fxb%d" % i, ph) for i in range(2)]
        sq = S.sbuf([128, 8, 512], F32, "fsq", ph)
        rstd = S.sbuf([128, 512], F32, "frstd", ph)
        ob = [S.sbuf([128, 8, 512], F32, "fob%d" % i, ph) for i in range(2)]
        outv = self.out[b].rearrange("(k p) t -> p k t", p=128)
        for bi in range(4):
            s = L0 + 512 * bi
            xt = xb[bi % 2]
            o = ob[bi % 2]
            self.load(xt, XTv[:, :, s:s + 512], XT)
            S.i("act", "activation", sq[:], xt[:], AF.Square, reads=[xt], writes=[sq])
            ss = self.ps()
            for c in range(8):
                S.mm(ss, ss[:, :], self.ones, self.ones[:], sq, sq[:, c, :], start=(c == 0), stop=(c == 7))
            S.i("act", "activation", rstd[:], ss[:, :], AF.Sqrt, bias=self.eps[:], scale=1.0 / D, reads=[ss, self.eps], writes=[rstd])
            S.i("dve", "reciprocal", rstd[:], rstd[:], reads=[rstd], writes=[rstd])
            for c in range(8):
                S.i("dve", "scalar_tensor_tensor", o[:, c, :], xt[:, c, :], fg[:, c:c + 1], rstd[:], ALU.mult, ALU.mult, reads=[xt, fg, rstd], writes=[o])
            S.dma("sp", outv[:, :, 512 * bi:512 * (bi + 1)], o[:], reads=[o], writes=[self.out], semtile=o)

    def inproj_odd(self, b, hT, cqn, ckvn, KR, uS, ph):
        S, I = self.S, self.I
        wsrc = I["o_w_in"][:, :].rearrange("(k p) n -> p k n", p=128)
        wi = S.sbuf([128, 8, 928], BF16, "wi", ph)
        self.load(wi, wsrc, I["o_w_in"], q="pool")
        wkr = S.sbuf([128, 8, 128], BF16, "wkr", ph)
        S.i("pool", "memset", wkr[:], 0.0, writes=[wkr])
        S.i("dve", "tensor_copy", wkr[:, :, 64:96], wi[:, :, 384:416], reads=[wi], writes=[wkr])
        ng = S.sbuf([128, 3], F32, "ong", ph)
        self.load(ng, I["o_ng"][:], I["o_ng"])
        cos = S.sbuf([128, NLAT], F32, "cos", ph)
        sin = S.sbuf([128, NLAT], F32, "sin", ph)
        self.load(cos, I["cos_o"][:], I["cos_o"])
        self.load(sin, I["sin_o"][:], I["sin_o"])
        rotT = S.sbuf([128, 128], BF16, "rotT", ph)
        self.load(rotT, I["rotT_o"][:], I["rotT_o"])
        sq = S.sbuf([128, 2, 512], F32, "osq", ph)
        rs = S.sbuf([128, 512], F32, "ors", ph)
        krn = S.sbuf([128, 512], BF16, "krn", ph)
        t1 = S.sbuf([128, 512], F32, "ot1", ph)
        t2 = S.sbuf([128, 512], F32, "ot2", ph)
        KD = os.environ.get("KDBG", "ABCD")
        for (s, n, kind) in BLKS:
          if "A" in KD:
            pq = [self.ps(), self.ps()]
            for j in range(2):
                for kc in range(8):
                    S.mm(pq[j], pq[j][:, :n], wi, wi[:, kc, j * 128:(j + 1) * 128], hT[kc], hT[kc][:, s:s + n], start=(kc == 0), stop=(kc == 7))
                S.i("act", "activation", sq[:, j, :n], pq[j][:, :n], AF.Square, reads=[pq[j]], writes=[sq])
            ss = self.ps()
            for j in range(2):
                S.mm(ss, ss[:, :n], self.ones, self.ones[:], sq, sq[:, j, :n], start=(j == 0), stop=(j == 1))
            S.i("act", "activation", rs[:, :n], ss[:, :n], AF.Sqrt, bias=self.eps[:], scale=1.0 / 256, reads=[ss, self.eps], writes=[rs])
            S.i("dve", "reciprocal", rs[:, :n], rs[:, :n], reads=[rs], writes=[rs])
            for j in range(2):
                S.i("dve", "scalar_tensor_tensor", cqn[:, j, s:s + n], pq[j][:, :n], ng[:, j:j + 1], rs[:, :n], ALU.mult, ALU.mult, reads=[pq[j], ng, rs], writes=[cqn])
          if "B" in KD:
            pk = self.ps()
            for kc in range(8):
                S.mm(pk, pk[:, :n], wi, wi[:, kc, 256:384], hT[kc], hT[kc][:, s:s + n], start=(kc == 0), stop=(kc == 7))
            S.i("act", "activation", sq[:, 0, :n], pk[:, :n], AF.Square, reads=[pk], writes=[sq])
            ss = self.ps()
            S.mm(ss, ss[:, :n], self.ones, self.ones[:], sq, sq[:, 0, :n])
            S.i("act", "activation", rs[:, :n], ss[:, :n], AF.Sqrt, bias=self.eps[:], scale=1.0 / 128, reads=[ss, self.eps], writes=[rs])
            S.i("dve", "reciprocal", rs[:, :n], rs[:, :n], reads=[rs], writes=[rs])
            S.i("dve", "scalar_tensor_tensor", ckvn[:, s:s + n], pk[:, :n], ng[:, 2:3], rs[:, :n], ALU.mult, ALU.mult, reads=[pk, ng, rs], writes=[ckvn])
          if "C" in KD:
            pr = self.ps()
            for kc in range(8):
                S.mm(pr, pr[:, :n], wkr, wkr[:, kc, :], hT[kc], hT[kc][:, s:s + n], start=(kc == 0), stop=(kc == 7))
            if kind == "c":
                S.i("act", "activation", KR[:, s:s + n], pr[:, :n], AF.Copy, reads=[pr], writes=[KR])
            else:
                tc0 = s - L0
                S.i("act", "activation", krn[:, :n], pr[:, :n], AF.Copy, reads=[pr], writes=[krn])
                p3 = self.ps()
                S.mm(p3, p3[:, :n], rotT, rotT[:], krn, krn[:, :n])
                S.i("pool", "tensor_tensor", t1[:, :n], krn[:, :n], cos[:, tc0:tc0 + n], ALU.mult, reads=[krn, cos], writes=[t1])
                S.i("dve", "tensor_tensor", t2[:, :n], p3[:, :n], sin[:, tc0:tc0 + n], ALU.mult, reads=[p3, sin], writes=[t2])
                S.i("dve", "tensor_tensor", KR[:, s:s + n], t1[:, :n], t2[:, :n], ALU.add, reads=[t1, t2], writes=[KR])
          if "D" in KD:
            for j in range(4):
                pu = self.ps()
                for kc in range(8):
                    S.mm(pu, pu[:, :n], wi, wi[:, kc, 416 + j * 128:416 + (j + 1) * 128], hT[kc], hT[kc][:, s:s + n], start=(kc == 0), stop=(kc == 7))
                if kind == "c":
                    d0, d1 = 0, NLAT
                else:
                    d0, d1 = NCTX + (s - L0), s - L0
                S.i("act", "activation", uS[0][:, j, d0:d0 + n], pu[:, :n], AF.Copy, reads=[pu], writes=[uS[0]])
                S.i("pool", "tensor_copy", uS[1][:, j, d1:d1 + n], uS[0][:, j, d0:d0 + n], reads=[uS[0]], writes=[uS[1]])

    def s5_tables(self):
        S, I = self.S, self.I
        T_ = NCTX + NLAT
        self.ST_d = S.dram("ST_d", [32, 128, 4, T_], BF16)
        self.RMAG = S.sbuf([128, 32], F32, "s5rmag")
        with contextlib.ExitStack() as ph:
            prm = S.sbuf([128, 3, 32], F32, "s5prm", ph)
            self.load(prm, I["s5p"][:], I["s5p"])
            W_ = S.sbuf([128, 24, 32], F32, "s5w", ph)
            ki = S.sbuf([128, 32], mybir.dt.int32, "s5ki", ph)
            CO = S.sbuf([128, 4, 32], F32, "s5co", ph)
            PH = S.sbuf([128, 2, 32], F32, "s5ph", ph)
            a_re, a_im, ldt = prm[:, 0, :], prm[:, 1, :], prm[:, 2, :]
            allt = [W_, prm, CO, PH, self.RMAG]

            def w(i):
                return W_[:, i, :]

            def tt(o, x, y, op):
                S.i("dve", "tensor_tensor", o, x, y, op, reads=allt, writes=allt)

            def ts(o, x, s1, s2, o1, o2):
                S.i("dve", "tensor_scalar", o, x, s1, s2, o1, o2, reads=allt, writes=allt)

            def fracfix(o, y):
                S.i("dve", "tensor_copy", ki[:], y, reads=allt, writes=[ki])
                S.i("dve", "tensor_copy", w(20), ki[:], reads=[ki], writes=allt)
                tt(o, y, w(20), ALU.subtract)
                S.i("dve", "tensor_single_scalar", w(20), o, 0.5, ALU.is_gt, reads=allt, writes=allt)
                tt(o, o, w(20), ALU.subtract)

            S.i("act", "activation", w(0), ldt, AF.Exp, reads=allt, writes=allt)
            tt(w(1), a_re, w(0), ALU.mult)
            S.i("act", "activation", self.RMAG[:], w(1), AF.Exp, reads=allt, writes=allt)
            tt(w(3), a_im, w(0), ALU.mult)
            ts(PH[:, 0, :], w(3), 1.0 / TWO_PI, 1.0, ALU.mult, ALU.mult)
            ts(w(4), PH[:, 0, :], 1.0, 16.0, ALU.mult, ALU.add)
            ts(w(5), PH[:, 0, :], 1.0, 16.25, ALU.mult, ALU.add)
            fracfix(w(21), w(4))
            S.i("act", "activation", w(6), w(21), AF.Sin, scale=TWO_PI, reads=allt, writes=allt)
            fracfix(w(21), w(5))
            S.i("act", "activation", w(7), w(21), AF.Sin, scale=TWO_PI, reads=allt, writes=allt)
            ts(w(22), PH[:, 0, :], 64.0, 16.0, ALU.mult, ALU.add)
            fracfix(PH[:, 1, :], w(22))
            tt(w(12), self.RMAG[:], w(7), ALU.mult)
            tt(w(13), self.RMAG[:], w(6), ALU.mult)
            ts(w(8), w(12), -1.0, 1.0, ALU.add, ALU.mult)
            tt(w(9), a_re, a_re, ALU.mult)
            tt(w(10), a_im, a_im, ALU.mult)
            tt(w(9), w(9), w(10), ALU.add)
            S.i("dve", "reciprocal", w(9), w(9), reads=allt, writes=allt)
            tt(w(10), w(8), a_re, ALU.mult)
            tt(w(11), w(13), a_im, ALU.mult)
            tt(w(10), w(10), w(11), ALU.add)
            tt(CO[:, 0, :], w(10), w(9), ALU.mult)
            tt(w(10), w(13), a_re, ALU.mult)
            tt(w(11), w(8), a_im, ALU.mult)
            tt(w(10), w(10), w(11), ALU.subtract)
            tt(CO[:, 1, :], w(10), w(9), ALU.mult)
            ts(CO[:, 2, :], CO[:, 0, :], -1.0, 1.0, ALU.mult, ALU.mult)
            ts(CO[:, 3, :], CO[:, 1, :], -1.0, 1.0, ALU.mult, ALU.mult)
            k1s = S.sbuf([128, 32, 36], F32, "s5k1s", ph)
            k0s = S.sbuf([128, 32, 64], F32, "s5k0s", ph)
            self.load(k1s, I["s5k1s"][:], I["s5k1s"])
            self.load(k0s, I["s5k0s"][:], I["s5k0s"])
            TA = S.sbuf([128, 6, 32, 36], F32, "s5TA", ph)
            TB = S.sbuf([128, 3, 32, 64], F32, "s5TB", ph)
            ya = S.sbuf([128, 32, 36], F32, "s5ya", ph)
            yb_ = S.sbuf([128, 32, 64], F32, "s5yb", ph)
            kia = S.sbuf([128, 32, 36], mybir.dt.int32, "s5kia", ph)
            kib = S.sbuf([128, 32, 64], mybir.dt.int32, "s5kib", ph)
            fa = S.sbuf([128, 32, 36], F32, "s5fa", ph)
            fb_ = S.sbuf([128, 32, 64], F32, "s5fb", ph)
            small = [TA, TB, ya, yb_, fa, fb_, PH, CO]

            def sincos(yt, kit, ft_, ktab, phi_idx, n_, dst_c, dst_s):
                for col in range(32):
                    S.i("dve", "tensor_scalar", yt[:, col, :], ktab[:, col, :], PH[:, phi_idx, col:col + 1], 16.0, ALU.mult, ALU.add, reads=[ktab] + small, writes=small)
                for (off, dst) in ((0.0, dst_s), (0.25, dst_c)):
                    if off:
                        S.i("dve", "tensor_single_scalar", yt[:], yt[:], off, ALU.add, reads=small, writes=small)
                    S.i("dve", "tensor_copy", kit[:], yt[:], reads=small, writes=[kit])
                    S.i("dve", "tensor_copy", ft_[:], kit[:], reads=[kit], writes=small)
                    S.i("dve", "tensor_tensor", ft_[:], yt[:], ft_[:], ALU.subtract, reads=small, writes=small)
                    S.i("dve", "tensor_single_scalar", yt[:], ft_[:], 0.5, ALU.is_gt, reads=small, writes=small)
                    S.i("dve", "tensor_tensor", ft_[:], ft_[:], yt[:], ALU.subtract, reads=small, writes=small)
                    S.i("act", "activation", dst, ft_[:], AF.Sin, scale=TWO_PI, reads=small, writes=small)
                    S.i("dve", "tensor_copy", yt[:], kit[:], reads=[kit], writes=small)
                    S.i("dve", "tensor_tensor", yt[:], yt[:], ft_[:], ALU.add, reads=small, writes=small)

            sincos(ya, kia, fa, k1s, 1, 36, TA[:, 0], TA[:, 1])
            sincos(yb_, kib, fb_, k0s, 0, 64, TB[:, 0], TB[:, 1])
            for col in range(32):
                d = col // 16
                sg = -1.0 if d == 0 else 1.0
                c1 = slice(col, col + 1)
                cA, sA = TA[:, 0, col, :], TA[:, 1, col, :]
                S.i("dve", "tensor_single_scalar", TA[:, 2, col, :], cA, CO[:, 0, c1], ALU.mult, reads=small, writes=small)
                S.i("dve", "scalar_tensor_tensor", TA[:, 2, col, :], sA, CO[:, 3 if sg > 0 else 1, c1], TA[:, 2, col, :], ALU.mult, ALU.add, reads=small, writes=small)
                S.i("dve", "tensor_single_scalar", TA[:, 3, col, :], cA, CO[:, 1, c1], ALU.mult, reads=small, writes=small)
                S.i("dve", "scalar_tensor_tensor", TA[:, 3, col, :], sA, CO[:, 0 if sg > 0 else 2, c1], TA[:, 3, col, :], ALU.mult, ALU.add, reads=small, writes=small)
                S.i("dve", "tensor_single_scalar", TA[:, 4, col, :], cA, -sg, ALU.mult, reads=small, writes=small)
                S.i("dve", "tensor_single_scalar", TA[:, 5, col, :], sA, -sg, ALU.mult, reads=small, writes=small)
                S.i("dve", "tensor_single_scalar", TB[:, 2, col, :], TB[:, 1, col, :], sg, ALU.mult, reads=small, writes=small)
            t1 = S.sbuf([128, 36, 64], F32, "s5t1", ph)
            t2 = S.sbuf([128, 36, 64], F32, "s5t2", ph)
            tabs = [S.sbuf([128, 4, T_], BF16, "s5tab%d" % i, ph) for i in range(2)]

            def oa(i, col):
                return TA[:, i, col, :].unsqueeze(2).to_broadcast([128, 36, 64])

            def ob(i, col):
                return TB[:, i, col, :].unsqueeze(1).to_broadcast([128, 36, 64])

            for col in range(32):
                tab = tabs[col % 2]

                def tv(j):
                    return tab[:, j, :].rearrange("p (a b) -> p a b", b=64)

                for (j, (x1, y1, x2, y2, op)) in enumerate(((2, 0, 3, 2, ALU.subtract), (3, 0, 2, 2, ALU.add), (0, 0, 1, 1, ALU.subtract), (5, 0, 4, 1, ALU.add))):
                    S.i("dve", "tensor_tensor", t1[:], oa(x1, col), ob(y1, col), ALU.mult, reads=small, writes=[t1])
                    S.i("dve", "tensor_tensor", t2[:], oa(x2, col), ob(y2, col), ALU.mult, reads=small, writes=[t2])
                    S.i("dve", "tensor_tensor", tv(j), t1[:], t2[:], op, reads=[t1, t2], writes=[tab])
                S.dma("sp", self.ST_d[col], tab[:], reads=[tab], writes=[self.ST_d], semtile=tab)
            S.barrier()

    def s5(self, b, uS, mixT, ph):
        S, I = self.S, self.I
        T_ = NCTX + NLAT
        dsk = S.sbuf([128, 4], F32, "dsk", ph)
        self.load(dsk, I["dskT"][:], I["dskT"])
        gw = S.sbuf([128, 4, 512], BF16, "gw", ph)
        self.load(gw, I["glu_w"][:, :].rearrange("(k p) n -> p k n", p=128), I["glu_w"], q="pool")
        gb = S.sbuf([128, 4], F32, "gb", ph)
        self.load(gb, I["glu_bT"][:], I["glu_bT"])
        diagD = S.sbuf([128, 128], BF16, "diagD", ph)
        gT = [S.sbuf([128, NLAT], BF16, "gT%d" % i, ph) for i in range(4)]
        B = [S.sbuf([128, T_], F32, "s5B%d" % i, ph) for i in range(6)]
        Mb = S.sbuf([128, 4, NLAT], BF16, "s5M", ph)
        tabs = [S.sbuf([128, 4, T_], BF16, "s5tb%d" % i, ph) for i in range(2)]
        BDt = [S.sbuf([128, 2, 128], BF16, "BDt%d" % i, ph) for i in range(2)]
        CDt = [S.sbuf([128, 3, 128], BF16, "CDt%d" % i, ph) for i in range(2)]
        cblks = [(c0, min(512, T_ - c0)) for c0 in range(0, T_, 512)]
        it = 0
        order5 = [(d_, st_) for ft_ in range(4) for d_ in range(2) for st_ in range(4 * ft_, 4 * ft_ + 4)]

        def ld5(i_):
            d_, st_ = order5[i_]
            bd_, cd_, tab_ = BDt[i_ % 2], CDt[i_ % 2], tabs[i_ % 2]
            self.load(tab_, self.ST_d[d_ * 16 + st_], self.ST_d)
            for ri in range(2):
                self.load(bd_, I["s5BD"][ri, d_, st_], I["s5BD"], q="pool", dst_ap=bd_[:, ri, :])
                self.load(cd_, I["s5CD"][ri, d_, st_], I["s5CD"], q="pool", dst_ap=cd_[:, ri, :])
            S.i("pool", "tensor_single_scalar", cd_[:, 1, :], cd_[:, 1, :], -1.0, ALU.mult, reads=[cd_], writes=[cd_])
            S.i("pool", "tensor_single_scalar", cd_[:, 2, :], cd_[:, 0, :], -1.0, ALU.mult, reads=[cd_], writes=[cd_])

        def emit_bu(i_):
            d_, st_ = order5[i_]
            bd_ = BDt[i_ % 2]
            ft_ = st_ // 4
            for (c0, n) in cblks:
                pr = self.ps()
                pi = self.ps()
                S.mm(pr, pr[:, :n], bd_, bd_[:, 0, :], uS[d_], uS[d_][:, ft_, c0:c0 + n])
                S.mm(pi, pi[:, :n], bd_, bd_[:, 1, :], uS[d_], uS[d_][:, ft_, c0:c0 + n])
                S.i("act", "activation", B[0][:, c0:c0 + n], pr[:, :n], AF.Copy, reads=[pr], writes=[B[0]])
                S.i("act", "activation", B[1][:, c0:c0 + n], pi[:, :n], AF.Copy, reads=[pi], writes=[B[1]])

        for ft in range(4):
            yb = [self.ps(reserve=True) for _ in range(4)]
            S.i("dve", "tensor_single_scalar", diagD[:], self.ident[:], dsk[:, ft:ft + 1], ALU.mult, reads=[self.ident, dsk], writes=[diagD])
            for q4 in range(4):
                S.mm(yb[q4], yb[q4][:, :], diagD, diagD[:], uS[0], uS[0][:, ft, NCTX + 512 * q4:NCTX + 512 * (q4 + 1)], start=True, stop=False)
            for d in range(2):
                for st in range(4 * ft, 4 * ft + 4):
                    col = d * 16 + st
                    bd, cd, tab = BDt[it % 2], CDt[it % 2], tabs[it % 2]
                    if it == 0:
                        ld5(0)
                    if it + 1 < len(order5):
                        ld5(it + 1)
                    if it == 0:
                        emit_bu(0)
                    S.i("dve", "tensor_tensor", B[2][:], B[0][:], tab[:, 0, :], ALU.mult, reads=[B[0], tab], writes=[B[2]])
                    S.i("dve", "tensor_tensor", B[3][:], B[1][:], tab[:, 1, :], ALU.mult, reads=[B[1], tab], writes=[B[3]])
                    S.i("dve", "tensor_tensor", B[4][:], B[1][:], tab[:, 0, :], ALU.mult, reads=[B[1], tab], writes=[B[4]])
                    S.i("dve", "tensor_tensor", B[5][:], B[0][:], tab[:, 1, :], ALU.mult, reads=[B[0], tab], writes=[B[5]])
                    S.i("dve", "tensor_tensor", B[2][:], B[2][:], B[3][:], ALU.subtract, reads=[B[2], B[3]], writes=[B[2]])
                    S.i("dve", "tensor_tensor", B[4][:], B[4][:], B[5][:], ALU.add, reads=[B[4], B[5]], writes=[B[4]])
                    if it + 1 < len(order5):
                        emit_bu(it + 1)
                    it += 1
                    rm = self.RMAG[:, col:col + 1].to_broadcast([128, T_])
                    if d == 0:
                        S.i("dve", "tensor_tensor_scan", B[3][:], rm, B[2][:], 0.0, ALU.mult, ALU.add, reads=[B[2], self.RMAG], writes=[B[3]])
                        S.i("dve", "tensor_tensor_scan", B[5][:], rm, B[4][:], 0.0, ALU.mult, ALU.add, reads=[B[4], self.RMAG], writes=[B[5]])
                    else:
                        S.i("dve", "tensor_tensor_scan", B[3][:, ::-1], rm, B[2][:, ::-1], 0.0, ALU.mult, ALU.add, reads=[B[2], self.RMAG], writes=[B[3]])
                        S.i("dve", "tensor_tensor_scan", B[5][:, ::-1], rm, B[4][:, ::-1], 0.0, ALU.mult, ALU.add, reads=[B[4], self.RMAG], writes=[B[5]])
                    l0 = NCTX if d == 0 else 0
                    ls = slice(l0, l0 + NLAT)
                    S.i("dve", "tensor_tensor", Mb[:, 0, :], B[3][:, ls], tab[:, 2, ls], ALU.mult, reads=[B[3], tab], writes=[Mb])
                    S.i("dve", "tensor_tensor", Mb[:, 1, :], B[5][:, ls], tab[:, 3, ls], ALU.mult, reads=[B[5], tab], writes=[Mb])
                    S.i("dve", "tensor_tensor", Mb[:, 2, :], B[5][:, ls], tab[:, 2, ls], ALU.mult, reads=[B[5], tab], writes=[Mb])
                    S.i("dve", "tensor_tensor", Mb[:, 3, :], B[3][:, ls], tab[:, 3, ls], ALU.mult, reads=[B[3], tab], writes=[Mb])
                    last = (d == 1 and st == 4 * ft + 3)
                    for q4 in range(4):
                        cs_ = slice(512 * q4, 512 * (q4 + 1))
                        S.mm(yb[q4], yb[q4][:, :], cd, cd[:, 0, :], Mb, Mb[:, 0, cs_], start=False, stop=False)
                        S.mm(yb[q4], yb[q4][:, :], cd, cd[:, 2, :], Mb, Mb[:, 1, cs_], start=False, stop=False)
                        S.mm(yb[q4], yb[q4][:, :], cd, cd[:, 1, :], Mb, Mb[:, 2, cs_], start=False, stop=False)
                        S.mm(yb[q4], yb[q4][:, :], cd, cd[:, 1, :], Mb, Mb[:, 3, cs_], start=False, stop=last)
            for q4 in range(4):
                xs_, x2_ = B[2][:, 512 * q4:512 * (q4 + 1)], B[4][:, 512 * q4:512 * (q4 + 1)]
                S.i("act", "activation", xs_, yb[q4][:, :], AF.Copy, reads=[yb[q4]], writes=[B[2]])
                S.i("dve", "tensor_tensor", x2_, xs_, xs_, ALU.mult, reads=[B[2]], writes=[B[4]])
                S.i("dve", "tensor_scalar", x2_, x2_, 0.044715, 1.0, ALU.mult, ALU.add, reads=[B[4]], writes=[B[4]])
                S.i("dve", "tensor_tensor", x2_, x2_, xs_, ALU.mult, reads=[B[2], B[4]], writes=[B[4]])
                S.i("act", "activation", x2_, x2_, AF.Tanh, scale=math.sqrt(2.0 / math.pi), reads=[B[4]], writes=[B[4]])
                S.i("dve", "tensor_scalar", x2_, x2_, 1.0, 0.5, ALU.add, ALU.mult, reads=[B[4]], writes=[B[4]])
                S.i("dve", "tensor_tensor", gT[ft][:, 512 * q4:512 * (q4 + 1)], x2_, xs_, ALU.mult, reads=[B[2], B[4]], writes=[gT[ft]])
                self.ps_free(yb[q4])
        sgm = [S.sbuf([128, 512], BF16, "sgm%d" % i, ph) for i in range(2)]
        for f2 in range(4):
            for q4 in range(4):
                ps = self.ps()
                for ft in range(4):
                    S.mm(ps, ps[:, :], gw, gw[:, ft, f2 * 128:(f2 + 1) * 128], gT[ft], gT[ft][:, 512 * q4:512 * (q4 + 1)], start=(ft == 0), stop=(ft == 3))
                sg_ = sgm[(f2 * 4 + q4) % 2]
                S.i("act", "activation", sg_[:], ps[:, :], AF.Sigmoid, bias=gb[:, f2:f2 + 1], reads=[ps, gb], writes=[sg_])
                S.i("dve", "tensor_tensor", mixT[4 + f2][:, L0 + 512 * q4:L0 + 512 * (q4 + 1)], sg_[:], gT[f2][:, 512 * q4:512 * (q4 + 1)], ALU.mult, reads=[sg_, gT[f2]], writes=[mixT[4 + f2]])

    def mla(self, b, cqn, ckvn, KR, mixT, ph):
        S, I = self.S, self.I
        wuq = S.sbuf([128, 2, 768], BF16, "wuq", ph)
        self.load(wuq, I["o_w_uq"][:, :].rearrange("(k p) n -> p k n", p=128), I["o_w_uq"], q="pool")
        wukv = S.sbuf([128, 1024], BF16, "wukv", ph)
        self.load(wukv, I["o_w_ukv"][:, :], I["o_w_ukv"], q="pool")
        cos = S.sbuf([128, NLAT], F32, "cos", ph)
        sin = S.sbuf([128, NLAT], F32, "sin", ph)
        self.load(cos, I["cos_o"][:], I["cos_o"])
        self.load(sin, I["sin_o"][:], I["sin_o"])
        rotT = S.sbuf([128, 128], BF16, "rotT", ph)
        self.load(rotT, I["rotT_o"][:], I["rotT_o"])
        VA = S.sbuf([128, 18, 8, 128], BF16, "VA1", ph)
        S.i("pool", "memset", VA[:, :, :, 64:128], 1.0, writes=[VA])
        wv = wukv[:, :].rearrange("k (h t d) -> k h t d", h=8, t=2)
        for ti, (s, kind, _) in enumerate(TTILES):
            pv = self.ps()
            S.mm(pv, pv[:, :].rearrange("p (h d) -> p h d", h=8), ckvn, ckvn[:, s:s + 128], wukv, wv[:, :, 1, :])
            S.i("act", "activation", VA[:, ti, :, 0:64], pv[:, :].rearrange("p (h d) -> p h d", h=8), AF.Copy, reads=[pv], writes=[VA])
        Qh = [S.sbuf([128, NLAT], BF16, "Qh%d" % i, ph) for i in range(2)]
        Kh = [S.sbuf([128, W], BF16, "Kh%d" % i, ph) for i in range(2)]
        qn = S.sbuf([128, 512], BF16, "mqn", ph)
        t1 = S.sbuf([128, 512], F32, "mt1", ph)
        t2 = S.sbuf([128, 512], F32, "mt2", ph)
        T = {"pT": [S.sbuf([128, 512], BF16, "pT%d" % i, ph) for i in range(6)], "den": [S.sbuf([64, 512], F32, "den%d" % i, ph) for i in range(2)]}

        def kcol(kt):
            return (C0 + 128 * kt) if kt < 2 else (L0 + 128 * (kt - 2))

        for hp in range(4):
          for h in (2 * hp, 2 * hp + 1):
            Q, K = Qh[h % 2], Kh[h % 2]
            for (s, n, kind) in BLKS:
                pk = self.ps()
                S.mm(pk, pk[0:64, :n], wukv, wukv[:, h * 128:h * 128 + 64], ckvn, ckvn[:, s:s + n])
                S.i("act", "activation", K[0:64, s:s + n], pk[0:64, :n], AF.Copy, reads=[pk], writes=[K])
                S.i("pool", "tensor_copy", K[64:96, s:s + n], KR[64:96, s:s + n], reads=[KR], writes=[K])
            for qb in range(4):
                s = L0 + 512 * qb
                pq = self.ps()
                for j in range(2):
                    S.mm(pq, pq[0:96, :], wuq, wuq[:, j, h * 96:(h + 1) * 96], cqn, cqn[:, j, s:s + 512], start=(j == 0), stop=(j == 1))
                S.i("act", "activation", qn[0:96, :], pq[0:96, :], AF.Copy, reads=[pq], writes=[qn])
                p3 = self.ps()
                S.mm(p3, p3[0:96, :], rotT, rotT[0:96, 0:96], qn, qn[0:96, :])
                S.i("pool", "tensor_copy", Q[0:64, 512 * qb:512 * (qb + 1)], qn[0:64, :], reads=[qn], writes=[Q])
                S.i("pool", "tensor_tensor", t1[64:96, :], qn[64:96, :], cos[64:96, 512 * qb:512 * (qb + 1)], ALU.mult, reads=[qn, cos], writes=[t1])
                S.i("dve", "tensor_tensor", t2[64:96, :], p3[64:96, :], sin[64:96, 512 * qb:512 * (qb + 1)], ALU.mult, reads=[p3, sin], writes=[t2])
                S.i("dve", "tensor_tensor", Q[64:96, 512 * qb:512 * (qb + 1)], t1[64:96, :], t2[64:96, :], ALU.add, reads=[t1, t2], writes=[Q])
          out_t = mixT[hp]
          for qb in range(4):
            s = L0 + 512 * qb
            streams = []
            for h in (2 * hp, 2 * hp + 1):
                Q, K = Qh[h % 2], Kh[h % 2]
                po = (h % 2) * 64
                streams.append({"QTt": Q, "Qap": Q[0:96, 512 * qb:512 * (qb + 1)], "KTt": K,
                                "Kap": (lambda kt, K=K: K[0:96, kcol(kt):kcol(kt) + 128]),
                                "Vt": VA, "Vap": (lambda kt, h=h: VA[:, kt, h, :]), "out_t": out_t, "out_ap": out_t[po:po + 64, s:s + 512]})
            self.attn_multi(streams, 18, 512, 96 ** -0.5, T)


_HC = None


def get_hc():
    global _HC
    if _HC is None:
        _HC = host_consts()
    return _HC


def _dt_of(a):
    return BF16 if a.dtype == ml_dtypes.bfloat16 else F32


HC_SHAPES = {k: (list(v.shape), _dt_of(v)) for k, v in get_hc().items()}
DBG_SHAPES = {
    "MV": ([128, 2, 6, 8, 4], F32),
    "hT": ([8, 128, W], BF16),
    "HY": ([3, NCTX + NLAT, 512], BF16),
    "QT": ([4, 128, W], BF16),
    "KK": ([2, 128, W], BF16),
    "VA": ([128, 18, 2, 128], BF16),
    "mixT": ([8, 128, W], BF16),
    "Hf_l": ([2, 16, 128, 2, 512], F32),
    "Hf_c": ([2, 2, 128, 2, 512], F32),
    "XT": ([D, W], F32),
    "G": ([32, W], BF16),
}


def fm(v):
    return np.ascontiguousarray(np.asarray(v, np.float32).reshape(8, 128).T)


def host_prep(inputs, core):
    b0 = 2 * core
    P = {}
    x, ctx, c = inputs["x"], inputs["ctx"], inputs["c"]
    P["xT"] = _f(np.stack([np.concatenate([ctx[b].T, x[b].T], axis=1) for b in (b0, b0 + 1)]))
    P["cT"] = _f(np.stack([fm(c[b0]), fm(c[b0 + 1]), fm(inputs["c_ctx"]), fm(inputs["c_ctx"])], axis=-1))
    return P


def host_shared(inputs):
    Sh = {}
    Sh["w_mod"] = _f(inputs["w_mod"])
    Sh["bmodT"] = _f(inputs["b_mod"].reshape(2, 48, 128).transpose(2, 0, 1))
    Sh["ngT"] = _f(inputs["norm_g"].reshape(2, 2, 8, 128).transpose(3, 0, 1, 2))
    Sh["e_w_in"] = _f(inputs["e_w_in"][0])
    Sh["convw"] = _f(inputs["e_hy_conv_w"][0])
    Sh["convb"] = _f(inputs["e_hy_conv_b"][0][None])
    Sh["hy_w1"] = _f(inputs["e_hy_w1"][0])
    Sh["hy_w2"] = _f(inputs["e_hy_w2"][0])
    Sh["hy_w3"] = _f(inputs["e_hy_w3"][0])
    Sh["hy_vec"] = _f(np.stack([inputs["e_hy_b1"][0], inputs["e_hy_b2"][0], inputs["e_hy_freq"][0]], -1))
    Sh["fbias"] = _f(inputs["e_hy_fbias"][0])
    qkg = inputs["e_qk_g"][0]
    Sh["qkg"] = _f(np.stack([np.tile(qkg[0], 2), np.tile(qkg[1], 2)], -1))
    Sh["e_w_out"] = _f(inputs["e_w_out"][0])
    wr = np.concatenate([inputs["moe_w_rg"], inputs["moe_w_re"]], -1)
    Sh["wr"] = _f(wr.reshape(2, 8, 128, 36).transpose(2, 0, 1, 3))
    Sh["moe_w_gate"] = _f(inputs["moe_w_gate"])
    Sh["moe_w_up"] = _f(inputs["moe_w_up"])
    Sh["moe_w_down"] = _f(inputs["moe_w_down"])
    Sh["finalgT"] = fm(inputs["final_g"])
    Sh["o_w_in"] = _f(inputs["o_w_in"][0])
    qg = inputs["o_q_norm_g"][0]
    Sh["o_ng"] = _f(np.stack([qg[:128], qg[128:], inputs["o_kv_norm_g"][0]], -1))
    Sh["o_w_uq"] = _f(inputs["o_w_uq"][0])
    Sh["o_w_ukv"] = _f(inputs["o_w_ukv"][0])
    Sh["o_w_out"] = _f(inputs["o_w_out"][0])
    def st_layout(a):
        return a.reshape(2, 16, 2, 64).transpose(2, 3, 0, 1).reshape(128, 32)
    ldt = np.broadcast_to(inputs["o_s5_log_dt"][0][:, :, None], (2, 32, 64))
    Sh["s5p"] = _f(np.stack([st_layout(inputs["o_s5_a_re"][0]), st_layout(inputs["o_s5_a_im"][0]), st_layout(ldt)], 1))
    BD = np.zeros((2, 2, 16, 128, 128), np.float32)
    CD = np.zeros((2, 2, 16, 128, 128), np.float32)
    for ri, (bb, cc) in enumerate(((inputs["o_s5_b_re"][0], inputs["o_s5_c_re"][0]), (inputs["o_s5_b_im"][0], inputs["o_s5_c_im"][0]))):
        for d_ in range(2):
            for st in range(16):
                for gg in range(2):
                    g = 2 * st + gg
                    r0 = (g % 8) * 16
                    BD[ri, d_, st, r0:r0 + 16, gg * 64:(gg + 1) * 64] = bb[d_, g].T
                    CD[ri, d_, st, gg * 64:(gg + 1) * 64, r0:r0 + 16] = cc[d_, g].T
    Sh["s5BD"] = BD
    Sh["s5CD"] = CD
    Sh["dskT"] = _f(inputs["o_s5_d"][0].reshape(4, 128).T)
    Sh["glu_w"] = _f(inputs["o_glu_w"][0])
    Sh["glu_bT"] = _f(inputs["o_glu_b"][0].reshape(4, 128).T)
    Sh.update(get_hc())
    return Sh


def run(inputs, stop="all", dbg=(), cores=8):
    prog = Prog(stop, dbg)
    nc = prog.build()
    sh = host_shared(inputs)
    in_maps = []
    for core in range(cores):
        m = dict(sh)
        m.update(host_prep(inputs, core))
        in_maps.append({k: m[k] for k in prog.in_names})
    res = run_bass_kernel_spmd(nc, in_maps, core_ids=list(range(cores)))
    return prog, res


def kernel(**inputs):
    inputs = {k: np.asarray(v) for k, v in inputs.items()}
    prog, res = run(inputs)
    out = np.empty((16, NLAT, D), np.float32)
    for core in range(8):
        o = res.results[core]["outT"]
        for j in range(2):
            out[2 * core + j] = o[j].T
    return out
```

```python
import contextlib
import math
import numpy as np
import ml_dtypes
import concourse.bass as bass
import concourse.mybir as mybir
from concourse.bass_utils import run_bass_kernel_spmd

F32 = mybir.dt.float32
BF16 = mybir.dt.bfloat16
AF = mybir.ActivationFunctionType
ALU = mybir.AluOpType
AX = mybir.AxisListType


class Tile:
    def __init__(self, t, name=""):
        self.t = t
        self.name = name
        self.w = {}
        self.r = {}
        self.dkey = None

    def __getitem__(self, k):
        return self.t[k]


class Sched:
    ENG = ("pe", "act", "dve", "pool", "sp")

    def __init__(self, nc, stack):
        self.nc = nc
        self.stack = stack
        self.streams = {e: [] for e in self.ENG}
        self.cnt = {e: 0 for e in self.ENG}
        self.sems = {}
        self.known = {e: {} for e in self.ENG}
        for e in self.ENG:
            self.sems[e] = stack.enter_context(nc.semaphore("s_" + e))
        self.dfree = []
        self.dtiles = []
        self.dcount = {}
        self.ndsem = 0
        self.n_inst = 0
        self.uid = 0
        self.use_dummy = False

    def sbuf(self, shape, dtype, name, stack=None):
        self.uid += 1
        t = (stack or self.stack).enter_context(self.nc.sbuf_tensor("%s_%d" % (name, self.uid), list(shape), dtype))
        try:
            rem = self.nc.sbuf_bytes_remaining
            if rem < getattr(self, "min_rem", 1 << 60):
                self.min_rem = rem
                self.min_rem_at = name
        except Exception:
            pass
        return Tile(t, name)

    def psum(self, shape, dtype, name, stack=None):
        self.uid += 1
        t = (stack or self.stack).enter_context(self.nc.psum_tensor("%s_%d" % (name, self.uid), list(shape), dtype))
        return Tile(t, name)

    def dram(self, name, shape, dtype, kind="Internal"):
        t = self.nc.dram_tensor(name, list(shape), dtype, kind=kind)
        return Tile(t.ap(), name)

    def _dkey(self, tile):
        if tile.dkey is None:
            if self.dfree:
                tile.dkey = self.dfree.pop()
            else:
                key = "d%d" % self.ndsem
                self.ndsem += 1
                self.sems[key] = self.stack.enter_context(self.nc.semaphore("s_" + key))
                self.dcount[key] = 0
                tile.dkey = key
            self.dtiles.append(tile)
        return tile.dkey

    def release(self, tiles):
        for t in tiles:
            if t.dkey is not None:
                self.dfree.append(t.dkey)
                t.dkey = None

    def _waits(self, eng, reads, writes):
        need = {}

        def add(kv):
            if kv is None:
                return
            k, v = kv
            if k == eng and eng == "pe":
                return
            if need.get(k, 0) < v:
                need[k] = v

        for d in reads:
            for kv in d.w.items():
                add(kv)
        for d in writes:
            for kv in d.w.items():
                add(kv)
            for k, v in d.r.items():
                if k == eng:
                    continue
                add((k, v))
        out = []
        kn = self.known[eng]
        for k, v in need.items():
            if kn.get(k, 0) >= v:
                continue
            kn[k] = v
            out.append((k, v))
        return out

    def op(self, eng, fn, reads=(), writes=(), dummy=None):
        waits = self._waits(eng, reads, writes)
        self.cnt[eng] += 1
        val = self.cnt[eng]
        sem = self.sems[eng]
        sems = self.sems

        def emit(e, waits=waits, fn=fn, sem=sem):
            for k, v in waits:
                e.wait_ge(sems[k], v)
            if waits and dummy is not None and self.use_dummy:
                dummy(e)
                dummy(e)
            fn(e).then_inc(sem, 1)

        self.streams[eng].append(emit)
        for d in reads:
            if d.r.get(eng, 0) < val:
                d.r[eng] = val
        for d in writes:
            d.w[eng] = val
            d.r = {}
        self.n_inst += 1

    def i(self, eng, method, *args, reads=(), writes=(), **kw):
        def fn(e, method=method, args=args, kw=kw):
            return getattr(e, method)(*args, **kw)
        self.op(eng, fn, reads=reads, writes=writes)

    def mm(self, out_t, out_ap, lhs_t, lhs_ap, rhs_t, rhs_ap, start=True, stop=True, extra_reads=()):
        reads = [lhs_t, rhs_t] + list(extra_reads)
        M, N = out_ap.shape[0], out_ap.shape[-1]
        pd = self.pdummy

        def dummy(e):
            e.matmul(pd[0:M, 0:N], lhs_ap, rhs_ap, start=True, stop=True)

        self.op("pe", lambda e: e.matmul(out_ap, lhs_ap, rhs_ap, start=start, stop=stop), reads=reads, writes=[out_t], dummy=dummy)

    def tr(self, out_t, out_ap, in_t, in_ap, ident_t, ident_ap):
        pd = self.pdummy_bf if in_ap.dtype == BF16 else self.pdummy
        M, N = out_ap.shape[0], out_ap.shape[-1]

        def dummy(e):
            e.transpose(pd[0:M, 0:N], in_ap, ident_ap)

        self.op("pe", lambda e: e.transpose(out_ap, in_ap, ident_ap), reads=[in_t, ident_t], writes=[out_t], dummy=dummy)

    def dma(self, q, out_ap, in_ap, reads=(), writes=(), semtile=None, **kw):
        if semtile is None:
            semtile = writes[0]
        key = self._dkey(semtile)
        waits = self._waits(q, reads, writes)
        self.dcount[key] += 16
        val = self.dcount[key]
        sems = self.sems

        def emit(e, waits=waits):
            for k, v in waits:
                e.wait_ge(sems[k], v)
            e.dma_start(out=out_ap, in_=in_ap, **kw).then_inc(sems[key], 16)

        self.streams[q].append(emit)
        for d in reads:
            if d.r.get(key, 0) < val:
                d.r[key] = val
        for d in writes:
            d.w[key] = val
            d.r = {}
        self.n_inst += 1

    def barrier(self):
        targets = {e: self.cnt[e] for e in self.ENG if self.cnt[e] > 0}
        for k, v in self.dcount.items():
            if v > 0:
                targets[k] = v
        sems = self.sems
        for eng in self.ENG:
            kn = self.known[eng]
            ws = []
            for k, v in targets.items():
                if k == eng:
                    continue
                if kn.get(k, 0) >= v:
                    continue
                kn[k] = v
                ws.append((k, v))

            def emit(e, ws=ws):
                for k, v in ws:
                    e.wait_ge(sems[k], v)

            self.streams[eng].append(emit)
        for t in self.dtiles:
            if t.dkey is not None:
                self.dfree.append(t.dkey)
                t.dkey = None
        self.dtiles = []

    def emit(self):
        nc = self.nc
        with nc.Block() as block:
            @block.tensor
            def _(e):
                for f in self.streams["pe"]:
                    f(e)

            @block.scalar
            def _(e):
                for f in self.streams["act"]:
                    f(e)

            @block.vector
            def _(e):
                for f in self.streams["dve"]:
                    f(e)

            @block.gpsimd
            def _(e):
                for f in self.streams["pool"]:
                    f(e)

            @block.sync
            def _(e):
                for f in self.streams["sp"]:
                    f(e)
import os

D = 1024
NCTX = 256
NLAT = 2048
C0 = 2
L0 = 260
W = 2310
EPS = 1e-6
BLKS = [(C0, 256, "c")] + [(L0 + 512 * i, 512, "l") for i in range(4)]
TTILES = [(C0 + 128 * i, "c", i) for i in range(2)] + [(L0 + 128 * i, "l", i) for i in range(16)]
TWO_PI = 2.0 * math.pi


def _bf(a):
    return np.ascontiguousarray(a.astype(ml_dtypes.bfloat16))


def _f(a):
    return np.ascontiguousarray(a, dtype=np.float32)


def host_consts():
    Cn = {}
    Cn["ident"] = np.eye(128, dtype=np.float32)
    Cn["ones"] = np.ones((128, 128), np.float32)
    bo = np.zeros((128, 128), np.float32)
    bo[:64, :64] = 1.0
    bo[64:, 64:] = 1.0
    Cn["blockones"] = bo
    t = np.arange(NLAT)
    rows = (t // 64).astype(np.float32)
    cols = (t % 64).astype(np.float32)

    def rope_tab(dh):
        a = dh // 2
        half = a // 2
        freqs = (10000.0 ** (-np.arange(half, dtype=np.float32) / half)).astype(np.float32)
        cos = np.zeros((dh, NLAT), np.float32)
        sin = np.zeros((dh, NLAT), np.float32)
        R = np.zeros((dh, dh), np.float32)
        for d in range(dh):
            part = d // a
            i = d % a
            fi = i % half
            pos = rows if part == 0 else cols
            ang = (pos * freqs[fi]).astype(np.float32)
            cos[d] = np.cos(ang)
            sin[d] = np.sin(ang)
            if i < half:
                R[d, d + half] = -1.0
            else:
                R[d, d - half] = 1.0
        return cos, sin, R

    cos64, sin64, R64 = rope_tab(64)
    Cn["cos_e"] = np.concatenate([cos64, cos64], 0)
    Cn["sin_e"] = np.concatenate([sin64, sin64], 0)
    Rm = np.zeros((128, 128), np.float32)
    Rm[:64, :64] = R64
    Rm[64:, 64:] = R64
    Cn["rotT_e"] = _bf(Rm.T)
    cos32, sin32, R32 = rope_tab(32)
    co = np.zeros((128, NLAT), np.float32)
    so = np.zeros((128, NLAT), np.float32)
    co[64:96] = cos32
    so[64:96] = sin32
    Cn["cos_o"] = co
    Cn["sin_o"] = so
    Ro = np.zeros((128, 128), np.float32)
    Ro[64:96, 64:96] = R32
    Cn["rotT_o"] = _bf(Ro.T)
    for n, tag in ((NLAT, "l"), (NCTX, "c")):
        NT = n // 128
        N2 = 2 * n
        tt = np.arange(n, dtype=np.float64)
        kk = np.arange(n, dtype=np.float64)
        ang = 2.0 * np.pi * np.outer(tt, kk) / N2
        fre = np.cos(ang)
        fim = -np.sin(ang)
        fim[:, 0] = np.cos(np.pi * tt)
        fwd = np.concatenate([fre, fim], 1)
        fw = fwd.reshape(NT, 128, 2, NT, 128).transpose(3, 2, 1, 0, 4)
        Cn["fwd_" + tag] = _bf(fw)
        ire = (2.0 / N2) * np.cos(ang.T)
        ire[0, :] = 1.0 / N2
        iim = -(2.0 / N2) * np.sin(ang.T)
        iim[0, :] = (1.0 / N2) * np.cos(np.pi * tt)
        inv = np.concatenate([ire, iim], 0)
        iv = inv.reshape(2 * NT, 128, NT, 128).transpose(2, 1, 0, 3)
        Cn["inv_" + tag] = _bf(iv)
        tf = np.arange(n, dtype=np.float32)
        t_norm = tf / max(n - 1, 1)
        bands = np.linspace(1e-4, 15, 16, dtype=np.float32)
        a2 = (np.float32(2.0 * math.pi / n) * tf[:, None] * bands).astype(np.float32)
        z = np.concatenate([t_norm[:, None], np.cos(a2), np.sin(a2)], -1).astype(np.float32)
        Cn["zT_" + tag] = _f(z.T)
        HMAX = math.log(100.0) / 0.3
        HMIN = math.log(100.0) / 1.5
        deltas = np.linspace(HMIN, HMAX, 512, dtype=np.float32)
        dec = np.exp(-t_norm[:, None] * deltas).astype(np.float32)
        Cn["dec_" + tag] = _f(dec.reshape(NT, 128, 512).transpose(1, 0, 2))
    kk_ = np.arange(NCTX + NLAT)
    Cn["s5k1s"] = _f(np.broadcast_to(np.arange(36, dtype=np.float32), (128, 32, 36)))
    Cn["s5k0s"] = _f(np.broadcast_to(np.arange(64, dtype=np.float32), (128, 32, 64)))
    sel = np.zeros((32, 32, 128), np.float32)
    for e in range(32):
        sel[e, e, :] = 1.0
    return Cn


class Prog:
    def __init__(self, stop="all", dbg=()):
        self.stop = stop
        self.dbg = set(dbg)
        self.nc = bass.Bass("TRN2", target_bir_lowering=False)
        self.in_names = []
        self.out_names = []

    def inp(self, name, shape, dtype=F32):
        if self.stop == "modvec" and name not in ("cT", "w_mod", "bmodT", "ngT", "ident", "ones", "blockones"):
            return None
        self.in_names.append(name)
        return self.S.dram(name, shape, dtype, kind="ExternalInput")

    def outp(self, name, shape, dtype=F32):
        self.out_names.append(name)
        return self.S.dram(name, shape, dtype, kind="ExternalOutput")

    def load(self, dst, src_ap, src, q="sp", dst_ap=None):
        self.S.dma(q, dst[:] if dst_ap is None else dst_ap, src_ap, reads=[src], writes=[dst])

    def const_tile(self, name, shape, dtype, val):
        t = self.S.sbuf(shape, dtype, name)
        self.S.i("pool", "memset", t[:], val, writes=[t])
        return t

    def build(self):
        nc = self.nc
        with contextlib.ExitStack() as st:
            S = self.S = Sched(nc, st)
            self.declare_io()
            self.setup_consts()
            self.S.barrier()
            self.modvecs()
            self.S.barrier()
            if self.stop != "modvec":
                if self.stop not in ("mn1", "stage") and not os.environ.get("KSKIP0"):
                    self.hyena_filters()
                self.s5_tables()
                for b in range(2):
                    self.batch(b)
            self.S.barrier()
            S.emit()
        return nc

    def declare_io(self):
        I = self.I = {}
        I["xT"] = self.inp("xT", [2, D, NCTX + NLAT])
        I["cT"] = self.inp("cT", [128, 8, 4])
        I["w_mod"] = self.inp("w_mod", [2, D, 6 * D])
        I["bmodT"] = self.inp("bmodT", [128, 2, 48])
        I["ngT"] = self.inp("ngT", [128, 2, 2, 8])
        I["e_w_in"] = self.inp("e_w_in", [D, 2304])
        I["convw"] = self.inp("convw", [3, 1536])
        I["convb"] = self.inp("convb", [1, 1536])
        I["hy_w1"] = self.inp("hy_w1", [33, 64])
        I["hy_w2"] = self.inp("hy_w2", [64, 64])
        I["hy_w3"] = self.inp("hy_w3", [64, 2048])
        I["hy_vec"] = self.inp("hy_vec", [64, 3])
        I["fbias"] = self.inp("fbias", [2, 512])
        I["qkg"] = self.inp("qkg", [128, 2])
        I["e_w_out"] = self.inp("e_w_out", [D, D])
        I["wr"] = self.inp("wr", [128, 2, 8, 36])
        I["moe_w_gate"] = self.inp("moe_w_gate", [2, 32, D, 256])
        I["moe_w_up"] = self.inp("moe_w_up", [2, 32, D, 256])
        I["moe_w_down"] = self.inp("moe_w_down", [2, 32, 256, D])
        I["finalgT"] = self.inp("finalgT", [128, 8])
        I["o_w_in"] = self.inp("o_w_in", [D, 928])
        I["o_ng"] = self.inp("o_ng", [128, 3])
        I["o_w_uq"] = self.inp("o_w_uq", [256, 768])
        I["o_w_ukv"] = self.inp("o_w_ukv", [128, 1024])
        I["o_w_out"] = self.inp("o_w_out", [D, D])
        I["s5p"] = self.inp("s5p", [128, 3, 32])
        I["s5BD"] = self.inp("s5BD", [2, 2, 16, 128, 128])
        I["s5CD"] = self.inp("s5CD", [2, 2, 16, 128, 128])
        I["dskT"] = self.inp("dskT", [128, 4])
        I["glu_w"] = self.inp("glu_w", [512, 512])
        I["glu_bT"] = self.inp("glu_bT", [128, 4])
        for k, v in HC_SHAPES.items():
            I[k] = self.inp(k, v[0], v[1])
        self.out = self.outp("outT", [2, D, NLAT])
        S = self.S
        self.XT_d = [S.dram("XT_d%d" % b, [D, W], F32) for b in range(2)]
        self.HY_d = S.dram("HY_d", [3, NCTX + NLAT, 512], BF16)
        self.G_d = S.dram("G_d", [32, W], BF16)
        self.H2_d = S.dram("H2_d", [D, W], BF16)
        self.Hf_d = {"l": S.dram("Hf_l", [2, 16, 128, 2, 512], F32), "c": S.dram("Hf_c", [2, 2, 128, 2, 512], F32)}
        self.D = {}
        for name in self.dbg:
            self.D[name] = self.outp("dbg_" + name, DBG_SHAPES[name][0], DBG_SHAPES[name][1])

    def setup_consts(self):
        S, I = self.S, self.I
        self.ident = S.sbuf([128, 128], F32, "ident")
        self.load(self.ident, I["ident"][:], I["ident"])
        self.identb = S.sbuf([128, 128], BF16, "identb")
        self.load(self.identb, I["ident"][:], I["ident"], q="pool")
        self.ones = S.sbuf([128, 128], F32, "ones")
        self.load(self.ones, I["ones"][:], I["ones"])
        self.blockones = S.sbuf([128, 128], F32, "blockones")
        self.load(self.blockones, I["blockones"][:], I["blockones"])
        S.zeros_f = self.const_tile("zeros_f", [128, 128], F32, 0.0)
        S.zeros_bf = self.const_tile("zeros_bf", [128, 128], BF16, 0.0)
        self.eps = self.const_tile("eps", [128, 1], F32, EPS)
        self.negpi = self.const_tile("negpi", [128, 1], F32, -math.pi)
        self.MV = S.sbuf([128, 2, 6, 8, 4], F32, "MV")
        self.PS = [S.psum([128, 512], F32, "ps%d" % i) for i in range(7)]
        pdt = st_psum = S.psum([128, 512], F32, "psdummy")
        S.pdummy = pdt.t
        S.pdummy_bf = pdt[:, :].bitcast(BF16)
        self.psi = 0
        self.ps_res = set()

    def ps(self, reserve=False):
        while True:
            i = self.psi % 7
            self.psi += 1
            if i not in self.ps_res:
                break
        if reserve:
            self.ps_res.add(i)
        return self.PS[i]

    def ps_free(self, p):
        self.ps_res.discard(self.PS.index(p))

    def modvecs(self):
        S, I = self.S, self.I
        with contextlib.ExitStack() as ph:
            cT = S.sbuf([128, 8, 4], F32, "cT", ph)
            self.load(cT, I["cT"][:], I["cT"])
            scT = S.sbuf([128, 8, 4], F32, "scT", ph)
            S.i("act", "activation", scT[:], cT[:], AF.Silu, reads=[cT], writes=[scT])
            bm = S.sbuf([128, 2, 48], F32, "bm", ph)
            self.load(bm, I["bmodT"][:], I["bmodT"])
            ng = S.sbuf([128, 2, 2, 8], F32, "ng", ph)
            self.load(ng, I["ngT"][:], I["ngT"])
            wm = [S.sbuf([128, 8, 512], F32, "wm%d" % i, ph) for i in range(2)]
            modT = S.sbuf([128, 48, 4], F32, "modT", ph)
            MV = self.MV
            for i in range(2):
                pm = self.ps()
                pmv = pm[:, 0:192].rearrange("p (m n) -> p m n", n=4)
                wsrc = I["w_mod"][i].rearrange("(k p) n -> p k n", p=128)
                for cb in range(12):
                    w = wm[cb % 2]
                    self.load(w, wsrc[:, :, cb * 512:(cb + 1) * 512], I["w_mod"])
                    for mm in range(4):
                        m = cb * 4 + mm
                        for k in range(8):
                            S.mm(pm, pmv[:, m, :], w, w[:, k, mm * 128:(mm + 1) * 128], scT, scT[:, k, :], start=(k == 0), stop=(k == 7))
                S.i("dve", "tensor_copy", modT[:].rearrange("p m n -> p (m n)"), pm[:, 0:192], reads=[pm], writes=[modT])
                for n_ in range(4):
                    S.i("dve", "tensor_tensor", modT[:, :, n_], modT[:, :, n_], bm[:, i, :], ALU.add, reads=[modT, bm], writes=[modT])
                for kind, lo in ((0, 0), (2, 16), (3, 24), (5, 40)):
                    S.i("dve", "tensor_copy", MV[:, i, kind], modT[:, lo:lo + 8, :], reads=[modT], writes=[MV])
                for kind, lo, w_ in ((1, 8, 0), (4, 32, 1)):
                    S.i("dve", "tensor_single_scalar", MV[:, i, kind], modT[:, lo:lo + 8, :], 1.0, ALU.add, reads=[modT], writes=[MV])
                    for n_ in range(4):
                        S.i("dve", "tensor_tensor", MV[:, i, kind, :, n_], MV[:, i, kind, :, n_], ng[:, i, w_, :], ALU.mult, reads=[MV, ng], writes=[MV])
            if "MV" in self.D:
                S.dma("sp", self.D["MV"][:], MV[:], reads=[MV], writes=[self.D["MV"]], semtile=MV)
            S.barrier()

    def mv(self, layer, kind, c, n):
        return self.MV[:, layer, kind, c, n:n + 1]

    def modnorm_block(self, xt, n, layer, kindA, kindS, ncol, outs, otiles, h32=None):
        S = self.S
        T = self._mn[self._mni % len(self._mn)]
        self._mni += 1
        sq, rstd, tmp = T["sq"], T["rstd"], T["tmp"]
        ss = self.ps()
        S.i("act", "activation", sq[:, :, :n], xt[:, :, :n], AF.Square, reads=[xt], writes=[sq])
        for c in range(8):
            S.mm(ss, ss[:, :n], self.ones, self.ones[:], sq, sq[:, c, :n], start=(c == 0), stop=(c == 7))
        S.i("act", "activation", rstd[:, :n], ss[:, :n], AF.Sqrt, bias=self.eps[:], scale=1.0 / D, reads=[ss, self.eps], writes=[rstd])
        S.i("dve", "reciprocal", rstd[:, :n], rstd[:, :n], reads=[rstd], writes=[rstd])
        for c in range(8):
            S.i("dve", "scalar_tensor_tensor", tmp[:, c, :n], xt[:, c, :n], self.mv(layer, kindA, c, ncol), rstd[:, :n], ALU.mult, ALU.mult,
                reads=[xt, self.MV, rstd], writes=[tmp])
        for c in range(8):
            S.i("act", "activation", outs[c], tmp[:, c, :n], AF.Identity, bias=self.mv(layer, kindS, c, ncol), reads=[tmp, self.MV], writes=[otiles[c]])
            if h32 is not None:
                S.i("act", "activation", h32[:, c, :n], tmp[:, c, :n], AF.Identity, bias=self.mv(layer, kindS, c, ncol), reads=[tmp, self.MV], writes=[h32])

    def mn_alloc(self, ph, nbuf=1):
        S = self.S
        self._mni = 0
        self._mn = [{"sq": S.sbuf([128, 8, 512], F32, "mn_sq", ph), "rstd": S.sbuf([128, 512], F32, "mn_rstd", ph),
                     "tmp": S.sbuf([128, 8, 512], F32, "mn_tmp", ph)} for _ in range(nbuf)]

    def dump(self, name, src_tile, src_ap, dst_ap=None):
        if name in self.D and getattr(self, "cur_b", 0) == 0:
            d = self.D[name]
            self.S.dma("sp", d[:] if dst_ap is None else dst_ap(d), src_ap, reads=[src_tile], writes=[d], semtile=d)

    def batch(self, b):
        S, I = self.S, self.I
        self.cur_b = b
        XT = self.XT_d[b]
        S.dma("sp", XT[:, C0:C0 + NCTX], I["xT"][b, :, 0:NCTX], reads=[I["xT"]], writes=[XT], semtile=XT)
        S.dma("sp", XT[:, L0:L0 + NLAT], I["xT"][b, :, NCTX:], reads=[I["xT"]], writes=[XT], semtile=XT)
        S.barrier()
        if self.stop == "stage":
            self.dump("XT", XT, XT[:])
            return
        if not os.environ.get("KSKIP0"):
            self.layer0(b)
        S.barrier()
        if self.stop in ("l0", "mn1", "inproj", "mix0", "xa0"):
            return
        self.layer1(b)
        S.barrier()

    def xt_view(self, b):
        return self.XT_d[b][:, :].rearrange("(k p) w -> p k w", p=128)

    def layer0(self, b):
        S, I = self.S, self.I
        XTv = self.xt_view(b)
        XT = self.XT_d[b]
        with contextlib.ExitStack() as LY:
            with contextlib.ExitStack() as L1:
                hT = [S.sbuf([128, W], BF16, "hT%d" % c, L1) for c in range(8)]
                for c in range(8):
                    for (a0, a1) in ((0, C0), (C0 + NCTX, L0), (L0 + NLAT, W)):
                        S.i("pool", "memset", hT[c][:, a0:a1], 0.0, writes=[hT[c]])
                with contextlib.ExitStack() as L2:
                    QT = [S.sbuf([128, W], BF16, "QT%d" % j, L2) for j in range(4)]
                    KK = [S.sbuf([128, W], BF16, "KK%d" % g, L2) for g in range(2)]
                    VA = S.sbuf([128, 18, 2, 128], BF16, "VA", L2)
                    S.i("pool", "memset", VA[:, :, :, 64:128], 1.0, writes=[VA])
                    with contextlib.ExitStack() as ph:
                        self.mn_alloc(ph, nbuf=2)
                        xb = [S.sbuf([128, 8, 512], F32, "xb%d" % i, ph) for i in range(2)]
                        for bi, (s, n, kind) in enumerate(BLKS):
                            xt = xb[bi % 2]
                            self.load(xt, XTv[:, :, s:s + n], XT, dst_ap=xt[:, :, :n])
                            self.modnorm_block(xt, n, 0, 1, 0, 2 if kind == "c" else b, [hT[c][:, s:s + n] for c in range(8)], hT)
                        S.barrier()
                    for c in range(8):
                        self.dump("hT", hT[c], hT[c][:], lambda d, c=c: d[c])
                    if self.stop == "mn1":
                        return
                    with contextlib.ExitStack() as ph:
                        self.inproj_even(b, hT, QT, KK, VA, ph)
                        S.barrier()
                    for j in range(4):
                        self.dump("QT", QT[j], QT[j][:], lambda d, j=j: d[j])
                    for g in range(2):
                        self.dump("KK", KK[g], KK[g][:], lambda d, g=g: d[g])
                    self.dump("VA", VA, VA[:])
                    self.dump("HY", self.HY_d, self.HY_d[:])
                    if self.stop == "inproj":
                        return
                    mixT = hT
                    with contextlib.ExitStack() as ph:
                        self.attention_even(b, QT, KK, VA, mixT, ph)
                        S.barrier()
                with contextlib.ExitStack() as ph:
                    self.hyena(b, mixT, ph)
                    S.barrier()
                for c in range(8):
                    self.dump("mixT", mixT[c], mixT[c][:], lambda d, c=c: d[c])
                if self.stop == "mix0":
                    return
                with contextlib.ExitStack() as ph:
                    self.outproj_norm_router(b, 0, mixT, I["e_w_out"], ph, with_ctx=True)
                    S.barrier()
                self.dump("XT", XT, XT[:])
                self.dump("G", self.G_d, self.G_d[:])
                if self.stop == "xa0":
                    return
            with contextlib.ExitStack() as ph:
                self.moe(b, 0, ph, with_ctx=True)
                S.barrier()
            if b == 0:
                self.dump("XT", XT, XT[:])

    def inproj_even(self, b, hT, QT, KK, VA, ph):
        S, I = self.S, self.I
        wsrc = I["e_w_in"][:, :].rearrange("(k p) n -> p k n", p=128)
        wq = S.sbuf([128, 8, 768], BF16, "wqkv", ph)
        self.load(wq, wsrc[:, :, 1536:2304], I["e_w_in"], q="pool")
        wkk = S.sbuf([128, 8, 2, 128], BF16, "wkk", ph)
        for g in range(2):
            for hf in range(2):
                S.i("dve", "tensor_copy", wkk[:, :, g, hf * 64:(hf + 1) * 64], wq[:, :, 512 + 64 * g:576 + 64 * g], reads=[wq], writes=[wkk])
        qkg = S.sbuf([128, 2], F32, "qkg", ph)
        self.load(qkg, I["qkg"][:], I["qkg"])
        cos = S.sbuf([128, NLAT], F32, "cos", ph)
        sin = S.sbuf([128, NLAT], F32, "sin", ph)
        self.load(cos, I["cos_e"][:], I["cos_e"])
        self.load(sin, I["sin_e"][:], I["sin_e"])
        rotT = S.sbuf([128, 128], BF16, "rotT", ph)
        self.load(rotT, I["rotT_e"][:], I["rotT_e"])
        VT = S.sbuf([128, W], BF16, "VT", ph)
        sqh = S.sbuf([128, 512], F32, "sqh", ph)
        rs = S.sbuf([128, 512], F32, "rs", ph)
        qn = S.sbuf([128, 512], BF16, "qn", ph)
        t1 = S.sbuf([128, 512], F32, "t1", ph)
        t2 = S.sbuf([128, 512], F32, "t2", ph)
        mt = [("q", j) for j in range(4)] + [("k", g) for g in range(2)] + [("v", 0)]
        for (s, n, kind) in BLKS:
            for (typ, j) in mt:
                ps = self.ps()
                for kc in range(8):
                    if typ == "q":
                        lap = wq[:, kc, j * 128:(j + 1) * 128]
                        lt = wq
                    elif typ == "k":
                        lap = wkk[:, kc, j, :]
                        lt = wkk
                    else:
                        lap = wq[:, kc, 640:768]
                        lt = wq
                    S.mm(ps, ps[:, :n], lt, lap, hT[kc], hT[kc][:, s:s + n], start=(kc == 0), stop=(kc == 7))
                if typ == "v":
                    S.i("act", "activation", VT[:, s:s + n], ps[:, :n], AF.Copy, reads=[ps], writes=[VT])
                    continue
                dst_t = QT[j] if typ == "q" else KK[j]
                gi = 0 if typ == "q" else 1
                S.i("act", "activation", sqh[:, :n], ps[:, :n], AF.Square, reads=[ps], writes=[sqh])
                p2 = self.ps()
                S.mm(p2, p2[:, :n], self.blockones, self.blockones[:], sqh, sqh[:, :n])
                S.i("act", "activation", rs[:, :n], p2[:, :n], AF.Sqrt, bias=self.eps[:], scale=1.0 / 64, reads=[p2, self.eps], writes=[rs])
                S.i("dve", "reciprocal", rs[:, :n], rs[:, :n], reads=[rs], writes=[rs])
                if kind == "c":
                    S.i("dve", "scalar_tensor_tensor", dst_t[:, s:s + n], ps[:, :n], qkg[:, gi:gi + 1], rs[:, :n], ALU.mult, ALU.mult, reads=[ps, qkg, rs], writes=[dst_t])
                else:
                    tc0 = s - L0
                    S.i("dve", "scalar_tensor_tensor", qn[:, :n], ps[:, :n], qkg[:, gi:gi + 1], rs[:, :n], ALU.mult, ALU.mult, reads=[ps, qkg, rs], writes=[qn])
                    p3 = self.ps()
                    S.mm(p3, p3[:, :n], rotT, rotT[:], qn, qn[:, :n])
                    S.i("pool", "tensor_tensor", t1[:, :n], qn[:, :n], cos[:, tc0:tc0 + n], ALU.mult, reads=[qn, cos], writes=[t1])
                    S.i("dve", "tensor_tensor", t2[:, :n], p3[:, :n], sin[:, tc0:tc0 + n], ALU.mult, reads=[p3, sin], writes=[t2])
                    S.i("dve", "tensor_tensor", dst_t[:, s:s + n], t1[:, :n], t2[:, :n], ALU.add, reads=[t1, t2], writes=[dst_t])
        for ti, (s, kind, _) in enumerate(TTILES):
            pt = self.ps()
            ptb = pt[:, 0:64].bitcast(BF16)
            S.tr(pt, ptb, VT, VT[:, s:s + 128], self.identb, self.identb[:])
            S.i("act", "activation", VA[:, ti, :, 0:64], ptb.rearrange("p (g d) -> p g d", g=2), AF.Copy, reads=[pt], writes=[VA])
        cw = S.sbuf([128, 3, 512], F32, "cw", ph)
        cb = S.sbuf([128, 512], F32, "cb", ph)
        wh = S.sbuf([128, 8, 512], BF16, "wh", ph)
        whs = [S.sbuf([128, 8, 512], BF16, "whs%d" % t, ph) for t in range(3)]
        ob = [S.sbuf([128, 512], BF16, "hyo%d" % i, ph) for i in range(2)]
        oi = 0
        for nb in range(3):
            self.load(wh, wsrc[:, :, nb * 512:(nb + 1) * 512], I["e_w_in"], q="pool")
            self.load(cw, I["convw"][:, nb * 512:(nb + 1) * 512].partition_broadcast(128), I["convw"])
            self.load(cb, I["convb"][0, nb * 512:(nb + 1) * 512].partition_broadcast(128), I["convb"])
            for tap in range(3):
                for kc in range(8):
                    S.i("dve", "tensor_tensor", whs[tap][:, kc, :], wh[:, kc, :], cw[:, tap, :], ALU.mult, reads=[wh, cw], writes=[whs[tap]])
            for ti, (s, kind, idx) in enumerate(TTILES):
                ps = self.ps()
                first = True
                for tap in range(3):
                    for kc in range(8):
                        S.mm(ps, ps[:, :], hT[kc], hT[kc][:, s + tap - 1:s + tap - 1 + 128], whs[tap], whs[tap][:, kc, :], start=first, stop=(tap == 2 and kc == 7))
                        first = False
                o = ob[oi % 2]
                oi += 1
                S.i("dve", "tensor_tensor", o[:], ps[:, :], cb[:], ALU.add, reads=[ps, cb], writes=[o])
                row = ti * 128
                S.dma("sp", self.HY_d[nb, row:row + 128, :], o[:], reads=[o], writes=[self.HY_d], semtile=o)


    def attn_multi(self, streams, nkt, n, scale, ph_t):
        S = self.S
        ns = len(streams)
        for st_ in streams:
            st_["oacc"] = self.ps(reserve=True)
        npt = len(ph_t["pT"])
        cnt = [0]

        def finish(si, kt_, sT_):
            st_ = streams[si]
            pT = ph_t["pT"][cnt[0] % npt]
            cnt[0] += 1
            oacc = st_["oacc"]
            S.i("act", "activation", pT[:, :n], sT_[:, :n], AF.Exp, scale=scale, reads=[sT_], writes=[pT])
            S.mm(oacc, oacc[:, :n], st_["Vt"], st_["Vap"](kt_), pT, pT[:, :n], start=(kt_ == 0), stop=(kt_ == nkt - 1))

        depth = 2 if ns == 1 else 1
        pend = []
        for kt in range(nkt):
            for si, st_ in enumerate(streams):
                sT = self.ps()
                S.mm(sT, sT[:, :n], st_["KTt"], st_["Kap"](kt), st_["QTt"], st_["Qap"])
                pend.append((si, kt, sT))
            while len(pend) > depth * ns:
                finish(*pend.pop(0))
        while pend:
            finish(*pend.pop(0))
        for si, st_ in enumerate(streams):
            den = ph_t["den"][si]
            oacc = st_["oacc"]
            S.i("act", "activation", den[0:64, :n], oacc[64:128, :n], AF.Copy, reads=[oacc], writes=[den])
            S.i("dve", "reciprocal", den[0:64, :n], den[0:64, :n], reads=[den], writes=[den])
            S.i("dve", "tensor_tensor", st_["out_ap"], oacc[0:64, :n], den[0:64, :n], ALU.mult, reads=[oacc, den], writes=[st_["out_t"]])
            self.ps_free(oacc)

    def attention_even(self, b, QT, KK, VA, mixT, ph):
        S = self.S
        T = {"pT": [S.sbuf([128, 512], BF16, "pT%d" % i, ph) for i in range(6)], "den": [S.sbuf([64, 512], F32, "den%d" % i, ph) for i in range(2)]}

        def kcol(kt):
            return (C0 + 128 * kt) if kt < 2 else (L0 + 128 * (kt - 2))

        for hp in range(4):
            g = hp // 2
            j = hp
            out_t = mixT[4 + j]
            blocks = [(C0, 256, 2)] + [(L0 + 512 * i, 512, 18) for i in range(4)]
            for (s, n, nkt) in blocks:
                streams = []
                for po in (0, 64):
                    streams.append({"QTt": QT[j], "Qap": QT[j][po:po + 64, s:s + n], "KTt": KK[g],
                                    "Kap": (lambda kt, po=po: KK[g][po:po + 64, kcol(kt):kcol(kt) + 128]),
                                    "Vt": VA, "Vap": (lambda kt: VA[:, kt, g, :]), "out_t": out_t, "out_ap": out_t[po:po + 64, s:s + n]})
                self.attn_multi(streams, nkt, n, 0.125, T)

    def hyena_filters(self):
        S, I = self.S, self.I
        with contextlib.ExitStack() as ph:
            w1 = S.sbuf([33, 64], F32, "hw1", ph)
            w2 = S.sbuf([64, 64], F32, "hw2", ph)
            w3 = S.sbuf([64, 2048], F32, "hw3", ph)
            hv = S.sbuf([64, 3], F32, "hv", ph)
            self.load(w1, I["hy_w1"][:], I["hy_w1"])
            self.load(w2, I["hy_w2"][:], I["hy_w2"])
            self.load(w3, I["hy_w3"][:], I["hy_w3"])
            self.load(hv, I["hy_vec"][:], I["hy_vec"])
            fb = S.sbuf([64, 2], F32, "fb", ph)
            for i in range(2):
                S.i("dve", "tensor_tensor", fb[:, i:i + 1], hv[:, i:i + 1], hv[:, 2:3], ALU.mult, reads=[hv], writes=[fb])
            OFF = math.pi + TWO_PI * 16
            for tag, n in (("c", NCTX), ("l", NLAT)):
                NT = n // 128
                with contextlib.ExitStack() as p2:
                    zT = S.sbuf([33, n], F32, "zT", p2)
                    self.load(zT, I["zT_" + tag][:], I["zT_" + tag])
                    h1 = S.sbuf([64, n], F32, "h1", p2)
                    h2 = S.sbuf([64, n], F32, "h2", p2)
                    dec = S.sbuf([128, NT, 512], F32, "dec", p2)
                    self.load(dec, I["dec_" + tag][:], I["dec_" + tag])
                    U4 = S.sbuf([128, NT, 2048], BF16, "U4", p2)
                    a1 = S.sbuf([64, 512], F32, "a1", p2)
                    ki = S.sbuf([64, 512], mybir.dt.int32, "ki", p2)
                    kf = S.sbuf([64, 512], F32, "kf", p2)
                    tp = S.sbuf([128, 512], F32, "tp", p2)
                    sq = S.sbuf([128, 512], F32, "sqf", p2)
                    rn = S.sbuf([128, 2, 512], F32, "rn", p2)
                    nb = min(512, n)
                    for (src, wt, wap, dst, bi) in ((zT, w1, w1[:, :], h1, 0), (h1, w2, w2[:, :], h2, 1)):
                        for c0 in range(0, n, nb):
                            p = self.ps()
                            K = 33 if bi == 0 else 64
                            S.mm(p, p[0:64, :nb], wt, wap, src, src[0:K, c0:c0 + nb])
                            S.i("dve", "tensor_scalar", a1[:, :nb], p[0:64, :nb], hv[:, 2:3], fb[:, bi:bi + 1], ALU.mult, ALU.add, reads=[p, hv, fb], writes=[a1])
                            S.i("dve", "tensor_scalar", a1[:, :nb], a1[:, :nb], 1.0 / TWO_PI, 16.0, ALU.mult, ALU.add, reads=[a1], writes=[a1])
                            S.i("dve", "tensor_copy", ki[:, :nb], a1[:, :nb], reads=[a1], writes=[ki])
                            S.i("dve", "tensor_copy", kf[:, :nb], ki[:, :nb], reads=[ki], writes=[kf])
                            S.i("dve", "tensor_tensor", a1[:, :nb], a1[:, :nb], kf[:, :nb], ALU.subtract, reads=[a1, kf], writes=[a1])
                            S.i("dve", "tensor_single_scalar", kf[:, :nb], a1[:, :nb], 0.5, ALU.is_gt, reads=[a1], writes=[kf])
                            S.i("dve", "tensor_tensor", a1[:, :nb], a1[:, :nb], kf[:, :nb], ALU.subtract, reads=[a1, kf], writes=[a1])
                            S.i("act", "activation", dst[:, c0:c0 + nb], a1[:, :nb], AF.Sin, scale=TWO_PI, reads=[a1], writes=[dst])
                    ssum = [self.ps(reserve=True), self.ps(reserve=True)]
                    tpd = [S.sbuf([128, 512], F32, "tpd%d" % i, p2) for i in range(2)]
                    for tt in range(NT):
                        for o_ in range(2):
                            for d_ in range(2):
                                cbk = d_ * 2 + o_
                                tpx = tpd[d_]
                                p = self.ps()
                                S.mm(p, p[:, :], h2, h2[:, tt * 128:(tt + 1) * 128], w3, w3[:, cbk * 512:(cbk + 1) * 512])
                                S.i("dve", "tensor_tensor", tpx[:], p[:, :], dec[:, tt, :], ALU.mult, reads=[p, dec], writes=[tpx])
                                if d_ == 1 and tt == 0:
                                    S.i("dve", "memset", tpx[0:1, :], 0.0, writes=[tpx])
                                S.i("act", "activation", sq[:], tpx[:], AF.Square, reads=[tpx], writes=[sq])
                                S.mm(ssum[o_], ssum[o_][:, :], self.ones, self.ones[:], sq, sq[:], start=(tt == 0 and d_ == 0), stop=(tt == NT - 1 and d_ == 1))
                            S.i("dve", "tensor_tensor", U4[:, tt, o_ * 512:(o_ + 1) * 512], tpd[0][:], tpd[1][:], ALU.add, reads=tpd, writes=[U4])
                            S.i("dve", "tensor_tensor", U4[:, tt, (2 + o_) * 512:(3 + o_) * 512], tpd[0][:], tpd[1][:], ALU.subtract, reads=tpd, writes=[U4])
                    for o_ in range(2):
                        S.i("act", "activation", rn[:, o_, :], ssum[o_][:, :], AF.Sqrt, bias=self.eps[:], reads=[ssum[o_], self.eps], writes=[rn])
                        S.i("dve", "reciprocal", rn[:, o_, :], rn[:, o_, :], reads=[rn], writes=[rn])
                        self.ps_free(ssum[o_])
                    fw = [S.sbuf([128, 2, NT, 128], BF16, "fw%d" % i, p2) for i in range(2)]
                    Ht = [S.sbuf([128, 2, 512], F32, "Ht%d" % i, p2) for i in range(2)]
                    hi = 0
                    self.load(fw[0], I["fwd_" + tag][0].rearrange("r p k m -> p r k m"), I["fwd_" + tag])
                    for f in range(NT):
                        fwt = fw[f % 2]
                        if f + 1 < NT:
                            self.load(fw[(f + 1) % 2], I["fwd_" + tag][f + 1].rearrange("r p k m -> p r k m"), I["fwd_" + tag])
                        for o_ in range(2):
                            H = Ht[hi % 2]
                            hi += 1
                            for ri in range(2):
                                pf = self.ps()
                                c_lo = (o_ if ri == 0 else 2 + o_) * 512
                                for kt in range(NT):
                                    S.mm(pf, pf[:, :], fwt, fwt[:, ri, kt, :], U4, U4[:, kt, c_lo:c_lo + 512], start=(kt == 0), stop=(kt == NT - 1))
                                S.i("dve", "tensor_tensor", H[:, ri, :], pf[:, :], rn[:, o_, :], ALU.mult, reads=[pf, rn], writes=[H])
                            if f == 0:
                                pn = self.ps()
                                for kt in range(NT):
                                    S.mm(pn, pn[:, :], fwt, fwt[:, 1, kt, :], U4, U4[:, kt, o_ * 512:(o_ + 1) * 512], start=(kt == 0), stop=(kt == NT - 1))
                                S.i("dve", "tensor_tensor", H[0:1, 1, :], pn[0:1, :], rn[0:1, o_, :], ALU.mult, reads=[pn, rn], writes=[H])
                            S.dma("sp", self.Hf_d[tag][o_, f], H[:], reads=[H], writes=[self.Hf_d[tag]], semtile=H)
                    S.barrier()
            for tag in ("l", "c"):
                self.dump("Hf_" + tag, self.Hf_d[tag], self.Hf_d[tag][:])
            S.barrier()

    def hyena(self, b, mixT, ph):
        S, I = self.S, self.I
        fbb = S.sbuf([128, 2, 512], F32, "fbb", ph)
        self.load(fbb, I["fbias"][:, :].partition_broadcast(128), I["fbias"])
        for tag, n, row0, col0 in (("c", NCTX, 0, C0), ("l", NLAT, NCTX, L0)):
            NT = n // 128
            with contextlib.ExitStack() as p2:
                u = S.sbuf([128, NT, 512], BF16, "hu", p2)
                z = S.sbuf([128, NT, 512], BF16, "hz", p2)
                Yf = S.sbuf([128, 2 * NT, 512], BF16, "Yf", p2)
                fw = [S.sbuf([128, 2, NT, 128], BF16, "cfw%d" % i, p2) for i in range(2)]
                iv = [S.sbuf([128, 2 * NT, 128], BF16, "civ%d" % i, p2) for i in range(2)]
                Hl = [S.sbuf([128, 2, 512], F32, "Hl%d" % i, p2) for i in range(2)]
                ta = S.sbuf([128, 512], F32, "ta", p2)
                tb = S.sbuf([128, 512], F32, "tb", p2)
                xg = [S.sbuf([128, 512], BF16, "xg%d" % i, p2) for i in range(2)]
                vg = [S.sbuf([128, 512], BF16, "vg%d" % i, p2) for i in range(2)]
                og = [S.sbuf([128, 512], BF16, "og%d" % i, p2) for i in range(2)]
                self.load(u, self.HY_d[2, row0:row0 + n, :].rearrange("(t p) c -> p t c", p=128), self.HY_d)
                for o_ in range(2):
                    src = u if o_ == 0 else z
                    def ld_f(f_):
                        self.load(fw[f_ % 2], I["fwd_" + tag][f_].rearrange("r p k m -> p r k m"), I["fwd_" + tag])
                        self.load(Hl[f_ % 2], self.Hf_d[tag][o_, f_], self.Hf_d[tag])

                    def ld_t(t_):
                        self.load(iv[t_ % 2], I["inv_" + tag][t_], I["inv_" + tag])
                        r0_ = row0 + t_ * 128
                        self.load(xg[t_ % 2], self.HY_d[o_, r0_:r0_ + 128, :], self.HY_d)

                    ld_f(0)
                    for f in range(NT):
                        fwt = fw[f % 2]
                        H = Hl[f % 2]
                        if f + 1 < NT:
                            ld_f(f + 1)
                        else:
                            ld_t(0)
                        pr = self.ps()
                        pi = self.ps()
                        for kt in range(NT):
                            S.mm(pr, pr[:, :], fwt, fwt[:, 0, kt, :], src, src[:, kt, :], start=(kt == 0), stop=(kt == NT - 1))
                        for kt in range(NT):
                            S.mm(pi, pi[:, :], fwt, fwt[:, 1, kt, :], src, src[:, kt, :], start=(kt == 0), stop=(kt == NT - 1))
                        S.i("dve", "tensor_tensor", ta[:], pr[:, :], H[:, 1, :], ALU.mult, reads=[pr, H], writes=[ta])
                        S.i("dve", "tensor_tensor", tb[:], pi[:, :], H[:, 0, :], ALU.mult, reads=[pi, H], writes=[tb])
                        S.i("dve", "tensor_tensor", Yf[:, NT + f, :], ta[:], tb[:], ALU.add, reads=[ta, tb], writes=[Yf])
                        S.i("dve", "tensor_tensor", ta[:], pr[:, :], H[:, 0, :], ALU.mult, reads=[pr, H], writes=[ta])
                        S.i("dve", "tensor_tensor", tb[:], pi[:, :], H[:, 1, :], ALU.mult, reads=[pi, H], writes=[tb])
                        S.i("dve", "tensor_tensor", Yf[:, f, :], ta[:], tb[:], ALU.subtract, reads=[ta, tb], writes=[Yf])
                        if f == 0:
                            S.i("dve", "tensor_copy", Yf[0:1, 0, :], ta[0:1, :], reads=[ta], writes=[Yf])
                            S.i("dve", "tensor_copy", Yf[0:1, NT, :], tb[0:1, :], reads=[tb], writes=[Yf])
                    for tt in range(NT):
                        ivt = iv[tt % 2]
                        xt_ = xg[tt % 2]
                        if tt + 1 < NT:
                            ld_t(tt + 1)
                        py = self.ps()
                        for jj in range(2 * NT):
                            S.mm(py, py[:, :], ivt, ivt[:, jj, :], Yf, Yf[:, jj, :], start=(jj == 0), stop=(jj == 2 * NT - 1))
                        S.i("pool", "tensor_tensor", ta[:], src[:, tt, :], fbb[:, o_, :], ALU.mult, reads=[src, fbb], writes=[ta])
                        S.i("dve", "tensor_tensor", tb[:], py[:, :], ta[:], ALU.add, reads=[py, ta], writes=[tb])
                        if o_ == 0:
                            S.i("dve", "tensor_tensor", z[:, tt, :], tb[:], xt_[:], ALU.mult, reads=[tb, xt_], writes=[z])
                        else:
                            ot = og[tt % 2]
                            S.i("dve", "tensor_tensor", ot[:], tb[:], xt_[:], ALU.mult, reads=[tb, xt_], writes=[ot])
                            for cc in range(4):
                                pt = self.ps()
                                ptb = pt[:, 0:64].bitcast(BF16)
                                S.tr(pt, ptb, ot, ot[:, cc * 128:(cc + 1) * 128], self.identb, self.identb[:])
                                S.i("act", "activation", mixT[cc][:, col0 + tt * 128:col0 + (tt + 1) * 128], ptb, AF.Copy, reads=[pt], writes=[mixT[cc]])
                S.barrier()


    def outproj_norm_router(self, b, layer, mixT, w_out, ph, with_ctx):
        S, I = self.S, self.I
        XTv = self.xt_view(b)
        XT = self.XT_d[b]
        wo = S.sbuf([128, 8, 1024], BF16, "wo", ph)
        self.load(wo, w_out[:, :].rearrange("(k p) n -> p k n", p=128), w_out, q="pool")
        wr = S.sbuf([128, 8, 36], F32, "wr", ph)
        self.load(wr, I["wr"][:, layer], I["wr"])
        self.mn_alloc(ph, nbuf=2)
        xb = [S.sbuf([128, 8, 512], F32, "xb%d" % i, ph) for i in range(2)]
        h32s = [S.sbuf([128, 8, 512], F32, "h32_%d" % i, ph) for i in range(2)]
        hbs = [S.sbuf([128, 8, 512], BF16, "hb%d" % i, ph) for i in range(1)]
        H2v = self.H2_d[:, :].rearrange("(k p) w -> p k w", p=128)
        self._rt = {"L": S.sbuf([128, 4, 36], F32, "rtL", ph), "gm": S.sbuf([128, 4], F32, "rtgm", ph), "d4": S.sbuf([128, 4, 4], F32, "rtd4", ph),
                    "mg": S.sbuf([128, 4, 4], F32, "rtmg", ph), "eg": S.sbuf([128, 4, 4], F32, "rteg", ph), "se": S.sbuf([128, 4], F32, "rtse", ph),
                    "les": S.sbuf([128, 4, 8], F32, "rtles", ph), "t8": S.sbuf([128, 4, 8], F32, "rtt8", ph), "dl": S.sbuf([128, 4, 8], F32, "rtdl", ph),
                    "mk1": S.sbuf([128, 4, 8], F32, "rtmk1", ph), "mk12": S.sbuf([128, 4, 8], F32, "rtmk12", ph), "m2": S.sbuf([128, 4], F32, "rtm2", ph),
                    "inner": S.sbuf([128, 4, 8], F32, "rtin", ph), "sg4": S.sbuf([128, 4, 4], F32, "rtsg4", ph), "gates": S.sbuf([128, 4, 32], F32, "rtgates", ph)}
        gT = S.sbuf([32, 512], BF16, "rgT", ph)
        blks = BLKS if with_ctx else BLKS[1:]
        xdep = [Tile(XT.t, "xtblk%d" % i) for i in range(len(blks))]

        def part1(bi):
            s, n, kind = blks[bi]
            ncol = 2 if kind == "c" else b
            xt = xb[bi % 2]
            self.load(xt, XTv[:, :, s:s + n], xdep[bi], dst_ap=xt[:, :, :n])
            for mo in range(8):
                ps = self.ps()
                for kc in range(8):
                    S.mm(ps, ps[:, :n], wo, wo[:, kc, mo * 128:(mo + 1) * 128], mixT[kc], mixT[kc][:, s:s + n], start=(kc == 0), stop=(kc == 7))
                S.i("dve", "scalar_tensor_tensor", xt[:, mo, :n], ps[:, :n], self.mv(layer, 2, mo, ncol), xt[:, mo, :n], ALU.mult, ALU.add, reads=[ps, self.MV, xt], writes=[xt])
            S.dma("sp", XTv[:, :, s:s + n], xt[:, :, :n], reads=[xt], writes=[xdep[bi]], semtile=xt)

        part1(0)
        for bi, (s, n, kind) in enumerate(blks):
            if bi + 1 < len(blks):
                part1(bi + 1)
            ncol = 2 if kind == "c" else b
            xt = xb[bi % 2]
            hb = hbs[bi % len(hbs)]
            h32 = h32s[bi % 2]
            self.modnorm_block(xt, n, layer, 4, 3, ncol, [hb[:, c, :n] for c in range(8)], [hb] * 8, h32=h32)
            S.dma("sp", H2v[:, :, s:s + n], hb[:, :, :n], reads=[hb], writes=[self.H2_d], semtile=hb)
            nt = n // 128
            pl = self.ps()
            plv = pl[:, 0:256].rearrange("p (t c) -> p t c", c=64)
            for t in range(nt):
                for c in range(8):
                    S.mm(pl, plv[:, t, 0:36], h32, h32[:, c, t * 128:(t + 1) * 128], wr, wr[:, c, :], start=(c == 0), stop=(c == 7))
            R = self._rt

            def bc1(ap2, w_):
                return ap2.unsqueeze(2).to_broadcast([128, nt, w_])

            Lb, gm, d4, mg, eg, se, les, t8, dl, mk1, mk12, m2, inner, sg4, gates = (R[x] for x in
                ("L", "gm", "d4", "mg", "eg", "se", "les", "t8", "dl", "mk1", "mk12", "m2", "inner", "sg4", "gates"))
            allr = list(R.values())

            def dv(method, *args):
                S.i("dve", method, *args, reads=allr, writes=allr)

            S.i("dve", "tensor_copy", Lb[:, :nt, :], plv[:, :nt, 0:36], reads=[pl], writes=allr)
            dv("reduce_max", gm[:, :nt], Lb[:, :nt, 0:4], AX.X)
            dv("tensor_tensor", d4[:, :nt, :], Lb[:, :nt, 0:4], bc1(gm[:, :nt], 4), ALU.subtract)
            dv("tensor_single_scalar", mg[:, :nt, :], d4[:, :nt, :], 0.0, ALU.is_ge)
            S.i("act", "activation", eg[:, :nt, :], d4[:, :nt, :], AF.Exp, reads=allr, writes=allr)
            dv("reduce_sum", se[:, :nt], eg[:, :nt, :], AX.X)
            dv("reciprocal", se[:, :nt], se[:, :nt])
            dv("tensor_tensor", les[:, :nt, :], Lb[:, :nt, 4:12], mg[:, :nt, 0:1].to_broadcast([128, nt, 8]), ALU.mult)
            for g in range(1, 4):
                dv("tensor_tensor", t8[:, :nt, :], Lb[:, :nt, 4 + 8 * g:12 + 8 * g], mg[:, :nt, g:g + 1].to_broadcast([128, nt, 8]), ALU.mult)
                dv("tensor_tensor", les[:, :nt, :], les[:, :nt, :], t8[:, :nt, :], ALU.add)
            dv("reduce_max", gm[:, :nt], les[:, :nt, :], AX.X)
            dv("tensor_tensor", dl[:, :nt, :], les[:, :nt, :], bc1(gm[:, :nt], 8), ALU.subtract)
            dv("tensor_single_scalar", mk1[:, :nt, :], dl[:, :nt, :], 0.0, ALU.is_ge)
            dv("scalar_tensor_tensor", t8[:, :nt, :], mk1[:, :nt, :], -1.0e30, dl[:, :nt, :], ALU.mult, ALU.add)
            dv("reduce_max", m2[:, :nt], t8[:, :nt, :], AX.X)
            dv("tensor_tensor", mk12[:, :nt, :], dl[:, :nt, :], bc1(m2[:, :nt], 8), ALU.is_ge)
            S.i("act", "activation", m2[:, :nt], m2[:, :nt], AF.Exp, reads=allr, writes=allr)
            dv("tensor_single_scalar", gm[:, :nt], m2[:, :nt], 1.0, ALU.add)
            dv("reciprocal", gm[:, :nt], gm[:, :nt])
            dv("tensor_tensor", m2[:, :nt], m2[:, :nt], gm[:, :nt], ALU.mult)
            dv("tensor_tensor", gm[:, :nt], gm[:, :nt], m2[:, :nt], ALU.subtract)
            dv("tensor_tensor", inner[:, :nt, :], mk12[:, :nt, :], bc1(m2[:, :nt], 8), ALU.mult)
            dv("tensor_tensor", t8[:, :nt, :], mk1[:, :nt, :], bc1(gm[:, :nt], 8), ALU.mult)
            dv("tensor_tensor", inner[:, :nt, :], inner[:, :nt, :], t8[:, :nt, :], ALU.add)
            dv("tensor_tensor", sg4[:, :nt, :], mg[:, :nt, :], bc1(se[:, :nt], 4), ALU.mult)
            for g in range(4):
                dv("tensor_tensor", gates[:, :nt, 8 * g:8 * g + 8], inner[:, :nt, :], sg4[:, :nt, g:g + 1].to_broadcast([128, nt, 8]), ALU.mult)
            for t in range(nt):
                pt = self.ps()
                S.tr(pt, pt[0:32, 0:128], gates, gates[:, t, :], self.ident, self.ident[:])
                S.i("act", "activation", gT[:, t * 128:(t + 1) * 128], pt[0:32, 0:128], AF.Copy, reads=[pt], writes=[gT])
            S.dma("sp", self.G_d[:, s:s + n], gT[:, :n], reads=[gT], writes=[self.G_d], semtile=gT)

    def moe(self, b, layer, ph, with_ctx, final=False):
        S, I = self.S, self.I
        XTv = self.xt_view(b)
        XT = self.XT_d[b]
        h2T = [S.sbuf([128, W], BF16, "h2T%d" % c, ph) for c in range(8)]
        for c in range(8):
            self.load(h2T[c], self.H2_d[c * 128:(c + 1) * 128, :], self.H2_d)
        xT = [S.sbuf([128, W], F32, "xT%d" % c, ph) for c in range(8)]
        for c in range(8):
            self.load(xT[c], XT[c * 128:(c + 1) * 128, :], XT)
        wgu = [S.sbuf([128, 2, 2, 8, 256], BF16, "wgu%d" % i, ph) for i in range(2)]
        wd = [S.sbuf([128, 2, 2, 1024], BF16, "wd%d" % i, ph) for i in range(2)]
        gbc = [S.sbuf([128, 2, W], BF16, "gbc%d" % i, ph) for i in range(2)]
        sg = [S.sbuf([128, 512], BF16, "sg%d" % i, ph) for i in range(2)]
        su = [S.sbuf([128, 512], BF16, "su%d" % i, ph) for i in range(2)]
        hid = [S.sbuf([128, 512], BF16, "hid%d" % i, ph) for i in range(8)]
        blks = BLKS if with_ctx else BLKS[1:]
        self._hcnt = 0

        def load_pair(ep):
            wg_, wd_, gb_ = wgu[ep % 2], wd[ep % 2], gbc[ep % 2]
            for e in range(2):
                E = 2 * ep + e
                self.load(wg_, I["moe_w_gate"][layer, E].rearrange("(k p) n -> p k n", p=128), I["moe_w_gate"], q="pool", dst_ap=wg_[:, e, 0])
                self.load(wg_, I["moe_w_up"][layer, E].rearrange("(k p) n -> p k n", p=128), I["moe_w_up"], q="pool", dst_ap=wg_[:, e, 1])
                self.load(wd_, I["moe_w_down"][layer, E].rearrange("(k p) n -> p k n", p=128), I["moe_w_down"], q="pool", dst_ap=wd_[:, e])
            self.load(gb_, self.G_d[2 * ep:2 * ep + 2, :].partition_broadcast(128), self.G_d)

        def stage_a(ep, s, n):
            wg_, gb_ = wgu[ep % 2], gbc[ep % 2]
            hs = []
            for e in range(2):
                for m in range(2):
                    pg = self.ps()
                    pu = self.ps()
                    for kc in range(8):
                        S.mm(pg, pg[:, :n], wg_, wg_[:, e, 0, kc, m * 128:(m + 1) * 128], h2T[kc], h2T[kc][:, s:s + n], start=(kc == 0), stop=(kc == 7))
                    for kc in range(8):
                        S.mm(pu, pu[:, :n], wg_, wg_[:, e, 1, kc, m * 128:(m + 1) * 128], h2T[kc], h2T[kc][:, s:s + n], start=(kc == 0), stop=(kc == 7))
                    a_, u_ = sg[self._hcnt % 2], su[self._hcnt % 2]
                    hd = hid[self._hcnt % 8]
                    self._hcnt += 1
                    S.i("act", "activation", a_[:, :n], pg[:, :n], AF.Silu, reads=[pg], writes=[a_])
                    S.i("act", "activation", u_[:, :n], pu[:, :n], AF.Copy, reads=[pu], writes=[u_])
                    S.i("dve", "tensor_tensor", a_[:, :n], a_[:, :n], u_[:, :n], ALU.mult, reads=[a_, u_], writes=[a_])
                    S.i("dve", "tensor_tensor", hd[:, :n], a_[:, :n], gb_[:, e, s:s + n], ALU.mult, reads=[a_, gb_], writes=[hd])
                    hs.append((hd, e, m))
            return hs

        def stage_b(ep, s, n, ncol, hs):
            wd_ = wd[ep % 2]
            for mo in range(8):
                py = self.ps()
                for ii, (hd, e, m) in enumerate(hs):
                    S.mm(py, py[:, :n], wd_, wd_[:, e, m, mo * 128:(mo + 1) * 128], hd, hd[:, :n], start=(ii == 0), stop=(ii == 3))
                S.i("dve", "scalar_tensor_tensor", xT[mo][:, s:s + n], py[:, :n], self.mv(layer, 5, mo, ncol), xT[mo][:, s:s + n], ALU.mult, ALU.add,
                    reads=[py, self.MV, xT[mo]], writes=[xT[mo]])

        load_pair(0)
        for ep in range(16):
            if ep + 1 < 16:
                load_pair(ep + 1)
            pend = None
            for (s, n, kind) in blks:
                ncol = 2 if kind == "c" else b
                hs = stage_a(ep, s, n)
                if pend is not None:
                    stage_b(*pend)
                pend = (ep, s, n, ncol, hs)
            stage_b(*pend)
        if not final:
            for c in range(8):
                S.dma("sp", XT[c * 128:(c + 1) * 128, :], xT[c][:], reads=[xT[c]], writes=[XT], semtile=xT[c])
            return
        fg = S.sbuf([128, 8], F32, "fg", ph)
        self.load(fg, I["finalgT"][:], I["finalgT"])
        sq_t, rs_t = wgu[0], gbc[0]
        sq = sq_t[:].rearrange("p a b c d -> p (a b c d)").bitcast(F32).rearrange("p (c n) -> p c n", n=512)
        rsv = rs_t[:].rearrange("p a w -> p (a w)").bitcast(F32)
        for bi in range(4):
            s = L0 + 512 * bi
            rs_ = rsv[:, 512 * (bi % 2):512 * (bi % 2 + 1)]
            for c in range(8):
                S.i("act", "activation", sq[:, c, :], xT[c][:, s:s + 512], AF.Square, reads=[xT[c]], writes=[sq_t])
            ss = self.ps()
            for c in range(8):
                S.mm(ss, ss[:, :], self.ones, self.ones[:], sq_t, sq[:, c, :], start=(c == 0), stop=(c == 7))
            S.i("act", "activation", rs_, ss[:, :], AF.Sqrt, bias=self.eps[:], scale=1.0 / D, reads=[ss, self.eps], writes=[rs_t])
            S.i("dve", "reciprocal", rs_, rs_, reads=[rs_t], writes=[rs_t])
            for c in range(8):
                S.i("dve", "scalar_tensor_tensor", xT[c][:, s:s + 512], xT[c][:, s:s + 512], fg[:, c:c + 1], rs_, ALU.mult, ALU.mult, reads=[xT[c], fg, rs_t], writes=[xT[c]])
        for c in range(8):
            S.dma("sp", self.out[b, c * 128:(c + 1) * 128, :], xT[c][:, L0:L0 + NLAT], reads=[xT[c]], writes=[self.out], semtile=xT[c])

    def layer1(self, b):
        S, I = self.S, self.I
        XTv = self.xt_view(b)
        XT = self.XT_d[b]
        with contextlib.ExitStack() as LY:
            with contextlib.ExitStack() as L1:
                hT = [S.sbuf([128, W], BF16, "hT%d" % c, L1) for c in range(8)]
                with contextlib.ExitStack() as ph:
                    self.mn_alloc(ph, nbuf=2)
                    xb = [S.sbuf([128, 8, 512], F32, "xb%d" % i, ph) for i in range(2)]
                    for bi, (s, n, kind) in enumerate(BLKS):
                        xt = xb[bi % 2]
                        self.load(xt, XTv[:, :, s:s + n], XT, dst_ap=xt[:, :, :n])
                        self.modnorm_block(xt, n, 1, 1, 0, 2 if kind == "c" else b, [hT[c][:, s:s + n] for c in range(8)], hT)
                    S.barrier()
                mixT = hT
                with contextlib.ExitStack() as L2:
                    uS = [S.sbuf([128, 4, NCTX + NLAT], BF16, "uS%d" % d, L2) for d in range(2)]
                    with contextlib.ExitStack() as L3:
                        cqn = S.sbuf([128, 2, W], BF16, "cqn", L3)
                        ckvn = S.sbuf([128, W], BF16, "ckvn", L3)
                        KR = S.sbuf([128, W], BF16, "KR", L3)
                        with contextlib.ExitStack() as ph:
                            self.inproj_odd(b, hT, cqn, ckvn, KR, uS, ph)
                            S.barrier()
                        with contextlib.ExitStack() as ph:
                            self.mla(b, cqn, ckvn, KR, mixT, ph)
                            S.barrier()
                    with contextlib.ExitStack() as ph:
                        self.s5(b, uS, mixT, ph)
                        S.barrier()
                for c in range(8):
                    self.dump("mixT", mixT[c], mixT[c][:], lambda d, c=c: d[c])
                if self.stop == "mix1":
                    return
                with contextlib.ExitStack() as ph:
                    self.outproj_norm_router(b, 1, mixT, I["o_w_out"], ph, with_ctx=False)
                    S.barrier()
                if self.stop == "xa1":
                    self.dump("XT", XT, XT[:])
                    return
            with contextlib.ExitStack() as ph:
                self.moe(b, 1, ph, with_ctx=False, final=True)
                S.barrier()

    def final_norm(self, b, ph):
        S, I = self.S, self.I
        XTv = self.xt_view(b)
        XT = self.XT_d[b]
        fg = S.sbuf([128, 8], F32, "fg", ph)
        self.load(fg, I["finalgT"][:], I["finalgT"])
        xb = [S.sbuf([128, 8, 512], F32, "fxb%d" % i, ph) for i in range(2)]
        sq = S.sbuf([128, 8, 512], F32, "fsq", ph)
        rstd = S.sbuf([128, 512], F32, "frstd", ph)
        ob = [S.sbuf([128, 8, 512], F32, "fob%d" % i, ph) for i in range(2)]
        outv = self.out[b].rearrange("(k p) t -> p k t", p=128)
        for bi in range(4):
            s = L0 + 512 * bi
            xt = xb[bi % 2]
            o = ob[bi % 2]
            self.load(xt, XTv[:, :, s:s + 512], XT)
            S.i("act", "activation", sq[:], xt[:], AF.Square, reads=[xt], writes=[sq])
            ss = self.ps()
            for c in range(8):
                S.mm(ss, ss[:, :], self.ones, self.ones[:], sq, sq[:, c, :], start=(c == 0), stop=(c == 7))
            S.i("act", "activation", rstd[:], ss[:, :], AF.Sqrt, bias=self.eps[:], scale=1.0 / D, reads=[ss, self.eps], writes=[rstd])
            S.i("dve", "reciprocal", rstd[:], rstd[:], reads=[rstd], writes=[rstd])
            for c in range(8):
                S.i("dve", "scalar_tensor_tensor", o[:, c, :], xt[:, c, :], fg[:, c:c + 1], rstd[:], ALU.mult, ALU.mult, reads=[xt, fg, rstd], writes=[o])
            S.dma("sp", outv[:, :, 512 * bi:512 * (bi + 1)], o[:], reads=[o], writes=[self.out], semtile=o)

    def inproj_odd(self, b, hT, cqn, ckvn, KR, uS, ph):
        S, I = self.S, self.I
        wsrc = I["o_w_in"][:, :].rearrange("(k p) n -> p k n", p=128)
        wi = S.sbuf([128, 8, 928], BF16, "wi", ph)
        self.load(wi, wsrc, I["o_w_in"], q="pool")
        wkr = S.sbuf([128, 8, 128], BF16, "wkr", ph)
        S.i("pool", "memset", wkr[:], 0.0, writes=[wkr])
        S.i("dve", "tensor_copy", wkr[:, :, 64:96], wi[:, :, 384:416], reads=[wi], writes=[wkr])
        ng = S.sbuf([128, 3], F32, "ong", ph)
        self.load(ng, I["o_ng"][:], I["o_ng"])
        cos = S.sbuf([128, NLAT], F32, "cos", ph)
        sin = S.sbuf([128, NLAT], F32, "sin", ph)
        self.load(cos, I["cos_o"][:], I["cos_o"])
        self.load(sin, I["sin_o"][:], I["sin_o"])
        rotT = S.sbuf([128, 128], BF16, "rotT", ph)
        self.load(rotT, I["rotT_o"][:], I["rotT_o"])
        sq = S.sbuf([128, 2, 512], F32, "osq", ph)
        rs = S.sbuf([128, 512], F32, "ors", ph)
        krn = S.sbuf([128, 512], BF16, "krn", ph)
        t1 = S.sbuf([128, 512], F32, "ot1", ph)
        t2 = S.sbuf([128, 512], F32, "ot2", ph)
        KD = os.environ.get("KDBG", "ABCD")
        for (s, n, kind) in BLKS:
          if "A" in KD:
            pq = [self.ps(), self.ps()]
            for j in range(2):
                for kc in range(8):
                    S.mm(pq[j], pq[j][:, :n], wi, wi[:, kc, j * 128:(j + 1) * 128], hT[kc], hT[kc][:, s:s + n], start=(kc == 0), stop=(kc == 7))
                S.i("act", "activation", sq[:, j, :n], pq[j][:, :n], AF.Square, reads=[pq[j]], writes=[sq])
            ss = self.ps()
            for j in range(2):
                S.mm(ss, ss[:, :n], self.ones, self.ones[:], sq, sq[:, j, :n], start=(j == 0), stop=(j == 1))
            S.i("act", "activation", rs[:, :n], ss[:, :n], AF.Sqrt, bias=self.eps[:], scale=1.0 / 256, reads=[ss, self.eps], writes=[rs])
            S.i("dve", "reciprocal", rs[:, :n], rs[:, :n], reads=[rs], writes=[rs])
            for j in range(2):
                S.i("dve", "scalar_tensor_tensor", cqn[:, j, s:s + n], pq[j][:, :n], ng[:, j:j + 1], rs[:, :n], ALU.mult, ALU.mult, reads=[pq[j], ng, rs], writes=[cqn])
          if "B" in KD:
            pk = self.ps()
            for kc in range(8):
                S.mm(pk, pk[:, :n], wi, wi[:, kc, 256:384], hT[kc], hT[kc][:, s:s + n], start=(kc == 0), stop=(kc == 7))
            S.i("act", "activation", sq[:, 0, :n], pk[:, :n], AF.Square, reads=[pk], writes=[sq])
            ss = self.ps()
            S.mm(ss, ss[:, :n], self.ones, self.ones[:], sq, sq[:, 0, :n])
            S.i("act", "activation", rs[:, :n], ss[:, :n], AF.Sqrt, bias=self.eps[:], scale=1.0 / 128, reads=[ss, self.eps], writes=[rs])
            S.i("dve", "reciprocal", rs[:, :n], rs[:, :n], reads=[rs], writes=[rs])
            S.i("dve", "scalar_tensor_tensor", ckvn[:, s:s + n], pk[:, :n], ng[:, 2:3], rs[:, :n], ALU.mult, ALU.mult, reads=[pk, ng, rs], writes=[ckvn])
          if "C" in KD:
            pr = self.ps()
            for kc in range(8):
                S.mm(pr, pr[:, :n], wkr, wkr[:, kc, :], hT[kc], hT[kc][:, s:s + n], start=(kc == 0), stop=(kc == 7))
            if kind == "c":
                S.i("act", "activation", KR[:, s:s + n], pr[:, :n], AF.Copy, reads=[pr], writes=[KR])
            else:
                tc0 = s - L0
                S.i("act", "activation", krn[:, :n], pr[:, :n], AF.Copy, reads=[pr], writes=[krn])
                p3 = self.ps()
                S.mm(p3, p3[:, :n], rotT, rotT[:], krn, krn[:, :n])
                S.i("pool", "tensor_tensor", t1[:, :n], krn[:, :n], cos[:, tc0:tc0 + n], ALU.mult, reads=[krn, cos], writes=[t1])
                S.i("dve", "tensor_tensor", t2[:, :n], p3[:, :n], sin[:, tc0:tc0 + n], ALU.mult, reads=[p3, sin], writes=[t2])
                S.i("dve", "tensor_tensor", KR[:, s:s + n], t1[:, :n], t2[:, :n], ALU.add, reads=[t1, t2], writes=[KR])
          if "D" in KD:
            for j in range(4):
                pu = self.ps()
                for kc in range(8):
                    S.mm(pu, pu[:, :n], wi, wi[:, kc, 416 + j * 128:416 + (j + 1) * 128], hT[kc], hT[kc][:, s:s + n], start=(kc == 0), stop=(kc == 7))
                if kind == "c":
                    d0, d1 = 0, NLAT
                else:
                    d0, d1 = NCTX + (s - L0), s - L0
                S.i("act", "activation", uS[0][:, j, d0:d0 + n], pu[:, :n], AF.Copy, reads=[pu], writes=[uS[0]])
                S.i("pool", "tensor_copy", uS[1][:, j, d1:d1 + n], uS[0][:, j, d0:d0 + n], reads=[uS[0]], writes=[uS[1]])

    def s5_tables(self):
        S, I = self.S, self.I
        T_ = NCTX + NLAT
        self.ST_d = S.dram("ST_d", [32, 128, 4, T_], BF16)
        self.RMAG = S.sbuf([128, 32], F32, "s5rmag")
        with contextlib.ExitStack() as ph:
            prm = S.sbuf([128, 3, 32], F32, "s5prm", ph)
            self.load(prm, I["s5p"][:], I["s5p"])
            W_ = S.sbuf([128, 24, 32], F32, "s5w", ph)
            ki = S.sbuf([128, 32], mybir.dt.int32, "s5ki", ph)
            CO = S.sbuf([128, 4, 32], F32, "s5co", ph)
            PH = S.sbuf([128, 2, 32], F32, "s5ph", ph)
            a_re, a_im, ldt = prm[:, 0, :], prm[:, 1, :], prm[:, 2, :]
            allt = [W_, prm, CO, PH, self.RMAG]

            def w(i):
                return W_[:, i, :]

            def tt(o, x, y, op):
                S.i("dve", "tensor_tensor", o, x, y, op, reads=allt, writes=allt)

            def ts(o, x, s1, s2, o1, o2):
                S.i("dve", "tensor_scalar", o, x, s1, s2, o1, o2, reads=allt, writes=allt)

            def fracfix(o, y):
                S.i("dve", "tensor_copy", ki[:], y, reads=allt, writes=[ki])
                S.i("dve", "tensor_copy", w(20), ki[:], reads=[ki], writes=allt)
                tt(o, y, w(20), ALU.subtract)
                S.i("dve", "tensor_single_scalar", w(20), o, 0.5, ALU.is_gt, reads=allt, writes=allt)
                tt(o, o, w(20), ALU.subtract)

            S.i("act", "activation", w(0), ldt, AF.Exp, reads=allt, writes=allt)
            tt(w(1), a_re, w(0), ALU.mult)
            S.i("act", "activation", self.RMAG[:], w(1), AF.Exp, reads=allt, writes=allt)
            tt(w(3), a_im, w(0), ALU.mult)
            ts(PH[:, 0, :], w(3), 1.0 / TWO_PI, 1.0, ALU.mult, ALU.mult)
            ts(w(4), PH[:, 0, :], 1.0, 16.0, ALU.mult, ALU.add)
            ts(w(5), PH[:, 0, :], 1.0, 16.25, ALU.mult, ALU.add)
            fracfix(w(21), w(4))
            S.i("act", "activation", w(6), w(21), AF.Sin, scale=TWO_PI, reads=allt, writes=allt)
            fracfix(w(21), w(5))
            S.i("act", "activation", w(7), w(21), AF.Sin, scale=TWO_PI, reads=allt, writes=allt)
            ts(w(22), PH[:, 0, :], 64.0, 16.0, ALU.mult, ALU.add)
            fracfix(PH[:, 1, :], w(22))
            tt(w(12), self.RMAG[:], w(7), ALU.mult)
            tt(w(13), self.RMAG[:], w(6), ALU.mult)
            ts(w(8), w(12), -1.0, 1.0, ALU.add, ALU.mult)
            tt(w(9), a_re, a_re, ALU.mult)
            tt(w(10), a_im, a_im, ALU.mult)
            tt(w(9), w(9), w(10), ALU.add)
            S.i("dve", "reciprocal", w(9), w(9), reads=allt, writes=allt)
            tt(w(10), w(8), a_re, ALU.mult)
            tt(w(11), w(13), a_im, ALU.mult)
            tt(w(10), w(10), w(11), ALU.add)
            tt(CO[:, 0, :], w(10), w(9), ALU.mult)
            tt(w(10), w(13), a_re, ALU.mult)
            tt(w(11), w(8), a_im, ALU.mult)
            tt(w(10), w(10), w(11), ALU.subtract)
            tt(CO[:, 1, :], w(10), w(9), ALU.mult)
            ts(CO[:, 2, :], CO[:, 0, :], -1.0, 1.0, ALU.mult, ALU.mult)
            ts(CO[:, 3, :], CO[:, 1, :], -1.0, 1.0, ALU.mult, ALU.mult)
            k1s = S.sbuf([128, 32, 36], F32, "s5k1s", ph)
            k0s = S.sbuf([128, 32, 64], F32, "s5k0s", ph)
            self.load(k1s, I["s5k1s"][:], I["s5k1s"])
            self.load(k0s, I["s5k0s"][:], I["s5k0s"])
            TA = S.sbuf([128, 6, 32, 36], F32, "s5TA", ph)
            TB = S.sbuf([128, 3, 32, 64], F32, "s5TB", ph)
            ya = S.sbuf([128, 32, 36], F32, "s5ya", ph)
            yb_ = S.sbuf([128, 32, 64], F32, "s5yb", ph)
            kia = S.sbuf([128, 32, 36], mybir.dt.int32, "s5kia", ph)
            kib = S.sbuf([128, 32, 64], mybir.dt.int32, "s5kib", ph)
            fa = S.sbuf([128, 32, 36], F32, "s5fa", ph)
            fb_ = S.sbuf([128, 32, 64], F32, "s5fb", ph)
            small = [TA, TB, ya, yb_, fa, fb_, PH, CO]

            def sincos(yt, kit, ft_, ktab, phi_idx, n_, dst_c, dst_s):
                for col in range(32):
                    S.i("dve", "tensor_scalar", yt[:, col, :], ktab[:, col, :], PH[:, phi_idx, col:col + 1], 16.0, ALU.mult, ALU.add, reads=[ktab] + small, writes=small)
                for (off, dst) in ((0.0, dst_s), (0.25, dst_c)):
                    if off:
                        S.i("dve", "tensor_single_scalar", yt[:], yt[:], off, ALU.add, reads=small, writes=small)
                    S.i("dve", "tensor_copy", kit[:], yt[:], reads=small, writes=[kit])
                    S.i("dve", "tensor_copy", ft_[:], kit[:], reads=[kit], writes=small)
                    S.i("dve", "tensor_tensor", ft_[:], yt[:], ft_[:], ALU.subtract, reads=small, writes=small)
                    S.i("dve", "tensor_single_scalar", yt[:], ft_[:], 0.5, ALU.is_gt, reads=small, writes=small)
                    S.i("dve", "tensor_tensor", ft_[:], ft_[:], yt[:], ALU.subtract, reads=small, writes=small)
                    S.i("act", "activation", dst, ft_[:], AF.Sin, scale=TWO_PI, reads=small, writes=small)
                    S.i("dve", "tensor_copy", yt[:], kit[:], reads=[kit], writes=small)
                    S.i("dve", "tensor_tensor", yt[:], yt[:], ft_[:], ALU.add, reads=small, writes=small)

            sincos(ya, kia, fa, k1s, 1, 36, TA[:, 0], TA[:, 1])
            sincos(yb_, kib, fb_, k0s, 0, 64, TB[:, 0], TB[:, 1])
            for col in range(32):
                d = col // 16
                sg = -1.0 if d == 0 else 1.0
                c1 = slice(col, col + 1)
                cA, sA = TA[:, 0, col, :], TA[:, 1, col, :]
                S.i("dve", "tensor_single_scalar", TA[:, 2, col, :], cA, CO[:, 0, c1], ALU.mult, reads=small, writes=small)
                S.i("dve", "scalar_tensor_tensor", TA[:, 2, col, :], sA, CO[:, 3 if sg > 0 else 1, c1], TA[:, 2, col, :], ALU.mult, ALU.add, reads=small, writes=small)
                S.i("dve", "tensor_single_scalar", TA[:, 3, col, :], cA, CO[:, 1, c1], ALU.mult, reads=small, writes=small)
                S.i("dve", "scalar_tensor_tensor", TA[:, 3, col, :], sA, CO[:, 0 if sg > 0 else 2, c1], TA[:, 3, col, :], ALU.mult, ALU.add, reads=small, writes=small)
                S.i("dve", "tensor_single_scalar", TA[:, 4, col, :], cA, -sg, ALU.mult, reads=small, writes=small)
                S.i("dve", "tensor_single_scalar", TA[:, 5, col, :], sA, -sg, ALU.mult, reads=small, writes=small)
                S.i("dve", "tensor_single_scalar", TB[:, 2, col, :], TB[:, 1, col, :], sg, ALU.mult, reads=small, writes=small)
            t1 = S.sbuf([128, 36, 64], F32, "s5t1", ph)
            t2 = S.sbuf([128, 36, 64], F32, "s5t2", ph)
            tabs = [S.sbuf([128, 4, T_], BF16, "s5tab%d" % i, ph) for i in range(2)]

            def oa(i, col):
                return TA[:, i, col, :].unsqueeze(2).to_broadcast([128, 36, 64])

            def ob(i, col):
                return TB[:, i, col, :].unsqueeze(1).to_broadcast([128, 36, 64])

            for col in range(32):
                tab = tabs[col % 2]

                def tv(j):
                    return tab[:, j, :].rearrange("p (a b) -> p a b", b=64)

                for (j, (x1, y1, x2, y2, op)) in enumerate(((2, 0, 3, 2, ALU.subtract), (3, 0, 2, 2, ALU.add), (0, 0, 1, 1, ALU.subtract), (5, 0, 4, 1, ALU.add))):
                    S.i("dve", "tensor_tensor", t1[:], oa(x1, col), ob(y1, col), ALU.mult, reads=small, writes=[t1])
                    S.i("dve", "tensor_tensor", t2[:], oa(x2, col), ob(y2, col), ALU.mult, reads=small, writes=[t2])
                    S.i("dve", "tensor_tensor", tv(j), t1[:], t2[:], op, reads=[t1, t2], writes=[tab])
                S.dma("sp", self.ST_d[col], tab[:], reads=[tab], writes=[self.ST_d], semtile=tab)
            S.barrier()

    def s5(self, b, uS, mixT, ph):
        S, I = self.S, self.I
        T_ = NCTX + NLAT
        dsk = S.sbuf([128, 4], F32, "dsk", ph)
        self.load(dsk, I["dskT"][:], I["dskT"])
        gw = S.sbuf([128, 4, 512], BF16, "gw", ph)
        self.load(gw, I["glu_w"][:, :].rearrange("(k p) n -> p k n", p=128), I["glu_w"], q="pool")
        gb = S.sbuf([128, 4], F32, "gb", ph)
        self.load(gb, I["glu_bT"][:], I["glu_bT"])
        diagD = S.sbuf([128, 128], BF16, "diagD", ph)
        gT = [S.sbuf([128, NLAT], BF16, "gT%d" % i, ph) for i in range(4)]
        B = [S.sbuf([128, T_], F32, "s5B%d" % i, ph) for i in range(6)]
        Mb = S.sbuf([128, 4, NLAT], BF16, "s5M", ph)
        tabs = [S.sbuf([128, 4, T_], BF16, "s5tb%d" % i, ph) for i in range(2)]
        BDt = [S.sbuf([128, 2, 128], BF16, "BDt%d" % i, ph) for i in range(2)]
        CDt = [S.sbuf([128, 3, 128], BF16, "CDt%d" % i, ph) for i in range(2)]
        cblks = [(c0, min(512, T_ - c0)) for c0 in range(0, T_, 512)]
        it = 0
        order5 = [(d_, st_) for ft_ in range(4) for d_ in range(2) for st_ in range(4 * ft_, 4 * ft_ + 4)]

        def ld5(i_):
            d_, st_ = order5[i_]
            bd_, cd_, tab_ = BDt[i_ % 2], CDt[i_ % 2], tabs[i_ % 2]
            self.load(tab_, self.ST_d[d_ * 16 + st_], self.ST_d)
            for ri in range(2):
                self.load(bd_, I["s5BD"][ri, d_, st_], I["s5BD"], q="pool", dst_ap=bd_[:, ri, :])
                self.load(cd_, I["s5CD"][ri, d_, st_], I["s5CD"], q="pool", dst_ap=cd_[:, ri, :])
            S.i("pool", "tensor_single_scalar", cd_[:, 1, :], cd_[:, 1, :], -1.0, ALU.mult, reads=[cd_], writes=[cd_])
            S.i("pool", "tensor_single_scalar", cd_[:, 2, :], cd_[:, 0, :], -1.0, ALU.mult, reads=[cd_], writes=[cd_])

        def emit_bu(i_):
            d_, st_ = order5[i_]
            bd_ = BDt[i_ % 2]
            ft_ = st_ // 4
            for (c0, n) in cblks:
                pr = self.ps()
                pi = self.ps()
                S.mm(pr, pr[:, :n], bd_, bd_[:, 0, :], uS[d_], uS[d_][:, ft_, c0:c0 + n])
                S.mm(pi, pi[:, :n], bd_, bd_[:, 1, :], uS[d_], uS[d_][:, ft_, c0:c0 + n])
                S.i("act", "activation", B[0][:, c0:c0 + n], pr[:, :n], AF.Copy, reads=[pr], writes=[B[0]])
                S.i("act", "activation", B[1][:, c0:c0 + n], pi[:, :n], AF.Copy, reads=[pi], writes=[B[1]])

        for ft in range(4):
            yb = [self.ps(reserve=True) for _ in range(4)]
            S.i("dve", "tensor_single_scalar", diagD[:], self.ident[:], dsk[:, ft:ft + 1], ALU.mult, reads=[self.ident, dsk], writes=[diagD])
            for q4 in range(4):
                S.mm(yb[q4], yb[q4][:, :], diagD, diagD[:], uS[0], uS[0][:, ft, NCTX + 512 * q4:NCTX + 512 * (q4 + 1)], start=True, stop=False)
            for d in range(2):
                for st in range(4 * ft, 4 * ft + 4):
                    col = d * 16 + st
                    bd, cd, tab = BDt[it % 2], CDt[it % 2], tabs[it % 2]
                    if it == 0:
                        ld5(0)
                    if it + 1 < len(order5):
                        ld5(it + 1)
                    if it == 0:
                        emit_bu(0)
                    S.i("dve", "tensor_tensor", B[2][:], B[0][:], tab[:, 0, :], ALU.mult, reads=[B[0], tab], writes=[B[2]])
                    S.i("dve", "tensor_tensor", B[3][:], B[1][:], tab[:, 1, :], ALU.mult, reads=[B[1], tab], writes=[B[3]])
                    S.i("dve", "tensor_tensor", B[4][:], B[1][:], tab[:, 0, :], ALU.mult, reads=[B[1], tab], writes=[B[4]])
                    S.i("dve", "tensor_tensor", B[5][:], B[0][:], tab[:, 1, :], ALU.mult, reads=[B[0], tab], writes=[B[5]])
                    S.i("dve", "tensor_tensor", B[2][:], B[2][:], B[3][:], ALU.subtract, reads=[B[2], B[3]], writes=[B[2]])
                    S.i("dve", "tensor_tensor", B[4][:], B[4][:], B[5][:], ALU.add, reads=[B[4], B[5]], writes=[B[4]])
                    if it + 1 < len(order5):
                        emit_bu(it + 1)
                    it += 1
                    rm = self.RMAG[:, col:col + 1].to_broadcast([128, T_])
                    if d == 0:
                        S.i("dve", "tensor_tensor_scan", B[3][:], rm, B[2][:], 0.0, ALU.mult, ALU.add, reads=[B[2], self.RMAG], writes=[B[3]])
                        S.i("dve", "tensor_tensor_scan", B[5][:], rm, B[4][:], 0.0, ALU.mult, ALU.add, reads=[B[4], self.RMAG], writes=[B[5]])
                    else:
                        S.i("dve", "tensor_tensor_scan", B[3][:, ::-1], rm, B[2][:, ::-1], 0.0, ALU.mult, ALU.add, reads=[B[2], self.RMAG], writes=[B[3]])
                        S.i("dve", "tensor_tensor_scan", B[5][:, ::-1], rm, B[4][:, ::-1], 0.0, ALU.mult, ALU.add, reads=[B[4], self.RMAG], writes=[B[5]])
                    l0 = NCTX if d == 0 else 0
                    ls = slice(l0, l0 + NLAT)
                    S.i("dve", "tensor_tensor", Mb[:, 0, :], B[3][:, ls], tab[:, 2, ls], ALU.mult, reads=[B[3], tab], writes=[Mb])
                    S.i("dve", "tensor_tensor", Mb[:, 1, :], B[5][:, ls], tab[:, 3, ls], ALU.mult, reads=[B[5], tab], writes=[Mb])
                    S.i("dve", "tensor_tensor", Mb[:, 2, :], B[5][:, ls], tab[:, 2, ls], ALU.mult, reads=[B[5], tab], writes=[Mb])
                    S.i("dve", "tensor_tensor", Mb[:, 3, :], B[3][:, ls], tab[:, 3, ls], ALU.mult, reads=[B[3], tab], writes=[Mb])
                    last = (d == 1 and st == 4 * ft + 3)
                    for q4 in range(4):
                        cs_ = slice(512 * q4, 512 * (q4 + 1))
                        S.mm(yb[q4], yb[q4][:, :], cd, cd[:, 0, :], Mb, Mb[:, 0, cs_], start=False, stop=False)
                        S.mm(yb[q4], yb[q4][:, :], cd, cd[:, 2, :], Mb, Mb[:, 1, cs_], start=False, stop=False)
                        S.mm(yb[q4], yb[q4][:, :], cd, cd[:, 1, :], Mb, Mb[:, 2, cs_], start=False, stop=False)
                        S.mm(yb[q4], yb[q4][:, :], cd, cd[:, 1, :], Mb, Mb[:, 3, cs_], start=False, stop=last)
            for q4 in range(4):
                xs_, x2_ = B[2][:, 512 * q4:512 * (q4 + 1)], B[4][:, 512 * q4:512 * (q4 + 1)]
                S.i("act", "activation", xs_, yb[q4][:, :], AF.Copy, reads=[yb[q4]], writes=[B[2]])
                S.i("dve", "tensor_tensor", x2_, xs_, xs_, ALU.mult, reads=[B[2]], writes=[B[4]])
                S.i("dve", "tensor_scalar", x2_, x2_, 0.044715, 1.0, ALU.mult, ALU.add, reads=[B[4]], writes=[B[4]])
                S.i("dve", "tensor_tensor", x2_, x2_, xs_, ALU.mult, reads=[B[2], B[4]], writes=[B[4]])
                S.i("act", "activation", x2_, x2_, AF.Tanh, scale=math.sqrt(2.0 / math.pi), reads=[B[4]], writes=[B[4]])
                S.i("dve", "tensor_scalar", x2_, x2_, 1.0, 0.5, ALU.add, ALU.mult, reads=[B[4]], writes=[B[4]])
                S.i("dve", "tensor_tensor", gT[ft][:, 512 * q4:512 * (q4 + 1)], x2_, xs_, ALU.mult, reads=[B[2], B[4]], writes=[gT[ft]])
                self.ps_free(yb[q4])
        sgm = [S.sbuf([128, 512], BF16, "sgm%d" % i, ph) for i in range(2)]
        for f2 in range(4):
            for q4 in range(4):
                ps = self.ps()
                for ft in range(4):
                    S.mm(ps, ps[:, :], gw, gw[:, ft, f2 * 128:(f2 + 1) * 128], gT[ft], gT[ft][:, 512 * q4:512 * (q4 + 1)], start=(ft == 0), stop=(ft == 3))
                sg_ = sgm[(f2 * 4 + q4) % 2]
                S.i("act", "activation", sg_[:], ps[:, :], AF.Sigmoid, bias=gb[:, f2:f2 + 1], reads=[ps, gb], writes=[sg_])
                S.i("dve", "tensor_tensor", mixT[4 + f2][:, L0 + 512 * q4:L0 + 512 * (q4 + 1)], sg_[:], gT[f2][:, 512 * q4:512 * (q4 + 1)], ALU.mult, reads=[sg_, gT[f2]], writes=[mixT[4 + f2]])

    def mla(self, b, cqn, ckvn, KR, mixT, ph):
        S, I = self.S, self.I
        wuq = S.sbuf([128, 2, 768], BF16, "wuq", ph)
        self.load(wuq, I["o_w_uq"][:, :].rearrange("(k p) n -> p k n", p=128), I["o_w_uq"], q="pool")
        wukv = S.sbuf([128, 1024], BF16, "wukv", ph)
        self.load(wukv, I["o_w_ukv"][:, :], I["o_w_ukv"], q="pool")
        cos = S.sbuf([128, NLAT], F32, "cos", ph)
        sin = S.sbuf([128, NLAT], F32, "sin", ph)
        self.load(cos, I["cos_o"][:], I["cos_o"])
        self.load(sin, I["sin_o"][:], I["sin_o"])
        rotT = S.sbuf([128, 128], BF16, "rotT", ph)
        self.load(rotT, I["rotT_o"][:], I["rotT_o"])
        VA = S.sbuf([128, 18, 8, 128], BF16, "VA1", ph)
        S.i("pool", "memset", VA[:, :, :, 64:128], 1.0, writes=[VA])
        wv = wukv[:, :].rearrange("k (h t d) -> k h t d", h=8, t=2)
        for ti, (s, kind, _) in enumerate(TTILES):
            pv = self.ps()
            S.mm(pv, pv[:, :].rearrange("p (h d) -> p h d", h=8), ckvn, ckvn[:, s:s + 128], wukv, wv[:, :, 1, :])
            S.i("act", "activation", VA[:, ti, :, 0:64], pv[:, :].rearrange("p (h d) -> p h d", h=8), AF.Copy, reads=[pv], writes=[VA])
        Qh = [S.sbuf([128, NLAT], BF16, "Qh%d" % i, ph) for i in range(2)]
        Kh = [S.sbuf([128, W], BF16, "Kh%d" % i, ph) for i in range(2)]
        qn = S.sbuf([128, 512], BF16, "mqn", ph)
        t1 = S.sbuf([128, 512], F32, "mt1", ph)
        t2 = S.sbuf([128, 512], F32, "mt2", ph)
        T = {"pT": [S.sbuf([128, 512], BF16, "pT%d" % i, ph) for i in range(6)], "den": [S.sbuf([64, 512], F32, "den%d" % i, ph) for i in range(2)]}

        def kcol(kt):
            return (C0 + 128 * kt) if kt < 2 else (L0 + 128 * (kt - 2))

        for hp in range(4):
          for h in (2 * hp, 2 * hp + 1):
            Q, K = Qh[h % 2], Kh[h % 2]
            for (s, n, kind) in BLKS:
                pk = self.ps()
                S.mm(pk, pk[0:64, :n], wukv, wukv[:, h * 128:h * 128 + 64], ckvn, ckvn[:, s:s + n])
                S.i("act", "activation", K[0:64, s:s + n], pk[0:64, :n], AF.Copy, reads=[pk], writes=[K])
                S.i("pool", "tensor_copy", K[64:96, s:s + n], KR[64:96, s:s + n], reads=[KR], writes=[K])
            for qb in range(4):
                s = L0 + 512 * qb
                pq = self.ps()
                for j in range(2):
                    S.mm(pq, pq[0:96, :], wuq, wuq[:, j, h * 96:(h + 1) * 96], cqn, cqn[:, j, s:s + 512], start=(j == 0), stop=(j == 1))
                S.i("act", "activation", qn[0:96, :], pq[0:96, :], AF.Copy, reads=[pq], writes=[qn])
                p3 = self.ps()
                S.mm(p3, p3[0:96, :], rotT, rotT[0:96, 0:96], qn, qn[0:96, :])
                S.i("pool", "tensor_copy", Q[0:64, 512 * qb:512 * (qb + 1)], qn[0:64, :], reads=[qn], writes=[Q])
                S.i("pool", "tensor_tensor", t1[64:96, :], qn[64:96, :], cos[64:96, 512 * qb:512 * (qb + 1)], ALU.mult, reads=[qn, cos], writes=[t1])
                S.i("dve", "tensor_tensor", t2[64:96, :], p3[64:96, :], sin[64:96, 512 * qb:512 * (qb + 1)], ALU.mult, reads=[p3, sin], writes=[t2])
                S.i("dve", "tensor_tensor", Q[64:96, 512 * qb:512 * (qb + 1)], t1[64:96, :], t2[64:96, :], ALU.add, reads=[t1, t2], writes=[Q])
          out_t = mixT[hp]
          for qb in range(4):
            s = L0 + 512 * qb
            streams = []
            for h in (2 * hp, 2 * hp + 1):
                Q, K = Qh[h % 2], Kh[h % 2]
                po = (h % 2) * 64
                streams.append({"QTt": Q, "Qap": Q[0:96, 512 * qb:512 * (qb + 1)], "KTt": K,
                                "Kap": (lambda kt, K=K: K[0:96, kcol(kt):kcol(kt) + 128]),
                                "Vt": VA, "Vap": (lambda kt, h=h: VA[:, kt, h, :]), "out_t": out_t, "out_ap": out_t[po:po + 64, s:s + 512]})
            self.attn_multi(streams, 18, 512, 96 ** -0.5, T)


_HC = None


def get_hc():
    global _HC
    if _HC is None:
        _HC = host_consts()
    return _HC


def _dt_of(a):
    return BF16 if a.dtype == ml_dtypes.bfloat16 else F32


HC_SHAPES = {k: (list(v.shape), _dt_of(v)) for k, v in get_hc().items()}
DBG_SHAPES = {
    "MV": ([128, 2, 6, 8, 4], F32),
    "hT": ([8, 128, W], BF16),
    "HY": ([3, NCTX + NLAT, 512], BF16),
    "QT": ([4, 128, W], BF16),
    "KK": ([2, 128, W], BF16),
    "VA": ([128, 18, 2, 128], BF16),
    "mixT": ([8, 128, W], BF16),
    "Hf_l": ([2, 16, 128, 2, 512], F32),
    "Hf_c": ([2, 2, 128, 2, 512], F32),
    "XT": ([D, W], F32),
    "G": ([32, W], BF16),
}


def fm(v):
    return np.ascontiguousarray(np.asarray(v, np.float32).reshape(8, 128).T)


def host_prep(inputs, core):
    b0 = 2 * core
    P = {}
    x, ctx, c = inputs["x"], inputs["ctx"], inputs["c"]
    P["xT"] = _f(np.stack([np.concatenate([ctx[b].T, x[b].T], axis=1) for b in (b0, b0 + 1)]))
    P["cT"] = _f(np.stack([fm(c[b0]), fm(c[b0 + 1]), fm(inputs["c_ctx"]), fm(inputs["c_ctx"])], axis=-1))
    return P


def host_shared(inputs):
    Sh = {}
    Sh["w_mod"] = _f(inputs["w_mod"])
    Sh["bmodT"] = _f(inputs["b_mod"].reshape(2, 48, 128).transpose(2, 0, 1))
    Sh["ngT"] = _f(inputs["norm_g"].reshape(2, 2, 8, 128).transpose(3, 0, 1, 2))
    Sh["e_w_in"] = _f(inputs["e_w_in"][0])
    Sh["convw"] = _f(inputs["e_hy_conv_w"][0])
    Sh["convb"] = _f(inputs["e_hy_conv_b"][0][None])
    Sh["hy_w1"] = _f(inputs["e_hy_w1"][0])
    Sh["hy_w2"] = _f(inputs["e_hy_w2"][0])
    Sh["hy_w3"] = _f(inputs["e_hy_w3"][0])
    Sh["hy_vec"] = _f(np.stack([inputs["e_hy_b1"][0], inputs["e_hy_b2"][0], inputs["e_hy_freq"][0]], -1))
    Sh["fbias"] = _f(inputs["e_hy_fbias"][0])
    qkg = inputs["e_qk_g"][0]
    Sh["qkg"] = _f(np.stack([np.tile(qkg[0], 2), np.tile(qkg[1], 2)], -1))
    Sh["e_w_out"] = _f(inputs["e_w_out"][0])
    wr = np.concatenate([inputs["moe_w_rg"], inputs["moe_w_re"]], -1)
    Sh["wr"] = _f(wr.reshape(2, 8, 128, 36).transpose(2, 0, 1, 3))
    Sh["moe_w_gate"] = _f(inputs["moe_w_gate"])
    Sh["moe_w_up"] = _f(inputs["moe_w_up"])
    Sh["moe_w_down"] = _f(inputs["moe_w_down"])
    Sh["finalgT"] = fm(inputs["final_g"])
    Sh["o_w_in"] = _f(inputs["o_w_in"][0])
    qg = inputs["o_q_norm_g"][0]
    Sh["o_ng"] = _f(np.stack([qg[:128], qg[128:], inputs["o_kv_norm_g"][0]], -1))
    Sh["o_w_uq"] = _f(inputs["o_w_uq"][0])
    Sh["o_w_ukv"] = _f(inputs["o_w_ukv"][0])
    Sh["o_w_out"] = _f(inputs["o_w_out"][0])
    def st_layout(a):
        return a.reshape(2, 16, 2, 64).transpose(2, 3, 0, 1).reshape(128, 32)
    ldt = np.broadcast_to(inputs["o_s5_log_dt"][0][:, :, None], (2, 32, 64))
    Sh["s5p"] = _f(np.stack([st_layout(inputs["o_s5_a_re"][0]), st_layout(inputs["o_s5_a_im"][0]), st_layout(ldt)], 1))
    BD = np.zeros((2, 2, 16, 128, 128), np.float32)
    CD = np.zeros((2, 2, 16, 128, 128), np.float32)
    for ri, (bb, cc) in enumerate(((inputs["o_s5_b_re"][0], inputs["o_s5_c_re"][0]), (inputs["o_s5_b_im"][0], inputs["o_s5_c_im"][0]))):
        for d_ in range(2):
            for st in range(16):
                for gg in range(2):
                    g = 2 * st + gg
                    r0 = (g % 8) * 16
                    BD[ri, d_, st, r0:r0 + 16, gg * 64:(gg + 1) * 64] = bb[d_, g].T
                    CD[ri, d_, st, gg * 64:(gg + 1) * 64, r0:r0 + 16] = cc[d_, g].T
    Sh["s5BD"] = BD
    Sh["s5CD"] = CD
    Sh["dskT"] = _f(inputs["o_s5_d"][0].reshape(4, 128).T)
    Sh["glu_w"] = _f(inputs["o_glu_w"][0])
    Sh["glu_bT"] = _f(inputs["o_glu_b"][0].reshape(4, 128).T)
    Sh.update(get_hc())
    return Sh


def run(inputs, stop="all", dbg=(), cores=8):
    prog = Prog(stop, dbg)
    nc = prog.build()
    sh = host_shared(inputs)
    in_maps = []
    for core in range(cores):
        m = dict(sh)
        m.update(host_prep(inputs, core))
        in_maps.append({k: m[k] for k in prog.in_names})
    res = run_bass_kernel_spmd(nc, in_maps, core_ids=list(range(cores)))
    return prog, res


def kernel(**inputs):
    inputs = {k: np.asarray(v) for k, v in inputs.items()}
    prog, res = run(inputs)
    out = np.empty((16, NLAT, D), np.float32)
    for core in range(8):
        o = res.results[core]["outT"]
        for j in range(2):
            out[2 * core + j] = o[j].T
    return out
```

```python
import contextlib
import math
import numpy as np
import ml_dtypes
import concourse.bass as bass
import concourse.mybir as mybir
from concourse.bass_utils import run_bass_kernel_spmd

F32 = mybir.dt.float32
BF16 = mybir.dt.bfloat16
AF = mybir.ActivationFunctionType
ALU = mybir.AluOpType
AX = mybir.AxisListType


class Tile:
    def __init__(self, t, name=""):
        self.t = t
        self.name = name
        self.w = {}
        self.r = {}
        self.dkey = None

    def __getitem__(self, k):
        return self.t[k]


class Sched:
    ENG = ("pe", "act", "dve", "pool", "sp")

    def __init__(self, nc, stack):
        self.nc = nc
        self.stack = stack
        self.streams = {e: [] for e in self.ENG}
        self.cnt = {e: 0 for e in self.ENG}
        self.sems = {}
        self.known = {e: {} for e in self.ENG}
        for e in self.ENG:
            self.sems[e] = stack.enter_context(nc.semaphore("s_" + e))
        self.dfree = []
        self.dtiles = []
        self.dcount = {}
        self.ndsem = 0
        self.n_inst = 0
        self.uid = 0
        self.use_dummy = False

    def sbuf(self, shape, dtype, name, stack=None):
        self.uid += 1
        t = (stack or self.stack).enter_context(self.nc.sbuf_tensor("%s_%d" % (name, self.uid), list(shape), dtype))
        try:
            rem = self.nc.sbuf_bytes_remaining
            if rem < getattr(self, "min_rem", 1 << 60):
                self.min_rem = rem
                self.min_rem_at = name
        except Exception:
            pass
        return Tile(t, name)

    def psum(self, shape, dtype, name, stack=None):
        self.uid += 1
        t = (stack or self.stack).enter_context(self.nc.psum_tensor("%s_%d" % (name, self.uid), list(shape), dtype))
        return Tile(t, name)

    def dram(self, name, shape, dtype, kind="Internal"):
        t = self.nc.dram_tensor(name, list(shape), dtype, kind=kind)
        return Tile(t.ap(), name)

    def _dkey(self, tile):
        if tile.dkey is None:
            if self.dfree:
                tile.dkey = self.dfree.pop()
            else:
                key = "d%d" % self.ndsem
                self.ndsem += 1
                self.sems[key] = self.stack.enter_context(self.nc.semaphore("s_" + key))
                self.dcount[key] = 0
                tile.dkey = key
            self.dtiles.append(tile)
        return tile.dkey

    def release(self, tiles):
        for t in tiles:
            if t.dkey is not None:
                self.dfree.append(t.dkey)
                t.dkey = None

    def _waits(self, eng, reads, writes):
        need = {}

        def add(kv):
            if kv is None:
                return
            k, v = kv
            if k == eng and eng == "pe":
                return
            if need.get(k, 0) < v:
                need[k] = v

        for d in reads:
            for kv in d.w.items():
                add(kv)
        for d in writes:
            for kv in d.w.items():
                add(kv)
            for k, v in d.r.items():
                if k == eng:
                    continue
                add((k, v))
        out = []
        kn = self.known[eng]
        for k, v in need.items():
            if kn.get(k, 0) >= v:
                continue
            kn[k] = v
            out.append((k, v))
        return out

    def op(self, eng, fn, reads=(), writes=(), dummy=None):
        waits = self._waits(eng, reads, writes)
        self.cnt[eng] += 1
        val = self.cnt[eng]
        sem = self.sems[eng]
        sems = self.sems

        def emit(e, waits=waits, fn=fn, sem=sem):
            for k, v in waits:
                e.wait_ge(sems[k], v)
            if waits and dummy is not None and self.use_dummy:
                dummy(e)
                dummy(e)
            fn(e).then_inc(sem, 1)

        self.streams[eng].append(emit)
        for d in reads:
            if d.r.get(eng, 0) < val:
                d.r[eng] = val
        for d in writes:
            d.w[eng] = val
            d.r = {}
        self.n_inst += 1

    def i(self, eng, method, *args, reads=(), writes=(), **kw):
        def fn(e, method=method, args=args, kw=kw):
            return getattr(e, method)(*args, **kw)
        self.op(eng, fn, reads=reads, writes=writes)

    def mm(self, out_t, out_ap, lhs_t, lhs_ap, rhs_t, rhs_ap, start=True, stop=True, extra_reads=()):
        reads = [lhs_t, rhs_t] + list(extra_reads)
        M, N = out_ap.shape[0], out_ap.shape[-1]
        pd = self.pdummy

        def dummy(e):
            e.matmul(pd[0:M, 0:N], lhs_ap, rhs_ap, start=True, stop=True)

        self.op("pe", lambda e: e.matmul(out_ap, lhs_ap, rhs_ap, start=start, stop=stop), reads=reads, writes=[out_t], dummy=dummy)

    def tr(self, out_t, out_ap, in_t, in_ap, ident_t, ident_ap):
        pd = self.pdummy_bf if in_ap.dtype == BF16 else self.pdummy
        M, N = out_ap.shape[0], out_ap.shape[-1]

        def dummy(e):
            e.transpose(pd[0:M, 0:N], in_ap, ident_ap)

        self.op("pe", lambda e: e.transpose(out_ap, in_ap, ident_ap), reads=[in_t, ident_t], writes=[out_t], dummy=dummy)

    def dma(self, q, out_ap, in_ap, reads=(), writes=(), semtile=None, **kw):
        if semtile is None:
            semtile = writes[0]
        key = self._dkey(semtile)
        waits = self._waits(q, reads, writes)
        self.dcount[key] += 16
        val = self.dcount[key]
        sems = self.sems

        def emit(e, waits=waits):
            for k, v in waits:
                e.wait_ge(sems[k], v)
            e.dma_start(out=out_ap, in_=in_ap, **kw).then_inc(sems[key], 16)

        self.streams[q].append(emit)
        for d in reads:
            if d.r.get(key, 0) < val:
                d.r[key] = val
        for d in writes:
            d.w[key] = val
            d.r = {}
        self.n_inst += 1

    def barrier(self):
        targets = {e: self.cnt[e] for e in self.ENG if self.cnt[e] > 0}
        for k, v in self.dcount.items():
            if v > 0:
                targets[k] = v
        sems = self.sems
        for eng in self.ENG:
            kn = self.known[eng]
            ws = []
            for k, v in targets.items():
                if k == eng:
                    continue
                if kn.get(k, 0) >= v:
                    continue
                kn[k] = v
                ws.append((k, v))

            def emit(e, ws=ws):
                for k, v in ws:
                    e.wait_ge(sems[k], v)

            self.streams[eng].append(emit)
        for t in self.dtiles:
            if t.dkey is not None:
                self.dfree.append(t.dkey)
                t.dkey = None
        self.dtiles = []

    def emit(self):
        nc = self.nc
        with nc.Block() as block:
            @block.tensor
            def _(e):
                for f in self.streams["pe"]:
                    f(e)

            @block.scalar
            def _(e):
                for f in self.streams["act"]:
                    f(e)

            @block.vector
            def _(e):
                for f in self.streams["dve"]:
                    f(e)

            @block.gpsimd
            def _(e):
                for f in self.streams["pool"]:
                    f(e)

            @block.sync
            def _(e):
                for f in self.streams["sp"]:
                    f(e)
import os

D = 1024
NCTX = 256
NLAT = 2048
C0 = 2
L0 = 260
W = 2310
EPS = 1e-6
BLKS = [(C0, 256, "c")] + [(L0 + 512 * i, 512, "l") for i in range(4)]
TTILES = [(C0 + 128 * i, "c", i) for i in range(2)] + [(L0 + 128 * i, "l", i) for i in range(16)]
TWO_PI = 2.0 * math.pi


def _bf(a):
    return np.ascontiguousarray(a.astype(ml_dtypes.bfloat16))


def _f(a):
    return np.ascontiguousarray(a, dtype=np.float32)


def host_consts():
    Cn = {}
    Cn["ident"] = np.eye(128, dtype=np.float32)
    Cn["ones"] = np.ones((128, 128), np.float32)
    bo = np.zeros((128, 128), np.float32)
    bo[:64, :64] = 1.0
    bo[64:, 64:] = 1.0
    Cn["blockones"] = bo
    t = np.arange(NLAT)
    rows = (t // 64).astype(np.float32)
    cols = (t % 64).astype(np.float32)

    def rope_tab(dh):
        a = dh // 2
        half = a // 2
        freqs = (10000.0 ** (-np.arange(half, dtype=np.float32) / half)).astype(np.float32)
        cos = np.zeros((dh, NLAT), np.float32)
        sin = np.zeros((dh, NLAT), np.float32)
        R = np.zeros((dh, dh), np.float32)
        for d in range(dh):
            part = d // a
            i = d % a
            fi = i % half
            pos = rows if part == 0 else cols
            ang = (pos * freqs[fi]).astype(np.float32)
            cos[d] = np.cos(ang)
            sin[d] = np.sin(ang)
            if i < half:
                R[d, d + half] = -1.0
            else:
                R[d, d - half] = 1.0
        return cos, sin, R

    cos64, sin64, R64 = rope_tab(64)
    Cn["cos_e"] = np.concatenate([cos64, cos64], 0)
    Cn["sin_e"] = np.concatenate([sin64, sin64], 0)
    Rm = np.zeros((128, 128), np.float32)
    Rm[:64, :64] = R64
    Rm[64:, 64:] = R64
    Cn["rotT_e"] = _bf(Rm.T)
    cos32, sin32, R32 = rope_tab(32)
    co = np.zeros((128, NLAT), np.float32)
    so = np.zeros((128, NLAT), np.float32)
    co[64:96] = cos32
    so[64:96] = sin32
    Cn["cos_o"] = co
    Cn["sin_o"] = so
    Ro = np.zeros((128, 128), np.float32)
    Ro[64:96, 64:96] = R32
    Cn["rotT_o"] = _bf(Ro.T)
    for n, tag in ((NLAT, "l"), (NCTX, "c")):
        NT = n // 128
        N2 = 2 * n
        tt = np.arange(n, dtype=np.float64)
        kk = np.arange(n, dtype=np.float64)
        ang = 2.0 * np.pi * np.outer(tt, kk) / N2
        fre = np.cos(ang)
        fim = -np.sin(ang)
        fim[:, 0] = np.cos(np.pi * tt)
        fwd = np.concatenate([fre, fim], 1)
        fw = fwd.reshape(NT, 128, 2, NT, 128).transpose(3, 2, 1, 0, 4)
        Cn["fwd_" + tag] = _bf(fw)
        ire = (2.0 / N2) * np.cos(ang.T)
        ire[0, :] = 1.0 / N2
        iim = -(2.0 / N2) * np.sin(ang.T)
        iim[0, :] = (1.0 / N2) * np.cos(np.pi * tt)
        inv = np.concatenate([ire, iim], 0)
        iv = inv.reshape(2 * NT, 128, NT, 128).transpose(2, 1, 0, 3)
        Cn["inv_" + tag] = _bf(iv)
        tf = np.arange(n, dtype=np.float32)
        t_norm = tf / max(n - 1, 1)
        bands = np.linspace(1e-4, 15, 16, dtype=np.float32)
        a2 = (np.float32(2.0 * math.pi / n) * tf[:, None] * bands).astype(np.float32)
        z = np.concatenate([t_norm[:, None], np.cos(a2), np.sin(a2)], -1).astype(np.float32)
        Cn["zT_" + tag] = _f(z.T)
        HMAX = math.log(100.0) / 0.3
        HMIN = math.log(100.0) / 1.5
        deltas = np.linspace(HMIN, HMAX, 512, dtype=np.float32)
        dec = np.exp(-t_norm[:, None] * deltas).astype(np.float32)
        Cn["dec_" + tag] = _f(dec.reshape(NT, 128, 512).transpose(1, 0, 2))
    kk_ = np.arange(NCTX + NLAT)
    Cn["s5k1s"] = _f(np.broadcast_to(np.arange(36, dtype=np.float32), (128, 32, 36)))
    Cn["s5k0s"] = _f(np.broadcast_to(np.arange(64, dtype=np.float32), (128, 32, 64)))
    sel = np.zeros((32, 32, 128), np.float32)
    for e in range(32):
        sel[e, e, :] = 1.0
    return Cn


class Prog:
    def __init__(self, stop="all", dbg=()):
        self.stop = stop
        self.dbg = set(dbg)
        self.nc = bass.Bass("TRN2", target_bir_lowering=False)
        self.in_names = []
        self.out_names = []

    def inp(self, name, shape, dtype=F32):
        if self.stop == "modvec" and name not in ("cT", "w_mod", "bmodT", "ngT", "ident", "ones", "blockones"):
            return None
        self.in_names.append(name)
        return self.S.dram(name, shape, dtype, kind="ExternalInput")

    def outp(self, name, shape, dtype=F32):
        self.out_names.append(name)
        return self.S.dram(name, shape, dtype, kind="ExternalOutput")

    def load(self, dst, src_ap, src, q="sp", dst_ap=None):
        self.S.dma(q, dst[:] if dst_ap is None else dst_ap, src_ap, reads=[src], writes=[dst])

    def const_tile(self, name, shape, dtype, val):
        t = self.S.sbuf(shape, dtype, name)
        self.S.i("pool", "memset", t[:], val, writes=[t])
        return t

    def build(self):
        nc = self.nc
        with contextlib.ExitStack() as st:
            S = self.S = Sched(nc, st)
            self.declare_io()
            self.setup_consts()
            self.S.barrier()
            self.modvecs()
            self.S.barrier()
            if self.stop != "modvec":
                if self.stop not in ("mn1", "stage") and not os.environ.get("KSKIP0"):
                    self.hyena_filters()
                self.s5_tables()
                for b in range(2):
                    self.batch(b)
            self.S.barrier()
            S.emit()
        return nc

    def declare_io(self):
        I = self.I = {}
        I["xT"] = self.inp("xT", [2, D, NCTX + NLAT])
        I["cT"] = self.inp("cT", [128, 8, 4])
        I["w_mod"] = self.inp("w_mod", [2, D, 6 * D])
        I["bmodT"] = self.inp("bmodT", [128, 2, 48])
        I["ngT"] = self.inp("ngT", [128, 2, 2, 8])
        I["e_w_in"] = self.inp("e_w_in", [D, 2304])
        I["convw"] = self.inp("convw", [3, 1536])
        I["convb"] = self.inp("convb", [1, 1536])
        I["hy_w1"] = self.inp("hy_w1", [33, 64])
        I["hy_w2"] = self.inp("hy_w2", [64, 64])
        I["hy_w3"] = self.inp("hy_w3", [64, 2048])
        I["hy_vec"] = self.inp("hy_vec", [64, 3])
        I["fbias"] = self.inp("fbias", [2, 512])
        I["qkg"] = self.inp("qkg", [128, 2])
        I["e_w_out"] = self.inp("e_w_out", [D, D])
        I["wr"] = self.inp("wr", [128, 2, 8, 36])
        I["moe_w_gate"] = self.inp("moe_w_gate", [2, 32, D, 256])
        I["moe_w_up"] = self.inp("moe_w_up", [2, 32, D, 256])
        I["moe_w_down"] = self.inp("moe_w_down", [2, 32, 256, D])
        I["finalgT"] = self.inp("finalgT", [128, 8])
        I["o_w_in"] = self.inp("o_w_in", [D, 928])
        I["o_ng"] = self.inp("o_ng", [128, 3])
        I["o_w_uq"] = self.inp("o_w_uq", [256, 768])
        I["o_w_ukv"] = self.inp("o_w_ukv", [128, 1024])
        I["o_w_out"] = self.inp("o_w_out", [D, D])
        I["s5p"] = self.inp("s5p", [128, 3, 32])
        I["s5BD"] = self.inp("s5BD", [2, 2, 16, 128, 128])
        I["s5CD"] = self.inp("s5CD", [2, 2, 16, 128, 128])
        I["dskT"] = self.inp("dskT", [128, 4])
        I["glu_w"] = self.inp("glu_w", [512, 512])
        I["glu_bT"] = self.inp("glu_bT", [128, 4])
        for k, v in HC_SHAPES.items():
            I[k] = self.inp(k, v[0], v[1])
        self.out = self.outp("outT", [2, D, NLAT])
        S = self.S
        self.XT_d = [S.dram("XT_d%d" % b, [D, W], F32) for b in range(2)]
        self.HY_d = S.dram("HY_d", [3, NCTX + NLAT, 512], BF16)
        self.G_d = S.dram("G_d", [32, W], BF16)
        self.H2_d = S.dram("H2_d", [D, W], BF16)
        self.Hf_d = {"l": S.dram("Hf_l", [2, 16, 128, 2, 512], F32), "c": S.dram("Hf_c", [2, 2, 128, 2, 512], F32)}
        self.D = {}
        for name in self.dbg:
            self.D[name] = self.outp("dbg_" + name, DBG_SHAPES[name][0], DBG_SHAPES[name][1])

    def setup_consts(self):
        S, I = self.S, self.I
        self.ident = S.sbuf([128, 128], F32, "ident")
        self.load(self.ident, I["ident"][:], I["ident"])
        self.identb = S.sbuf([128, 128], BF16, "identb")
        self.load(self.identb, I["ident"][:], I["ident"], q="pool")
        self.ones = S.sbuf([128, 128], F32, "ones")
        self.load(self.ones, I["ones"][:], I["ones"])
        self.blockones = S.sbuf([128, 128], F32, "blockones")
        self.load(self.blockones, I["blockones"][:], I["blockones"])
        S.zeros_f = self.const_tile("zeros_f", [128, 128], F32, 0.0)
        S.zeros_bf = self.const_tile("zeros_bf", [128, 128], BF16, 0.0)
        self.eps = self.const_tile("eps", [128, 1], F32, EPS)
        self.negpi = self.const_tile("negpi", [128, 1], F32, -math.pi)
        self.MV = S.sbuf([128, 2, 6, 8, 4], F32, "MV")
        self.PS = [S.psum([128, 512], F32, "ps%d" % i) for i in range(7)]
        pdt = st_psum = S.psum([128, 512], F32, "psdummy")
        S.pdummy = pdt.t
        S.pdummy_bf = pdt[:, :].bitcast(BF16)
        self.psi = 0
        self.ps_res = set()

    def ps(self, reserve=False):
        while True:
            i = self.psi % 7
            self.psi += 1
            if i not in self.ps_res:
                break
        if reserve:
            self.ps_res.add(i)
        return self.PS[i]

    def ps_free(self, p):
        self.ps_res.discard(self.PS.index(p))

    def modvecs(self):
        S, I = self.S, self.I
        with contextlib.ExitStack() as ph:
            cT = S.sbuf([128, 8, 4], F32, "cT", ph)
            self.load(cT, I["cT"][:], I["cT"])
            scT = S.sbuf([128, 8, 4], F32, "scT", ph)
            S.i("act", "activation", scT[:], cT[:], AF.Silu, reads=[cT], writes=[scT])
            bm = S.sbuf([128, 2, 48], F32, "bm", ph)
            self.load(bm, I["bmodT"][:], I["bmodT"])
            ng = S.sbuf([128, 2, 2, 8], F32, "ng", ph)
            self.load(ng, I["ngT"][:], I["ngT"])
            wm = [S.sbuf([128, 8, 512], F32, "wm%d" % i, ph) for i in range(2)]
            modT = S.sbuf([128, 48, 4], F32, "modT", ph)
            MV = self.MV
            for i in range(2):
                pm = self.ps()
                pmv = pm[:, 0:192].rearrange("p (m n) -> p m n", n=4)
                wsrc = I["w_mod"][i].rearrange("(k p) n -> p k n", p=128)
                for cb in range(12):
                    w = wm[cb % 2]
                    self.load(w, wsrc[:, :, cb * 512:(cb + 1) * 512], I["w_mod"])
                    for mm in range(4):
                        m = cb * 4 + mm
                        for k in range(8):
                            S.mm(pm, pmv[:, m, :], w, w[:, k, mm * 128:(mm + 1) * 128], scT, scT[:, k, :], start=(k == 0), stop=(k == 7))
                S.i("dve", "tensor_copy", modT[:].rearrange("p m n -> p (m n)"), pm[:, 0:192], reads=[pm], writes=[modT])
                for n_ in range(4):
                    S.i("dve", "tensor_tensor", modT[:, :, n_], modT[:, :, n_], bm[:, i, :], ALU.add, reads=[modT, bm], writes=[modT])
                for kind, lo in ((0, 0), (2, 16), (3, 24), (5, 40)):
                    S.i("dve", "tensor_copy", MV[:, i, kind], modT[:, lo:lo + 8, :], reads=[modT], writes=[MV])
                for kind, lo, w_ in ((1, 8, 0), (4, 32, 1)):
                    S.i("dve", "tensor_single_scalar", MV[:, i, kind], modT[:, lo:lo + 8, :], 1.0, ALU.add, reads=[modT], writes=[MV])
                    for n_ in range(4):
                        S.i("dve", "tensor_tensor", MV[:, i, kind, :, n_], MV[:, i, kind, :, n_], ng[:, i, w_, :], ALU.mult, reads=[MV, ng], writes=[MV])
            if "MV" in self.D:
                S.dma("sp", self.D["MV"][:], MV[:], reads=[MV], writes=[self.D["MV"]], semtile=MV)
            S.barrier()

    def mv(self, layer, kind, c, n):
        return self.MV[:, layer, kind, c, n:n + 1]

    def modnorm_block(self, xt, n, layer, kindA, kindS, ncol, outs, otiles, h32=None):
        S = self.S
        T = self._mn[self._mni % len(self._mn)]
        self._mni += 1
        sq, rstd, tmp = T["sq"], T["rstd"], T["tmp"]
        ss = self.ps()
        S.i("act", "activation", sq[:, :, :n], xt[:, :, :n], AF.Square, reads=[xt], writes=[sq])
        for c in range(8):
            S.mm(ss, ss[:, :n], self.ones, self.ones[:], sq, sq[:, c, :n], start=(c == 0), stop=(c == 7))
        S.i("act", "activation", rstd[:, :n], ss[:, :n], AF.Sqrt, bias=self.eps[:], scale=1.0 / D, reads=[ss, self.eps], writes=[rstd])
        S.i("dve", "reciprocal", rstd[:, :n], rstd[:, :n], reads=[rstd], writes=[rstd])
        for c in range(8):
            S.i("dve", "scalar_tensor_tensor", tmp[:, c, :n], xt[:, c, :n], self.mv(layer, kindA, c, ncol), rstd[:, :n], ALU.mult, ALU.mult,
                reads=[xt, self.MV, rstd], writes=[tmp])
        for c in range(8):
            S.i("act", "activation", outs[c], tmp[:, c, :n], AF.Identity, bias=self.mv(layer, kindS, c, ncol), reads=[tmp, self.MV], writes=[otiles[c]])
            if h32 is not None:
                S.i("act", "activation", h32[:, c, :n], tmp[:, c, :n], AF.Identity, bias=self.mv(layer, kindS, c, ncol), reads=[tmp, self.MV], writes=[h32])

    def mn_alloc(self, ph, nbuf=1):
        S = self.S
        self._mni = 0
        self._mn = [{"sq": S.sbuf([128, 8, 512], F32, "mn_sq", ph), "rstd": S.sbuf([128, 512], F32, "mn_rstd", ph),
                     "tmp": S.sbuf([128, 8, 512], F32, "mn_tmp", ph)} for _ in range(nbuf)]

    def dump(self, name, src_tile, src_ap, dst_ap=None):
        if name in self.D and getattr(self, "cur_b", 0) == 0:
            d = self.D[name]
            self.S.dma("sp", d[:] if dst_ap is None else dst_ap(d), src_ap, reads=[src_tile], writes=[d], semtile=d)

    def batch(self, b):
        S, I = self.S, self.I
        self.cur_b = b
        XT = self.XT_d[b]
        S.dma("sp", XT[:, C0:C0 + NCTX], I["xT"][b, :, 0:NCTX], reads=[I["xT"]], writes=[XT], semtile=XT)
        S.dma("sp", XT[:, L0:L0 + NLAT], I["xT"][b, :, NCTX:], reads=[I["xT"]], writes=[XT], semtile=XT)
        S.barrier()
        if self.stop == "stage":
            self.dump("XT", XT, XT[:])
            return
        if not os.environ.get("KSKIP0"):
            self.layer0(b)
        S.barrier()
        if self.stop in ("l0", "mn1", "inproj", "mix0", "xa0"):
            return
        self.layer1(b)
        S.barrier()

    def xt_view(self, b):
        return self.XT_d[b][:, :].rearrange("(k p) w -> p k w", p=128)

    def layer0(self, b):
        S, I = self.S, self.I
        XTv = self.xt_view(b)
        XT = self.XT_d[b]
        with contextlib.ExitStack() as LY:
            with contextlib.ExitStack() as L1:
                hT = [S.sbuf([128, W], BF16, "hT%d" % c, L1) for c in range(8)]
                for c in range(8):
                    for (a0, a1) in ((0, C0), (C0 + NCTX, L0), (L0 + NLAT, W)):
                        S.i("pool", "memset", hT[c][:, a0:a1], 0.0, writes=[hT[c]])
                with contextlib.ExitStack() as L2:
                    QT = [S.sbuf([128, W], BF16, "QT%d" % j, L2) for j in range(4)]
                    KK = [S.sbuf([128, W], BF16, "KK%d" % g, L2) for g in range(2)]
                    VA = S.sbuf([128, 18, 2, 128], BF16, "VA", L2)
                    S.i("pool", "memset", VA[:, :, :, 64:128], 1.0, writes=[VA])
                    with contextlib.ExitStack() as ph:
                        self.mn_alloc(ph, nbuf=2)
                        xb = [S.sbuf([128, 8, 512], F32, "xb%d" % i, ph) for i in range(2)]
                        for bi, (s, n, kind) in enumerate(BLKS):
                            xt = xb[bi % 2]
                            self.load(xt, XTv[:, :, s:s + n], XT, dst_ap=xt[:, :, :n])
                            self.modnorm_block(xt, n, 0, 1, 0, 2 if kind == "c" else b, [hT[c][:, s:s + n] for c in range(8)], hT)
                        S.barrier()
                    for c in range(8):
                        self.dump("hT", hT[c], hT[c][:], lambda d, c=c: d[c])
                    if self.stop == "mn1":
                        return
                    with contextlib.ExitStack() as ph:
                        self.inproj_even(b, hT, QT, KK, VA, ph)
                        S.barrier()
                    for j in range(4):
                        self.dump("QT", QT[j], QT[j][:], lambda d, j=j: d[j])
                    for g in range(2):
                        self.dump("KK", KK[g], KK[g][:], lambda d, g=g: d[g])
                    self.dump("VA", VA, VA[:])
                    self.dump("HY", self.HY_d, self.HY_d[:])
                    if self.stop == "inproj":
                        return
                    mixT = hT
                    with contextlib.ExitStack() as ph:
                        self.attention_even(b, QT, KK, VA, mixT, ph)
                        S.barrier()
                with contextlib.ExitStack() as ph:
                    self.hyena(b, mixT, ph)
                    S.barrier()
                for c in range(8):
                    self.dump("mixT", mixT[c], mixT[c][:], lambda d, c=c: d[c])
                if self.stop == "mix0":
                    return
                with contextlib.ExitStack() as ph:
                    self.outproj_norm_router(b, 0, mixT, I["e_w_out"], ph, with_ctx=True)
                    S.barrier()
                self.dump("XT", XT, XT[:])
                self.dump("G", self.G_d, self.G_d[:])
                if self.stop == "xa0":
                    return
            with contextlib.ExitStack() as ph:
                self.moe(b, 0, ph, with_ctx=True)
                S.barrier()
            if b == 0:
                self.dump("XT", XT, XT[:])

    def inproj_even(self, b, hT, QT, KK, VA, ph):
        S, I = self.S, self.I
        wsrc = I["e_w_in"][:, :].rearrange("(k p) n -> p k n", p=128)
        wq = S.sbuf([128, 8, 768], BF16, "wqkv", ph)
        self.load(wq, wsrc[:, :, 1536:2304], I["e_w_in"], q="pool")
        wkk = S.sbuf([128, 8, 2, 128], BF16, "wkk", ph)
        for g in range(2):
            for hf in range(2):
                S.i("dve", "tensor_copy", wkk[:, :, g, hf * 64:(hf + 1) * 64], wq[:, :, 512 + 64 * g:576 + 64 * g], reads=[wq], writes=[wkk])
        qkg = S.sbuf([128, 2], F32, "qkg", ph)
        self.load(qkg, I["qkg"][:], I["qkg"])
        cos = S.sbuf([128, NLAT], F32, "cos", ph)
        sin = S.sbuf([128, NLAT], F32, "sin", ph)
        self.load(cos, I["cos_e"][:], I["cos_e"])
        self.load(sin, I["sin_e"][:], I["sin_e"])
        rotT = S.sbuf([128, 128], BF16, "rotT", ph)
        self.load(rotT, I["rotT_e"][:], I["rotT_e"])
        VT = S.sbuf([128, W], BF16, "VT", ph)
        sqh = S.sbuf([128, 512], F32, "sqh", ph)
        rs = S.sbuf([128, 512], F32, "rs", ph)
        qn = S.sbuf([128, 512], BF16, "qn", ph)
        t1 = S.sbuf([128, 512], F32, "t1", ph)
        t2 = S.sbuf([128, 512], F32, "t2", ph)
        mt = [("q", j) for j in range(4)] + [("k", g) for g in range(2)] + [("v", 0)]
        for (s, n, kind) in BLKS:
            for (typ, j) in mt:
                ps = self.ps()
                for kc in range(8):
                    if typ == "q":
                        lap = wq[:, kc, j * 128:(j + 1) * 128]
                        lt = wq
                    elif typ == "k":
                        lap = wkk[:, kc, j, :]
                        lt = wkk
                    else:
                        lap = wq[:, kc, 640:768]
                        lt = wq
                    S.mm(ps, ps[:, :n], lt, lap, hT[kc], hT[kc][:, s:s + n], start=(kc == 0), stop=(kc == 7))
                if typ == "v":
                    S.i("act", "activation", VT[:, s:s + n], ps[:, :n], AF.Copy, reads=[ps], writes=[VT])
                    continue
                dst_t = QT[j] if typ == "q" else KK[j]
                gi = 0 if typ == "q" else 1
                S.i("act", "activation", sqh[:, :n], ps[:, :n], AF.Square, reads=[ps], writes=[sqh])
                p2 = self.ps()
                S.mm(p2, p2[:, :n], self.blockones, self.blockones[:], sqh, sqh[:, :n])
                S.i("act", "activation", rs[:, :n], p2[:, :n], AF.Sqrt, bias=self.eps[:], scale=1.0 / 64, reads=[p2, self.eps], writes=[rs])
                S.i("dve", "reciprocal", rs[:, :n], rs[:, :n], reads=[rs], writes=[rs])
                if kind == "c":
                    S.i("dve", "scalar_tensor_tensor", dst_t[:, s:s + n], ps[:, :n], qkg[:, gi:gi + 1], rs[:, :n], ALU.mult, ALU.mult, reads=[ps, qkg, rs], writes=[dst_t])
                else:
                    tc0 = s - L0
                    S.i("dve", "scalar_tensor_tensor", qn[:, :n], ps[:, :n], qkg[:, gi:gi + 1], rs[:, :n], ALU.mult, ALU.mult, reads=[ps, qkg, rs], writes=[qn])
                    p3 = self.ps()
                    S.mm(p3, p3[:, :n], rotT, rotT[:], qn, qn[:, :n])
                    S.i("pool", "tensor_tensor", t1[:, :n], qn[:, :n], cos[:, tc0:tc0 + n], ALU.mult, reads=[qn, cos], writes=[t1])
                    S.i("dve", "tensor_tensor", t2[:, :n], p3[:, :n], sin[:, tc0:tc0 + n], ALU.mult, reads=[p3, sin], writes=[t2])
                    S.i("dve", "tensor_tensor", dst_t[:, s:s + n], t1[:, :n], t2[:, :n], ALU.add, reads=[t1, t2], writes=[dst_t])
        for ti, (s, kind, _) in enumerate(TTILES):
            pt = self.ps()
            ptb = pt[:, 0:64].bitcast(BF16)
            S.tr(pt, ptb, VT, VT[:, s:s + 128], self.identb, self.identb[:])
            S.i("act", "activation", VA[:, ti, :, 0:64], ptb.rearrange("p (g d) -> p g d", g=2), AF.Copy, reads=[pt], writes=[VA])
        cw = S.sbuf([128, 3, 512], F32, "cw", ph)
        cb = S.sbuf([128, 512], F32, "cb", ph)
        wh = S.sbuf([128, 8, 512], BF16, "wh", ph)
        whs = [S.sbuf([128, 8, 512], BF16, "whs%d" % t, ph) for t in range(3)]
        ob = [S.sbuf([128, 512], BF16, "hyo%d" % i, ph) for i in range(2)]
        oi = 0
        for nb in range(3):
            self.load(wh, wsrc[:, :, nb * 512:(nb + 1) * 512], I["e_w_in"], q="pool")
            self.load(cw, I["convw"][:, nb * 512:(nb + 1) * 512].partition_broadcast(128), I["convw"])
            self.load(cb, I["convb"][0, nb * 512:(nb + 1) * 512].partition_broadcast(128), I["convb"])
            for tap in range(3):
                for kc in range(8):
                    S.i("dve", "tensor_tensor", whs[tap][:, kc, :], wh[:, kc, :], cw[:, tap, :], ALU.mult, reads=[wh, cw], writes=[whs[tap]])
            for ti, (s, kind, idx) in enumerate(TTILES):
                ps = self.ps()
                first = True
                for tap in range(3):
                    for kc in range(8):
                        S.mm(ps, ps[:, :], hT[kc], hT[kc][:, s + tap - 1:s + tap - 1 + 128], whs[tap], whs[tap][:, kc, :], start=first, stop=(tap == 2 and kc == 7))
                        first = False
                o = ob[oi % 2]
                oi += 1
                S.i("dve", "tensor_tensor", o[:], ps[:, :], cb[:], ALU.add, reads=[ps, cb], writes=[o])
                row = ti * 128
                S.dma("sp", self.HY_d[nb, row:row + 128, :], o[:], reads=[o], writes=[self.HY_d], semtile=o)


    def attn_multi(self, streams, nkt, n, scale, ph_t):
        S = self.S
        ns = len(streams)
        for st_ in streams:
            st_["oacc"] = self.ps(reserve=True)
        npt = len(ph_t["pT"])
        cnt = [0]

        def finish(si, kt_, sT_):
            st_ = streams[si]
            pT = ph_t["pT"][cnt[0] % npt]
            cnt[0] += 1
            oacc = st_["oacc"]
            S.i("act", "activation", pT[:, :n], sT_[:, :n], AF.Exp, scale=scale, reads=[sT_], writes=[pT])
            S.mm(oacc, oacc[:, :n], st_["Vt"], st_["Vap"](kt_), pT, pT[:, :n], start=(kt_ == 0), stop=(kt_ == nkt - 1))

        depth = 2 if ns == 1 else 1
        pend = []
        for kt in range(nkt):
            for si, st_ in enumerate(streams):
                sT = self.ps()
                S.mm(sT, sT[:, :n], st_["KTt"], st_["Kap"](kt), st_["QTt"], st_["Qap"])
                pend.append((si, kt, sT))
            while len(pend) > depth * ns:
                finish(*pend.pop(0))
        while pend:
            finish(*pend.pop(0))
        for si, st_ in enumerate(streams):
            den = ph_t["den"][si]
            oacc = st_["oacc"]
            S.i("act", "activation", den[0:64, :n], oacc[64:128, :n], AF.Copy, reads=[oacc], writes=[den])
            S.i("dve", "reciprocal", den[0:64, :n], den[0:64, :n], reads=[den], writes=[den])
            S.i("dve", "tensor_tensor", st_["out_ap"], oacc[0:64, :n], den[0:64, :n], ALU.mult, reads=[oacc, den], writes=[st_["out_t"]])
            self.ps_free(oacc)

    def attention_even(self, b, QT, KK, VA, mixT, ph):
        S = self.S
        T = {"pT": [S.sbuf([128, 512], BF16, "pT%d" % i, ph) for i in range(6)], "den": [S.sbuf([64, 512], F32, "den%d" % i, ph) for i in range(2)]}

        def kcol(kt):
            return (C0 + 128 * kt) if kt < 2 else (L0 + 128 * (kt - 2))

        for hp in range(4):
            g = hp // 2
            j = hp
            out_t = mixT[4 + j]
            blocks = [(C0, 256, 2)] + [(L0 + 512 * i, 512, 18) for i in range(4)]
            for (s, n, nkt) in blocks:
                streams = []
                for po in (0, 64):
                    streams.append({"QTt": QT[j], "Qap": QT[j][po:po + 64, s:s + n], "KTt": KK[g],
                                    "Kap": (lambda kt, po=po: KK[g][po:po + 64, kcol(kt):kcol(kt) + 128]),
                                    "Vt": VA, "Vap": (lambda kt: VA[:, kt, g, :]), "out_t": out_t, "out_ap": out_t[po:po + 64, s:s + n]})
                self.attn_multi(streams, nkt, n, 0.125, T)

    def hyena_filters(self):
        S, I = self.S, self.I
        with contextlib.ExitStack() as ph:
            w1 = S.sbuf([33, 64], F32, "hw1", ph)
            w2 = S.sbuf([64, 64], F32, "hw2", ph)
            w3 = S.sbuf([64, 2048], F32, "hw3", ph)
            hv = S.sbuf([64, 3], F32, "hv", ph)
            self.load(w1, I["hy_w1"][:], I["hy_w1"])
            self.load(w2, I["hy_w2"][:], I["hy_w2"])
            self.load(w3, I["hy_w3"][:], I["hy_w3"])
            self.load(hv, I["hy_vec"][:], I["hy_vec"])
            fb = S.sbuf([64, 2], F32, "fb", ph)
            for i in range(2):
                S.i("dve", "tensor_tensor", fb[:, i:i + 1], hv[:, i:i + 1], hv[:, 2:3], ALU.mult, reads=[hv], writes=[fb])
            OFF = math.pi + TWO_PI * 16
            for tag, n in (("c", NCTX), ("l", NLAT)):
                NT = n // 128
                with contextlib.ExitStack() as p2:
                    zT = S.sbuf([33, n], F32, "zT", p2)
                    self.load(zT, I["zT_" + tag][:], I["zT_" + tag])
                    h1 = S.sbuf([64, n], F32, "h1", p2)
                    h2 = S.sbuf([64, n], F32, "h2", p2)
                    dec = S.sbuf([128, NT, 512], F32, "dec", p2)
                    self.load(dec, I["dec_" + tag][:], I["dec_" + tag])
                    U4 = S.sbuf([128, NT, 2048], BF16, "U4", p2)
                    a1 = S.sbuf([64, 512], F32, "a1", p2)
                    ki = S.sbuf([64, 512], mybir.dt.int32, "ki", p2)
                    kf = S.sbuf([64, 512], F32, "kf", p2)
                    tp = S.sbuf([128, 512], F32, "tp", p2)
                    sq = S.sbuf([128, 512], F32, "sqf", p2)
                    rn = S.sbuf([128, 2, 512], F32, "rn", p2)
                    nb = min(512, n)
                    for (src, wt, wap, dst, bi) in ((zT, w1, w1[:, :], h1, 0), (h1, w2, w2[:, :], h2, 1)):
                        for c0 in range(0, n, nb):
                            p = self.ps()
                            K = 33 if bi == 0 else 64
                            S.mm(p, p[0:64, :nb], wt, wap, src, src[0:K, c0:c0 + nb])
                            S.i("dve", "tensor_scalar", a1[:, :nb], p[0:64, :nb], hv[:, 2:3], fb[:, bi:bi + 1], ALU.mult, ALU.add, reads=[p, hv, fb], writes=[a1])
                            S.i("dve", "tensor_scalar", a1[:, :nb], a1[:, :nb], 1.0 / TWO_PI, 16.0, ALU.mult, ALU.add, reads=[a1], writes=[a1])
                            S.i("dve", "tensor_copy", ki[:, :nb], a1[:, :nb], reads=[a1], writes=[ki])
                            S.i("dve", "tensor_copy", kf[:, :nb], ki[:, :nb], reads=[ki], writes=[kf])
                            S.i("dve", "tensor_tensor", a1[:, :nb], a1[:, :nb], kf[:, :nb], ALU.subtract, reads=[a1, kf], writes=[a1])
                            S.i("dve", "tensor_single_scalar", kf[:, :nb], a1[:, :nb], 0.5, ALU.is_gt, reads=[a1], writes=[kf])
                            S.i("dve", "tensor_tensor", a1[:, :nb], a1[:, :nb], kf[:, :nb], ALU.subtract, reads=[a1, kf], writes=[a1])
                            S.i("act", "activation", dst[:, c0:c0 + nb], a1[:, :nb], AF.Sin, scale=TWO_PI, reads=[a1], writes=[dst])
                    ssum = [self.ps(reserve=True), self.ps(reserve=True)]
                    tpd = [S.sbuf([128, 512], F32, "tpd%d" % i, p2) for i in range(2)]
                    for tt in range(NT):
                        for o_ in range(2):
                            for d_ in range(2):
                                cbk = d_ * 2 + o_
                                tpx = tpd[d_]
                                p = self.ps()
                                S.mm(p, p[:, :], h2, h2[:, tt * 128:(tt + 1) * 128], w3, w3[:, cbk * 512:(cbk + 1) * 512])
                                S.i("dve", "tensor_tensor", tpx[:], p[:, :], dec[:, tt, :], ALU.mult, reads=[p, dec], writes=[tpx])
                                if d_ == 1 and tt == 0:
                                    S.i("dve", "memset", tpx[0:1, :], 0.0, writes=[tpx])
                                S.i("act", "activation", sq[:], tpx[:], AF.Square, reads=[tpx], writes=[sq])
                                S.mm(ssum[o_], ssum[o_][:, :], self.ones, self.ones[:], sq, sq[:], start=(tt == 0 and d_ == 0), stop=(tt == NT - 1 and d_ == 1))
                            S.i("dve", "tensor_tensor", U4[:, tt, o_ * 512:(o_ + 1) * 512], tpd[0][:], tpd[1][:], ALU.add, reads=tpd, writes=[U4])
                            S.i("dve", "tensor_tensor", U4[:, tt, (2 + o_) * 512:(3 + o_) * 512], tpd[0][:], tpd[1][:], ALU.subtract, reads=tpd, writes=[U4])
                    for o_ in range(2):
                        S.i("act", "activation", rn[:, o_, :], ssum[o_][:, :], AF.Sqrt, bias=self.eps[:], reads=[ssum[o_], self.eps], writes=[rn])
                        S.i("dve", "reciprocal", rn[:, o_, :], rn[:, o_, :], reads=[rn], writes=[rn])
                        self.ps_free(ssum[o_])
                    fw = [S.sbuf([128, 2, NT, 128], BF16, "fw%d" % i, p2) for i in range(2)]
                    Ht = [S.sbuf([128, 2, 512], F32, "Ht%d" % i, p2) for i in range(2)]
                    hi = 0
                    self.load(fw[0], I["fwd_" + tag][0].rearrange("r p k m -> p r k m"), I["fwd_" + tag])
                    for f in range(NT):
                        fwt = fw[f % 2]
                        if f + 1 < NT:
                            self.load(fw[(f + 1) % 2], I["fwd_" + tag][f + 1].rearrange("r p k m -> p r k m"), I["fwd_" + tag])
                        for o_ in range(2):
                            H = Ht[hi % 2]
                            hi += 1
                            for ri in range(2):
                                pf = self.ps()
                                c_lo = (o_ if ri == 0 else 2 + o_) * 512
                                for kt in range(NT):
                                    S.mm(pf, pf[:, :], fwt, fwt[:, ri, kt, :], U4, U4[:, kt, c_lo:c_lo + 512], start=(kt == 0), stop=(kt == NT - 1))
                                S.i("dve", "tensor_tensor", H[:, ri, :], pf[:, :], rn[:, o_, :], ALU.mult, reads=[pf, rn], writes=[H])
                            if f == 0:
                                pn = self.ps()
                                for kt in range(NT):
                                    S.mm(pn, pn[:, :], fwt, fwt[:, 1, kt, :], U4, U4[:, kt, o_ * 512:(o_ + 1) * 512], start=(kt == 0), stop=(kt == NT - 1))
                                S.i("dve", "tensor_tensor", H[0:1, 1, :], pn[0:1, :], rn[0:1, o_, :], ALU.mult, reads=[pn, rn], writes=[H])
                            S.dma("sp", self.Hf_d[tag][o_, f], H[:], reads=[H], writes=[self.Hf_d[tag]], semtile=H)
                    S.barrier()
            for tag in ("l", "c"):
                self.dump("Hf_" + tag, self.Hf_d[tag], self.Hf_d[tag][:])
            S.barrier()

    def hyena(self, b, mixT, ph):
        S, I = self.S, self.I
        fbb = S.sbuf([128, 2, 512], F32, "fbb", ph)
        self.load(fbb, I["fbias"][:, :].partition_broadcast(128), I["fbias"])
        for tag, n, row0, col0 in (("c", NCTX, 0, C0), ("l", NLAT, NCTX, L0)):
            NT = n // 128
            with contextlib.ExitStack() as p2:
                u = S.sbuf([128, NT, 512], BF16, "hu", p2)
                z = S.sbuf([128, NT, 512], BF16, "hz", p2)
                Yf = S.sbuf([128, 2 * NT, 512], BF16, "Yf", p2)
                fw = [S.sbuf([128, 2, NT, 128], BF16, "cfw%d" % i, p2) for i in range(2)]
                iv = [S.sbuf([128, 2 * NT, 128], BF16, "civ%d" % i, p2) for i in range(2)]
                Hl = [S.sbuf([128, 2, 512], F32, "Hl%d" % i, p2) for i in range(2)]
                ta = S.sbuf([128, 512], F32, "ta", p2)
                tb = S.sbuf([128, 512], F32, "tb", p2)
                xg = [S.sbuf([128, 512], BF16, "xg%d" % i, p2) for i in range(2)]
                vg = [S.sbuf([128, 512], BF16, "vg%d" % i, p2) for i in range(2)]
                og = [S.sbuf([128, 512], BF16, "og%d" % i, p2) for i in range(2)]
                self.load(u, self.HY_d[2, row0:row0 + n, :].rearrange("(t p) c -> p t c", p=128), self.HY_d)
                for o_ in range(2):
                    src = u if o_ == 0 else z
                    def ld_f(f_):
                        self.load(fw[f_ % 2], I["fwd_" + tag][f_].rearrange("r p k m -> p r k m"), I["fwd_" + tag])
                        self.load(Hl[f_ % 2], self.Hf_d[tag][o_, f_], self.Hf_d[tag])

                    def ld_t(t_):
                        self.load(iv[t_ % 2], I["inv_" + tag][t_], I["inv_" + tag])
                        r0_ = row0 + t_ * 128
                        self.load(xg[t_ % 2], self.HY_d[o_, r0_:r0_ + 128, :], self.HY_d)

                    ld_f(0)
                    for f in range(NT):
                        fwt = fw[f % 2]
                        H = Hl[f % 2]
                        if f + 1 < NT:
                            ld_f(f + 1)
                        else:
                            ld_t(0)
                        pr = self.ps()
                        pi = self.ps()
                        for kt in range(NT):
                            S.mm(pr, pr[:, :], fwt, fwt[:, 0, kt, :], src, src[:, kt, :], start=(kt == 0), stop=(kt == NT - 1))
                        for kt in range(NT):
                            S.mm(pi, pi[:, :], fwt, fwt[:, 1, kt, :], src, src[:, kt, :], start=(kt == 0), stop=(kt == NT - 1))
                        S.i("dve", "tensor_tensor", ta[:], pr[:, :], H[:, 1, :], ALU.mult, reads=[pr, H], writes=[ta])
                        S.i("dve", "tensor_tensor", tb[:], pi[:, :], H[:, 0, :], ALU.mult, reads=[pi, H], writes=[tb])
                        S.i("dve", "tensor_tensor", Yf[:, NT + f, :], ta[:], tb[:], ALU.add, reads=[ta, tb], writes=[Yf])
                        S.i("dve", "tensor_tensor", ta[:], pr[:, :], H[:, 0, :], ALU.mult, reads=[pr, H], writes=[ta])
                        S.i("dve", "tensor_tensor", tb[:], pi[:, :], H[:, 1, :], ALU.mult, reads=[pi, H], writes=[tb])
                        S.i("dve", "tensor_tensor", Yf[:, f, :], ta[:], tb[:], ALU.subtract, reads=[ta, tb], writes=[Yf])
                        if f == 0:
                            S.i("dve", "tensor_copy", Yf[0:1, 0, :], ta[0:1, :], reads=[ta], writes=[Yf])
                            S.i("dve", "tensor_copy", Yf[0:1, NT, :], tb[0:1, :], reads=[tb], writes=[Yf])
                    for tt in range(NT):
                        ivt = iv[tt % 2]
                        xt_ = xg[tt % 2]
                        if tt + 1 < NT:
                            ld_t(tt + 1)
                        py = self.ps()
                        for jj in range(2 * NT):
                            S.mm(py, py[:, :], ivt, ivt[:, jj, :], Yf, Yf[:, jj, :], start=(jj == 0), stop=(jj == 2 * NT - 1))
                        S.i("pool", "tensor_tensor", ta[:], src[:, tt, :], fbb[:, o_, :], ALU.mult, reads=[src, fbb], writes=[ta])
                        S.i("dve", "tensor_tensor", tb[:], py[:, :], ta[:], ALU.add, reads=[py, ta], writes=[tb])
                        if o_ == 0:
                            S.i("dve", "tensor_tensor", z[:, tt, :], tb[:], xt_[:], ALU.mult, reads=[tb, xt_], writes=[z])
                        else:
                            ot = og[tt % 2]
                            S.i("dve", "tensor_tensor", ot[:], tb[:], xt_[:], ALU.mult, reads=[tb, xt_], writes=[ot])
                            for cc in range(4):
                                pt = self.ps()
                                ptb = pt[:, 0:64].bitcast(BF16)
                                S.tr(pt, ptb, ot, ot[:, cc * 128:(cc + 1) * 128], self.identb, self.identb[:])
                                S.i("act", "activation", mixT[cc][:, col0 + tt * 128:col0 + (tt + 1) * 128], ptb, AF.Copy, reads=[pt], writes=[mixT[cc]])
                S.barrier()


    def outproj_norm_router(self, b, layer, mixT, w_out, ph, with_ctx):
        S, I = self.S, self.I
        XTv = self.xt_view(b)
        XT = self.XT_d[b]
        wo = S.sbuf([128, 8, 1024], BF16, "wo", ph)
        self.load(wo, w_out[:, :].rearrange("(k p) n -> p k n", p=128), w_out, q="pool")
        wr = S.sbuf([128, 8, 36], F32, "wr", ph)
        self.load(wr, I["wr"][:, layer], I["wr"])
        self.mn_alloc(ph, nbuf=2)
        xb = [S.sbuf([128, 8, 512], F32, "xb%d" % i, ph) for i in range(2)]
        h32s = [S.sbuf([128, 8, 512], F32, "h32_%d" % i, ph) for i in range(2)]
        hbs = [S.sbuf([128, 8, 512], BF16, "hb%d" % i, ph) for i in range(1)]
        H2v = self.H2_d[:, :].rearrange("(k p) w -> p k w", p=128)
        self._rt = {"L": S.sbuf([128, 4, 36], F32, "rtL", ph), "gm": S.sbuf([128, 4], F32, "rtgm", ph), "d4": S.sbuf([128, 4, 4], F32, "rtd4", ph),
                    "mg": S.sbuf([128, 4, 4], F32, "rtmg", ph), "eg": S.sbuf([128, 4, 4], F32, "rteg", ph), "se": S.sbuf([128, 4], F32, "rtse", ph),
                    "les": S.sbuf([128, 4, 8], F32, "rtles", ph), "t8": S.sbuf([128, 4, 8], F32, "rtt8", ph), "dl": S.sbuf([128, 4, 8], F32, "rtdl", ph),
                    "mk1": S.sbuf([128, 4, 8], F32, "rtmk1", ph), "mk12": S.sbuf([128, 4, 8], F32, "rtmk12", ph), "m2": S.sbuf([128, 4], F32, "rtm2", ph),
                    "inner": S.sbuf([128, 4, 8], F32, "rtin", ph), "sg4": S.sbuf([128, 4, 4], F32, "rtsg4", ph), "gates": S.sbuf([128, 4, 32], F32, "rtgates", ph)}
        gT = S.sbuf([32, 512], BF16, "rgT", ph)
        blks = BLKS if with_ctx else BLKS[1:]
        xdep = [Tile(XT.t, "xtblk%d" % i) for i in range(len(blks))]

        def part1(bi):
            s, n, kind = blks[bi]
            ncol = 2 if kind == "c" else b
            xt = xb[bi % 2]
            self.load(xt, XTv[:, :, s:s + n], xdep[bi], dst_ap=xt[:, :, :n])
            for mo in range(8):
                ps = self.ps()
                for kc in range(8):
                    S.mm(ps, ps[:, :n], wo, wo[:, kc, mo * 128:(mo + 1) * 128], mixT[kc], mixT[kc][:, s:s + n], start=(kc == 0), stop=(kc == 7))
                S.i("dve", "scalar_tensor_tensor", xt[:, mo, :n], ps[:, :n], self.mv(layer, 2, mo, ncol), xt[:, mo, :n], ALU.mult, ALU.add, reads=[ps, self.MV, xt], writes=[xt])
            S.dma("sp", XTv[:, :, s:s + n], xt[:, :, :n], reads=[xt], writes=[xdep[bi]], semtile=xt)

        part1(0)
        for bi, (s, n, kind) in enumerate(blks):
            if bi + 1 < len(blks):
                part1(bi + 1)
            ncol = 2 if kind == "c" else b
            xt = xb[bi % 2]
            hb = hbs[bi % len(hbs)]
            h32 = h32s[bi % 2]
            self.modnorm_block(xt, n, layer, 4, 3, ncol, [hb[:, c, :n] for c in range(8)], [hb] * 8, h32=h32)
            S.dma("sp", H2v[:, :, s:s + n], hb[:, :, :n], reads=[hb], writes=[self.H2_d], semtile=hb)
            nt = n // 128
            pl = self.ps()
            plv = pl[:, 0:256].rearrange("p (t c) -> p t c", c=64)
            for t in range(nt):
                for c in range(8):
                    S.mm(pl, plv[:, t, 0:36], h32, h32[:, c, t * 128:(t + 1) * 128], wr, wr[:, c, :], start=(c == 0), stop=(c == 7))
            R = self._rt

            def bc1(ap2, w_):
                return ap2.unsqueeze(2).to_broadcast([128, nt, w_])

            Lb, gm, d4, mg, eg, se, les, t8, dl, mk1, mk12, m2, inner, sg4, gates = (R[x] for x in
                ("L", "gm", "d4", "mg", "eg", "se", "les", "t8", "dl", "mk1", "mk12", "m2", "inner", "sg4", "gates"))
            allr = list(R.values())

            def dv(method, *args):
                S.i("dve", method, *args, reads=allr, writes=allr)

            S.i("dve", "tensor_copy", Lb[:, :nt, :], plv[:, :nt, 0:36], reads=[pl], writes=allr)
            dv("reduce_max", gm[:, :nt], Lb[:, :nt, 0:4], AX.X)
            dv("tensor_tensor", d4[:, :nt, :], Lb[:, :nt, 0:4], bc1(gm[:, :nt], 4), ALU.subtract)
            dv("tensor_single_scalar", mg[:, :nt, :], d4[:, :nt, :], 0.0, ALU.is_ge)
            S.i("act", "activation", eg[:, :nt, :], d4[:, :nt, :], AF.Exp, reads=allr, writes=allr)
            dv("reduce_sum", se[:, :nt], eg[:, :nt, :], AX.X)
            dv("reciprocal", se[:, :nt], se[:, :nt])
            dv("tensor_tensor", les[:, :nt, :], Lb[:, :nt, 4:12], mg[:, :nt, 0:1].to_broadcast([128, nt, 8]), ALU.mult)
            for g in range(1, 4):
                dv("tensor_tensor", t8[:, :nt, :], Lb[:, :nt, 4 + 8 * g:12 + 8 * g], mg[:, :nt, g:g + 1].to_broadcast([128, nt, 8]), ALU.mult)
                dv("tensor_tensor", les[:, :nt, :], les[:, :nt, :], t8[:, :nt, :], ALU.add)
            dv("reduce_max", gm[:, :nt], les[:, :nt, :], AX.X)
            dv("tensor_tensor", dl[:, :nt, :], les[:, :nt, :], bc1(gm[:, :nt], 8), ALU.subtract)
            dv("tensor_single_scalar", mk1[:, :nt, :], dl[:, :nt, :], 0.0, ALU.is_ge)
            dv("scalar_tensor_tensor", t8[:, :nt, :], mk1[:, :nt, :], -1.0e30, dl[:, :nt, :], ALU.mult, ALU.add)
            dv("reduce_max", m2[:, :nt], t8[:, :nt, :], AX.X)
            dv("tensor_tensor", mk12[:, :nt, :], dl[:, :nt, :], bc1(m2[:, :nt], 8), ALU.is_ge)
            S.i("act", "activation", m2[:, :nt], m2[:, :nt], AF.Exp, reads=allr, writes=allr)
            dv("tensor_single_scalar", gm[:, :nt], m2[:, :nt], 1.0, ALU.add)
            dv("reciprocal", gm[:, :nt], gm[:, :nt])
            dv("tensor_tensor", m2[:, :nt], m2[:, :nt], gm[:, :nt], ALU.mult)
            dv("tensor_tensor", gm[:, :nt], gm[:, :nt], m2[:, :nt], ALU.subtract)
            dv("tensor_tensor", inner[:, :nt, :], mk12[:, :nt, :], bc1(m2[:, :nt], 8), ALU.mult)
            dv("tensor_tensor", t8[:, :nt, :], mk1[:, :nt, :], bc1(gm[:, :nt], 8), ALU.mult)
            dv("tensor_tensor", inner[:, :nt, :], inner[:, :nt, :], t8[:, :nt, :], ALU.add)
            dv("tensor_tensor", sg4[:, :nt, :], mg[:, :nt, :], bc1(se[:, :nt], 4), ALU.mult)
            for g in range(4):
                dv("tensor_tensor", gates[:, :nt, 8 * g:8 * g + 8], inner[:, :nt, :], sg4[:, :nt, g:g + 1].to_broadcast([128, nt, 8]), ALU.mult)
            for t in range(nt):
                pt = self.ps()
                S.tr(pt, pt[0:32, 0:128], gates, gates[:, t, :], self.ident, self.ident[:])
                S.i("act", "activation", gT[:, t * 128:(t + 1) * 128], pt[0:32, 0:128], AF.Copy, reads=[pt], writes=[gT])
            S.dma("sp", self.G_d[:, s:s + n], gT[:, :n], reads=[gT], writes=[self.G_d], semtile=gT)

    def moe(self, b, layer, ph, with_ctx, final=False):
        S, I = self.S, self.I
        XTv = self.xt_view(b)
        XT = self.XT_d[b]
        h2T = [S.sbuf([128, W], BF16, "h2T%d" % c, ph) for c in range(8)]
        for c in range(8):
            self.load(h2T[c], self.H2_d[c * 128:(c + 1) * 128, :], self.H2_d)
        xT = [S.sbuf([128, W], F32, "xT%d" % c, ph) for c in range(8)]
        wgu = [S.sbuf([128, 2, 2, 8, 256], BF16, "wgu%d" % i, ph) for i in range(2)]
        wd = [S.sbuf([128, 2, 2, 1024], BF16, "wd%d" % i, ph) for i in range(2)]
        gbc = [S.sbuf([128, 2, W], BF16, "gbc%d" % i, ph) for i in range(2)]
        sg = [S.sbuf([128, 512], BF16, "sg%d" % i, ph) for i in range(2)]
        su = [S.sbuf([128, 512], BF16, "su%d" % i, ph) for i in range(2)]
        hid = [S.sbuf([128, 512], BF16, "hid%d" % i, ph) for i in range(8)]
        blks = BLKS if with_ctx else BLKS[1:]
        self._hcnt = 0

        def load_pair(ep):
            wg_, wd_, gb_ = wgu[ep % 2], wd[ep % 2], gbc[ep % 2]
            for e in range(2):
                E = 2 * ep + e
                self.load(wg_, I["moe_w_gate"][layer, E].rearrange("(k p) n -> p k n", p=128), I["moe_w_gate"], q="pool", dst_ap=wg_[:, e, 0])
                self.load(wg_, I["moe_w_up"][layer, E].rearrange("(k p) n -> p k n", p=128), I["moe_w_up"], q="pool", dst_ap=wg_[:, e, 1])
                self.load(wd_, I["moe_w_down"][layer, E].rearrange("(k p) n -> p k n", p=128), I["moe_w_down"], q="pool", dst_ap=wd_[:, e])
            self.load(gb_, self.G_d[2 * ep:2 * ep + 2, :].partition_broadcast(128), self.G_d)

        def stage_a(ep, s, n):
            wg_, gb_ = wgu[ep % 2], gbc[ep % 2]
            hs = []
            for e in range(2):
                for m in range(2):
                    pg = self.ps()
                    pu = self.ps()
                    for kc in range(8):
                        S.mm(pg, pg[:, :n], wg_, wg_[:, e, 0, kc, m * 128:(m + 1) * 128], h2T[kc], h2T[kc][:, s:s + n], start=(kc == 0), stop=(kc == 7))
                    for kc in range(8):
                        S.mm(pu, pu[:, :n], wg_, wg_[:, e, 1, kc, m * 128:(m + 1) * 128], h2T[kc], h2T[kc][:, s:s + n], start=(kc == 0), stop=(kc == 7))
                    a_, u_ = sg[self._hcnt % 2], su[self._hcnt % 2]
                    hd = hid[self._hcnt % 8]
                    self._hcnt += 1
                    S.i("act", "activation", a_[:, :n], pg[:, :n], AF.Silu, reads=[pg], writes=[a_])
                    S.i("act", "activation", u_[:, :n], pu[:, :n], AF.Copy, reads=[pu], writes=[u_])
                    S.i("dve", "tensor_tensor", a_[:, :n], a_[:, :n], u_[:, :n], ALU.mult, reads=[a_, u_], writes=[a_])
                    S.i("dve", "tensor_tensor", hd[:, :n], a_[:, :n], gb_[:, e, s:s + n], ALU.mult, reads=[a_, gb_], writes=[hd])
                    hs.append((hd, e, m))
            return hs

        def stage_b(ep, s, n, ncol, hs):
            wd_ = wd[ep % 2]
            for mo in range(8):
                py = self.ps()
                for ii, (hd, e, m) in enumerate(hs):
                    S.mm(py, py[:, :n], wd_, wd_[:, e, m, mo * 128:(mo + 1) * 128], hd, hd[:, :n], start=(ii == 0), stop=(ii == 3))
                S.i("dve", "scalar_tensor_tensor", xT[mo][:, s:s + n], py[:, :n], self.mv(layer, 5, mo, ncol), xT[mo][:, s:s + n], ALU.mult, ALU.add,
                    reads=[py, self.MV, xT[mo]], writes=[xT[mo]])

        load_pair(0)
        for c in range(8):
            self.load(xT[c], XT[c * 128:(c + 1) * 128, :], XT)
        for ep in range(16):
            if ep + 1 < 16:
                load_pair(ep + 1)
            pend = None
            for (s, n, kind) in blks:
                ncol = 2 if kind == "c" else b
                hs = stage_a(ep, s, n)
                if pend is not None:
                    stage_b(*pend)
                pend = (ep, s, n, ncol, hs)
            stage_b(*pend)
        if not final:
            for c in range(8):
                S.dma("sp", XT[c * 128:(c + 1) * 128, :], xT[c][:], reads=[xT[c]], writes=[XT], semtile=xT[c])
            return
        fg = S.sbuf([128, 8], F32, "fg", ph)
        self.load(fg, I["finalgT"][:], I["finalgT"])
        sq_t, rs_t = wgu[0], gbc[0]
        sq = sq_t[:].rearrange("p a b c d -> p (a b c d)").bitcast(F32).rearrange("p (c n) -> p c n", n=512)
        rsv = rs_t[:].rearrange("p a w -> p (a w)").bitcast(F32)
        for bi in range(4):
            s = L0 + 512 * bi
            rs_ = rsv[:, 512 * (bi % 2):512 * (bi % 2 + 1)]
            for c in range(8):
                S.i("act", "activation", sq[:, c, :], xT[c][:, s:s + 512], AF.Square, reads=[xT[c]], writes=[sq_t])
            ss = self.ps()
            for c in range(8):
                S.mm(ss, ss[:, :], self.ones, self.ones[:], sq_t, sq[:, c, :], start=(c == 0), stop=(c == 7))
            S.i("act", "activation", rs_, ss[:, :], AF.Sqrt, bias=self.eps[:], scale=1.0 / D, reads=[ss, self.eps], writes=[rs_t])
            S.i("dve", "reciprocal", rs_, rs_, reads=[rs_t], writes=[rs_t])
            for c in range(8):
                S.i("dve", "scalar_tensor_tensor", xT[c][:, s:s + 512], xT[c][:, s:s + 512], fg[:, c:c + 1], rs_, ALU.mult, ALU.mult, reads=[xT[c], fg, rs_t], writes=[xT[c]])
        for c in range(8):
            S.dma("sp", self.out[b, c * 128:(c + 1) * 128, :], xT[c][:, L0:L0 + NLAT], reads=[xT[c]], writes=[self.out], semtile=xT[c])

    def layer1(self, b):
        S, I = self.S, self.I
        XTv = self.xt_view(b)
        XT = self.XT_d[b]
        with contextlib.ExitStack() as LY:
            with contextlib.ExitStack() as L1:
                hT = [S.sbuf([128, W], BF16, "hT%d" % c, L1) for c in range(8)]
                with contextlib.ExitStack() as ph:
                    self.mn_alloc(ph, nbuf=2)
                    xb = [S.sbuf([128, 8, 512], F32, "xb%d" % i, ph) for i in range(2)]
                    for bi, (s, n, kind) in enumerate(BLKS):
                        xt = xb[bi % 2]
                        self.load(xt, XTv[:, :, s:s + n], XT, dst_ap=xt[:, :, :n])
                        self.modnorm_block(xt, n, 1, 1, 0, 2 if kind == "c" else b, [hT[c][:, s:s + n] for c in range(8)], hT)
                    S.barrier()
                mixT = hT
                with contextlib.ExitStack() as L2:
                    uS = [S.sbuf([128, 4, NCTX + NLAT], BF16, "uS%d" % d, L2) for d in range(2)]
                    with contextlib.ExitStack() as L3:
                        cqn = S.sbuf([128, 2, W], BF16, "cqn", L3)
                        ckvn = S.sbuf([128, W], BF16, "ckvn", L3)
                        KR = S.sbuf([128, W], BF16, "KR", L3)
                        with contextlib.ExitStack() as ph:
                            self.inproj_odd(b, hT, cqn, ckvn, KR, uS, ph)
                            S.barrier()
                        with contextlib.ExitStack() as ph:
                            self.mla(b, cqn, ckvn, KR, mixT, ph)
                            S.barrier()
                    with contextlib.ExitStack() as ph:
                        self.s5(b, uS, mixT, ph)
                        S.barrier()
                for c in range(8):
                    self.dump("mixT", mixT[c], mixT[c][:], lambda d, c=c: d[c])
                if self.stop == "mix1":
                    return
                with contextlib.ExitStack() as ph:
                    self.outproj_norm_router(b, 1, mixT, I["o_w_out"], ph, with_ctx=False)
                    S.barrier()
                if self.stop == "xa1":
                    self.dump("XT", XT, XT[:])
                    return
            with contextlib.ExitStack() as ph:
                self.moe(b, 1, ph, with_ctx=False, final=True)
                S.barrier()

    def final_norm(self, b, ph):
        S, I = self.S, self.I
        XTv = self.xt_view(b)
        XT = self.XT_d[b]
        fg = S.sbuf([128, 8], F32, "fg", ph)
        self.load(fg, I["finalgT"][:], I["finalgT"])
        xb = [S.sbuf([128, 8, 512], F32, "fxb%d" % i, ph) for i in range(2)]
        sq = S.sbuf([128, 8, 512], F32, "fsq", ph)
        rstd = S.sbuf([128, 512], F32, "frstd", ph)
        ob = [S.sbuf([128, 8, 512], F32, "fob%d" % i, ph) for i in range(2)]
        outv = self.out[b].rearrange("(k p) t -> p k t", p=128)
        for bi in range(4):
            s = L0 + 512 * bi
            xt = xb[bi % 2]
            o = ob[bi % 2]
            self.load(xt, XTv[:, :, s:s + 512], XT)
            S.i("act", "activation", sq[:], xt[:], AF.Square, reads=[xt], writes=[sq])
            ss = self.ps()
            for c in range(8):
                S.mm(ss, ss[:, :], self.ones, self.ones[:], sq, sq[:, c, :], start=(c == 0), stop=(c == 7))
            S.i("act", "activation", rstd[:], ss[:, :], AF.Sqrt, bias=self.eps[:], scale=1.0 / D, reads=[ss, self.eps], writes=[rstd])
            S.i("dve", "reciprocal", rstd[:], rstd[:], reads=[rstd], writes=[rstd])
            for c in range(8):
                S.i("dve", "scalar_tensor_tensor", o[:, c, :], xt[:, c, :], fg[:, c:c + 1], rstd[:], ALU.mult, ALU.mult, reads=[xt, fg, rstd], writes=[o])
            S.dma("sp", outv[:, :, 512 * bi:512 * (bi + 1)], o[:], reads=[o], writes=[self.out], semtile=o)

    def inproj_odd(self, b, hT, cqn, ckvn, KR, uS, ph):
        S, I = self.S, self.I
        wsrc = I["o_w_in"][:, :].rearrange("(k p) n -> p k n", p=128)
        wi = S.sbuf([128, 8, 928], BF16, "wi", ph)
        self.load(wi, wsrc, I["o_w_in"], q="pool")
        wkr = S.sbuf([128, 8, 128], BF16, "wkr", ph)
        S.i("pool", "memset", wkr[:], 0.0, writes=[wkr])
        S.i("dve", "tensor_copy", wkr[:, :, 64:96], wi[:, :, 384:416], reads=[wi], writes=[wkr])
        ng = S.sbuf([128, 3], F32, "ong", ph)
        self.load(ng, I["o_ng"][:], I["o_ng"])
        cos = S.sbuf([128, NLAT], F32, "cos", ph)
        sin = S.sbuf([128, NLAT], F32, "sin", ph)
        self.load(cos, I["cos_o"][:], I["cos_o"])
        self.load(sin, I["sin_o"][:], I["sin_o"])
        rotT = S.sbuf([128, 128], BF16, "rotT", ph)
        self.load(rotT, I["rotT_o"][:], I["rotT_o"])
        sq = S.sbuf([128, 2, 512], F32, "osq", ph)
        rs = S.sbuf([128, 512], F32, "ors", ph)
        krn = S.sbuf([128, 512], BF16, "krn", ph)
        t1 = S.sbuf([128, 512], F32, "ot1", ph)
        t2 = S.sbuf([128, 512], F32, "ot2", ph)
        KD = os.environ.get("KDBG", "ABCD")
        for (s, n, kind) in BLKS:
          if "A" in KD:
            pq = [self.ps(), self.ps()]
            for j in range(2):
                for kc in range(8):
                    S.mm(pq[j], pq[j][:, :n], wi, wi[:, kc, j * 128:(j + 1) * 128], hT[kc], hT[kc][:, s:s + n], start=(kc == 0), stop=(kc == 7))
                S.i("act", "activation", sq[:, j, :n], pq[j][:, :n], AF.Square, reads=[pq[j]], writes=[sq])
            ss = self.ps()
            for j in range(2):
                S.mm(ss, ss[:, :n], self.ones, self.ones[:], sq, sq[:, j, :n], start=(j == 0), stop=(j == 1))
            S.i("act", "activation", rs[:, :n], ss[:, :n], AF.Sqrt, bias=self.eps[:], scale=1.0 / 256, reads=[ss, self.eps], writes=[rs])
            S.i("dve", "reciprocal", rs[:, :n], rs[:, :n], reads=[rs], writes=[rs])
            for j in range(2):
                S.i("dve", "scalar_tensor_tensor", cqn[:, j, s:s + n], pq[j][:, :n], ng[:, j:j + 1], rs[:, :n], ALU.mult, ALU.mult, reads=[pq[j], ng, rs], writes=[cqn])
          if "B" in KD:
            pk = self.ps()
            for kc in range(8):
                S.mm(pk, pk[:, :n], wi, wi[:, kc, 256:384], hT[kc], hT[kc][:, s:s + n], start=(kc == 0), stop=(kc == 7))
            S.i("act", "activation", sq[:, 0, :n], pk[:, :n], AF.Square, reads=[pk], writes=[sq])
            ss = self.ps()
            S.mm(ss, ss[:, :n], self.ones, self.ones[:], sq, sq[:, 0, :n])
            S.i("act", "activation", rs[:, :n], ss[:, :n], AF.Sqrt, bias=self.eps[:], scale=1.0 / 128, reads=[ss, self.eps], writes=[rs])
            S.i("dve", "reciprocal", rs[:, :n], rs[:, :n], reads=[rs], writes=[rs])
            S.i("dve", "scalar_tensor_tensor", ckvn[:, s:s + n], pk[:, :n], ng[:, 2:3], rs[:, :n], ALU.mult, ALU.mult, reads=[pk, ng, rs], writes=[ckvn])
          if "C" in KD:
            pr = self.ps()
            for kc in range(8):
                S.mm(pr, pr[:, :n], wkr, wkr[:, kc, :], hT[kc], hT[kc][:, s:s + n], start=(kc == 0), stop=(kc == 7))
            if kind == "c":
                S.i("act", "activation", KR[:, s:s + n], pr[:, :n], AF.Copy, reads=[pr], writes=[KR])
            else:
                tc0 = s - L0
                S.i("act", "activation", krn[:, :n], pr[:, :n], AF.Copy, reads=[pr], writes=[krn])
                p3 = self.ps()
                S.mm(p3, p3[:, :n], rotT, rotT[:], krn, krn[:, :n])
                S.i("pool", "tensor_tensor", t1[:, :n], krn[:, :n], cos[:, tc0:tc0 + n], ALU.mult, reads=[krn, cos], writes=[t1])
                S.i("dve", "tensor_tensor", t2[:, :n], p3[:, :n], sin[:, tc0:tc0 + n], ALU.mult, reads=[p3, sin], writes=[t2])
                S.i("dve", "tensor_tensor", KR[:, s:s + n], t1[:, :n], t2[:, :n], ALU.add, reads=[t1, t2], writes=[KR])
          if "D" in KD:
            for j in range(4):
                pu = self.ps()
                for kc in range(8):
                    S.mm(pu, pu[:, :n], wi, wi[:, kc, 416 + j * 128:416 + (j + 1) * 128], hT[kc], hT[kc][:, s:s + n], start=(kc == 0), stop=(kc == 7))
                if kind == "c":
                    d0, d1 = 0, NLAT
                else:
                    d0, d1 = NCTX + (s - L0), s - L0
                S.i("act", "activation", uS[0][:, j, d0:d0 + n], pu[:, :n], AF.Copy, reads=[pu], writes=[uS[0]])
                S.i("pool", "tensor_copy", uS[1][:, j, d1:d1 + n], uS[0][:, j, d0:d0 + n], reads=[uS[0]], writes=[uS[1]])

    def s5_tables(self):
        S, I = self.S, self.I
        T_ = NCTX + NLAT
        self.ST_d = S.dram("ST_d", [32, 128, 4, T_], BF16)
        self.RMAG = S.sbuf([128, 32], F32, "s5rmag")
        with contextlib.ExitStack() as ph:
            prm = S.sbuf([128, 3, 32], F32, "s5prm", ph)
            self.load(prm, I["s5p"][:], I["s5p"])
            W_ = S.sbuf([128, 24, 32], F32, "s5w", ph)
            ki = S.sbuf([128, 32], mybir.dt.int32, "s5ki", ph)
            CO = S.sbuf([128, 4, 32], F32, "s5co", ph)
            PH = S.sbuf([128, 2, 32], F32, "s5ph", ph)
            a_re, a_im, ldt = prm[:, 0, :], prm[:, 1, :], prm[:, 2, :]
            allt = [W_, prm, CO, PH, self.RMAG]

            def w(i):
                return W_[:, i, :]

            def tt(o, x, y, op):
                S.i("dve", "tensor_tensor", o, x, y, op, reads=allt, writes=allt)

            def ts(o, x, s1, s2, o1, o2):
                S.i("dve", "tensor_scalar", o, x, s1, s2, o1, o2, reads=allt, writes=allt)

            def fracfix(o, y):
                S.i("dve", "tensor_copy", ki[:], y, reads=allt, writes=[ki])
                S.i("dve", "tensor_copy", w(20), ki[:], reads=[ki], writes=allt)
                tt(o, y, w(20), ALU.subtract)
                S.i("dve", "tensor_single_scalar", w(20), o, 0.5, ALU.is_gt, reads=allt, writes=allt)
                tt(o, o, w(20), ALU.subtract)

            S.i("act", "activation", w(0), ldt, AF.Exp, reads=allt, writes=allt)
            tt(w(1), a_re, w(0), ALU.mult)
            S.i("act", "activation", self.RMAG[:], w(1), AF.Exp, reads=allt, writes=allt)
            tt(w(3), a_im, w(0), ALU.mult)
            ts(PH[:, 0, :], w(3), 1.0 / TWO_PI, 1.0, ALU.mult, ALU.mult)
            ts(w(4), PH[:, 0, :], 1.0, 16.0, ALU.mult, ALU.add)
            ts(w(5), PH[:, 0, :], 1.0, 16.25, ALU.mult, ALU.add)
            fracfix(w(21), w(4))
            S.i("act", "activation", w(6), w(21), AF.Sin, scale=TWO_PI, reads=allt, writes=allt)
            fracfix(w(21), w(5))
            S.i("act", "activation", w(7), w(21), AF.Sin, scale=TWO_PI, reads=allt, writes=allt)
            ts(w(22), PH[:, 0, :], 64.0, 16.0, ALU.mult, ALU.add)
            fracfix(PH[:, 1, :], w(22))
            tt(w(12), self.RMAG[:], w(7), ALU.mult)
            tt(w(13), self.RMAG[:], w(6), ALU.mult)
            ts(w(8), w(12), -1.0, 1.0, ALU.add, ALU.mult)
            tt(w(9), a_re, a_re, ALU.mult)
            tt(w(10), a_im, a_im, ALU.mult)
            tt(w(9), w(9), w(10), ALU.add)
            S.i("dve", "reciprocal", w(9), w(9), reads=allt, writes=allt)
            tt(w(10), w(8), a_re, ALU.mult)
            tt(w(11), w(13), a_im, ALU.mult)
            tt(w(10), w(10), w(11), ALU.add)
            tt(CO[:, 0, :], w(10), w(9), ALU.mult)
            tt(w(10), w(13), a_re, ALU.mult)
            tt(w(11), w(8), a_im, ALU.mult)
            tt(w(10), w(10), w(11), ALU.subtract)
            tt(CO[:, 1, :], w(10), w(9), ALU.mult)
            ts(CO[:, 2, :], CO[:, 0, :], -1.0, 1.0, ALU.mult, ALU.mult)
            ts(CO[:, 3, :], CO[:, 1, :], -1.0, 1.0, ALU.mult, ALU.mult)
            k1s = S.sbuf([128, 32, 36], F32, "s5k1s", ph)
            k0s = S.sbuf([128, 32, 64], F32, "s5k0s", ph)
            self.load(k1s, I["s5k1s"][:], I["s5k1s"])
            self.load(k0s, I["s5k0s"][:], I["s5k0s"])
            TA = S.sbuf([128, 6, 32, 36], F32, "s5TA", ph)
            TB = S.sbuf([128, 3, 32, 64], F32, "s5TB", ph)
            ya = S.sbuf([128, 32, 36], F32, "s5ya", ph)
            yb_ = S.sbuf([128, 32, 64], F32, "s5yb", ph)
            kia = S.sbuf([128, 32, 36], mybir.dt.int32, "s5kia", ph)
            kib = S.sbuf([128, 32, 64], mybir.dt.int32, "s5kib", ph)
            fa = S.sbuf([128, 32, 36], F32, "s5fa", ph)
            fb_ = S.sbuf([128, 32, 64], F32, "s5fb", ph)
            small = [TA, TB, ya, yb_, fa, fb_, PH, CO]

            def sincos(yt, kit, ft_, ktab, phi_idx, n_, dst_c, dst_s):
                for col in range(32):
                    S.i("dve", "tensor_scalar", yt[:, col, :], ktab[:, col, :], PH[:, phi_idx, col:col + 1], 16.0, ALU.mult, ALU.add, reads=[ktab] + small, writes=small)
                for (off, dst) in ((0.0, dst_s), (0.25, dst_c)):
                    if off:
                        S.i("dve", "tensor_single_scalar", yt[:], yt[:], off, ALU.add, reads=small, writes=small)
                    S.i("dve", "tensor_copy", kit[:], yt[:], reads=small, writes=[kit])
                    S.i("dve", "tensor_copy", ft_[:], kit[:], reads=[kit], writes=small)
                    S.i("dve", "tensor_tensor", ft_[:], yt[:], ft_[:], ALU.subtract, reads=small, writes=small)
                    S.i("dve", "tensor_single_scalar", yt[:], ft_[:], 0.5, ALU.is_gt, reads=small, writes=small)
                    S.i("dve", "tensor_tensor", ft_[:], ft_[:], yt[:], ALU.subtract, reads=small, writes=small)
                    S.i("act", "activation", dst, ft_[:], AF.Sin, scale=TWO_PI, reads=small, writes=small)
                    S.i("dve", "tensor_copy", yt[:], kit[:], reads=[kit], writes=small)
                    S.i("dve", "tensor_tensor", yt[:], yt[:], ft_[:], ALU.add, reads=small, writes=small)

            sincos(ya, kia, fa, k1s, 1, 36, TA[:, 0], TA[:, 1])
            sincos(yb_, kib, fb_, k0s, 0, 64, TB[:, 0], TB[:, 1])
            for col in range(32):
                d = col // 16
                sg = -1.0 if d == 0 else 1.0
                c1 = slice(col, col + 1)
                cA, sA = TA[:, 0, col, :], TA[:, 1, col, :]
                S.i("dve", "tensor_single_scalar", TA[:, 2, col, :], cA, CO[:, 0, c1], ALU.mult, reads=small, writes=small)
                S.i("dve", "scalar_tensor_tensor", TA[:, 2, col, :], sA, CO[:, 3 if sg > 0 else 1, c1], TA[:, 2, col, :], ALU.mult, ALU.add, reads=small, writes=small)
                S.i("dve", "tensor_single_scalar", TA[:, 3, col, :], cA, CO[:, 1, c1], ALU.mult, reads=small, writes=small)
                S.i("dve", "scalar_tensor_tensor", TA[:, 3, col, :], sA, CO[:, 0 if sg > 0 else 2, c1], TA[:, 3, col, :], ALU.mult, ALU.add, reads=small, writes=small)
                S.i("dve", "tensor_single_scalar", TA[:, 4, col, :], cA, -sg, ALU.mult, reads=small, writes=small)
                S.i("dve", "tensor_single_scalar", TA[:, 5, col, :], sA, -sg, ALU.mult, reads=small, writes=small)
                S.i("dve", "tensor_single_scalar", TB[:, 2, col, :], TB[:, 1, col, :], sg, ALU.mult, reads=small, writes=small)
            t1 = S.sbuf([128, 36, 64], F32, "s5t1", ph)
            t2 = S.sbuf([128, 36, 64], F32, "s5t2", ph)
            tabs = [S.sbuf([128, 4, T_], BF16, "s5tab%d" % i, ph) for i in range(2)]

            def oa(i, col):
                return TA[:, i, col, :].unsqueeze(2).to_broadcast([128, 36, 64])

            def ob(i, col):
                return TB[:, i, col, :].unsqueeze(1).to_broadcast([128, 36, 64])

            for col in range(32):
                tab = tabs[col % 2]

                def tv(j):
                    return tab[:, j, :].rearrange("p (a b) -> p a b", b=64)

                for (j, (x1, y1, x2, y2, op)) in enumerate(((2, 0, 3, 2, ALU.subtract), (3, 0, 2, 2, ALU.add), (0, 0, 1, 1, ALU.subtract), (5, 0, 4, 1, ALU.add))):
                    S.i("dve", "tensor_tensor", t1[:], oa(x1, col), ob(y1, col), ALU.mult, reads=small, writes=[t1])
                    S.i("dve", "tensor_tensor", t2[:], oa(x2, col), ob(y2, col), ALU.mult, reads=small, writes=[t2])
                    S.i("dve", "tensor_tensor", tv(j), t1[:], t2[:], op, reads=[t1, t2], writes=[tab])
                S.dma("sp", self.ST_d[col], tab[:], reads=[tab], writes=[self.ST_d], semtile=tab)
            S.barrier()

    def s5(self, b, uS, mixT, ph):
        S, I = self.S, self.I
        T_ = NCTX + NLAT
        dsk = S.sbuf([128, 4], F32, "dsk", ph)
        self.load(dsk, I["dskT"][:], I["dskT"])
        gw = S.sbuf([128, 4, 512], BF16, "gw", ph)
        self.load(gw, I["glu_w"][:, :].rearrange("(k p) n -> p k n", p=128), I["glu_w"], q="pool")
        gb = S.sbuf([128, 4], F32, "gb", ph)
        self.load(gb, I["glu_bT"][:], I["glu_bT"])
        diagD = S.sbuf([128, 128], BF16, "diagD", ph)
        gT = [S.sbuf([128, NLAT], BF16, "gT%d" % i, ph) for i in range(4)]
        B = [S.sbuf([128, T_], F32, "s5B%d" % i, ph) for i in range(6)]
        Mb = S.sbuf([128, 4, NLAT], BF16, "s5M", ph)
        tabs = [S.sbuf([128, 4, T_], BF16, "s5tb%d" % i, ph) for i in range(2)]
        BDt = [S.sbuf([128, 2, 128], BF16, "BDt%d" % i, ph) for i in range(2)]
        CDt = [S.sbuf([128, 3, 128], BF16, "CDt%d" % i, ph) for i in range(2)]
        cblks = [(c0, min(512, T_ - c0)) for c0 in range(0, T_, 512)]
        it = 0
        order5 = [(d_, st_) for ft_ in range(4) for d_ in range(2) for st_ in range(4 * ft_, 4 * ft_ + 4)]

        def ld5(i_):
            d_, st_ = order5[i_]
            bd_, cd_, tab_ = BDt[i_ % 2], CDt[i_ % 2], tabs[i_ % 2]
            self.load(tab_, self.ST_d[d_ * 16 + st_], self.ST_d)
            for ri in range(2):
                self.load(bd_, I["s5BD"][ri, d_, st_], I["s5BD"], q="pool", dst_ap=bd_[:, ri, :])
                self.load(cd_, I["s5CD"][ri, d_, st_], I["s5CD"], q="pool", dst_ap=cd_[:, ri, :])
            S.i("pool", "tensor_single_scalar", cd_[:, 1, :], cd_[:, 1, :], -1.0, ALU.mult, reads=[cd_], writes=[cd_])
            S.i("pool", "tensor_single_scalar", cd_[:, 2, :], cd_[:, 0, :], -1.0, ALU.mult, reads=[cd_], writes=[cd_])

        def emit_bu(i_):
            d_, st_ = order5[i_]
            bd_ = BDt[i_ % 2]
            ft_ = st_ // 4
            for (c0, n) in cblks:
                pr = self.ps()
                pi = self.ps()
                S.mm(pr, pr[:, :n], bd_, bd_[:, 0, :], uS[d_], uS[d_][:, ft_, c0:c0 + n])
                S.mm(pi, pi[:, :n], bd_, bd_[:, 1, :], uS[d_], uS[d_][:, ft_, c0:c0 + n])
                S.i("act", "activation", B[0][:, c0:c0 + n], pr[:, :n], AF.Copy, reads=[pr], writes=[B[0]])
                S.i("act", "activation", B[1][:, c0:c0 + n], pi[:, :n], AF.Copy, reads=[pi], writes=[B[1]])

        for ft in range(4):
            yb = [self.ps(reserve=True) for _ in range(4)]
            S.i("dve", "tensor_single_scalar", diagD[:], self.ident[:], dsk[:, ft:ft + 1], ALU.mult, reads=[self.ident, dsk], writes=[diagD])
            for q4 in range(4):
                S.mm(yb[q4], yb[q4][:, :], diagD, diagD[:], uS[0], uS[0][:, ft, NCTX + 512 * q4:NCTX + 512 * (q4 + 1)], start=True, stop=False)
            for d in range(2):
                for st in range(4 * ft, 4 * ft + 4):
                    col = d * 16 + st
                    bd, cd, tab = BDt[it % 2], CDt[it % 2], tabs[it % 2]
                    if it == 0:
                        ld5(0)
                    if it + 1 < len(order5):
                        ld5(it + 1)
                    if it == 0:
                        emit_bu(0)
                    S.i("dve", "tensor_tensor", B[2][:], B[0][:], tab[:, 0, :], ALU.mult, reads=[B[0], tab], writes=[B[2]])
                    S.i("dve", "tensor_tensor", B[3][:], B[1][:], tab[:, 1, :], ALU.mult, reads=[B[1], tab], writes=[B[3]])
                    S.i("dve", "tensor_tensor", B[4][:], B[1][:], tab[:, 0, :], ALU.mult, reads=[B[1], tab], writes=[B[4]])
                    S.i("dve", "tensor_tensor", B[5][:], B[0][:], tab[:, 1, :], ALU.mult, reads=[B[0], tab], writes=[B[5]])
                    S.i("dve", "tensor_tensor", B[2][:], B[2][:], B[3][:], ALU.subtract, reads=[B[2], B[3]], writes=[B[2]])
                    S.i("dve", "tensor_tensor", B[4][:], B[4][:], B[5][:], ALU.add, reads=[B[4], B[5]], writes=[B[4]])
                    if it + 1 < len(order5):
                        emit_bu(it + 1)
                    it += 1
                    rm = self.RMAG[:, col:col + 1].to_broadcast([128, T_])
                    if d == 0:
                        S.i("dve", "tensor_tensor_scan", B[3][:], rm, B[2][:], 0.0, ALU.mult, ALU.add, reads=[B[2], self.RMAG], writes=[B[3]])
                        S.i("dve", "tensor_tensor_scan", B[5][:], rm, B[4][:], 0.0, ALU.mult, ALU.add, reads=[B[4], self.RMAG], writes=[B[5]])
                    else:
                        S.i("dve", "tensor_tensor_scan", B[3][:, ::-1], rm, B[2][:, ::-1], 0.0, ALU.mult, ALU.add, reads=[B[2], self.RMAG], writes=[B[3]])
                        S.i("dve", "tensor_tensor_scan", B[5][:, ::-1], rm, B[4][:, ::-1], 0.0, ALU.mult, ALU.add, reads=[B[4], self.RMAG], writes=[B[5]])
                    l0 = NCTX if d == 0 else 0
                    ls = slice(l0, l0 + NLAT)
                    S.i("dve", "tensor_tensor", Mb[:, 0, :], B[3][:, ls], tab[:, 2, ls], ALU.mult, reads=[B[3], tab], writes=[Mb])
                    S.i("dve", "tensor_tensor", Mb[:, 1, :], B[5][:, ls], tab[:, 3, ls], ALU.mult, reads=[B[5], tab], writes=[Mb])
                    S.i("dve", "tensor_tensor", Mb[:, 2, :], B[5][:, ls], tab[:, 2, ls], ALU.mult, reads=[B[5], tab], writes=[Mb])
                    S.i("dve", "tensor_tensor", Mb[:, 3, :], B[3][:, ls], tab[:, 3, ls], ALU.mult, reads=[B[3], tab], writes=[Mb])
                    last = (d == 1 and st == 4 * ft + 3)
                    for q4 in range(4):
                        cs_ = slice(512 * q4, 512 * (q4 + 1))
                        S.mm(yb[q4], yb[q4][:, :], cd, cd[:, 0, :], Mb, Mb[:, 0, cs_], start=False, stop=False)
                        S.mm(yb[q4], yb[q4][:, :], cd, cd[:, 2, :], Mb, Mb[:, 1, cs_], start=False, stop=False)
                        S.mm(yb[q4], yb[q4][:, :], cd, cd[:, 1, :], Mb, Mb[:, 2, cs_], start=False, stop=False)
                        S.mm(yb[q4], yb[q4][:, :], cd, cd[:, 1, :], Mb, Mb[:, 3, cs_], start=False, stop=last)
            for q4 in range(4):
                xs_, x2_ = B[2][:, 512 * q4:512 * (q4 + 1)], B[4][:, 512 * q4:512 * (q4 + 1)]
                S.i("act", "activation", xs_, yb[q4][:, :], AF.Copy, reads=[yb[q4]], writes=[B[2]])
                S.i("dve", "tensor_tensor", x2_, xs_, xs_, ALU.mult, reads=[B[2]], writes=[B[4]])
                S.i("dve", "tensor_scalar", x2_, x2_, 0.044715, 1.0, ALU.mult, ALU.add, reads=[B[4]], writes=[B[4]])
                S.i("dve", "tensor_tensor", x2_, x2_, xs_, ALU.mult, reads=[B[2], B[4]], writes=[B[4]])
                S.i("act", "activation", x2_, x2_, AF.Tanh, scale=math.sqrt(2.0 / math.pi), reads=[B[4]], writes=[B[4]])
                S.i("dve", "tensor_scalar", x2_, x2_, 1.0, 0.5, ALU.add, ALU.mult, reads=[B[4]], writes=[B[4]])
                S.i("dve", "tensor_tensor", gT[ft][:, 512 * q4:512 * (q4 + 1)], x2_, xs_, ALU.mult, reads=[B[2], B[4]], writes=[gT[ft]])
                self.ps_free(yb[q4])
        sgm = [S.sbuf([128, 512], BF16, "sgm%d" % i, ph) for i in range(2)]
        for f2 in range(4):
            for q4 in range(4):
                ps = self.ps()
                for ft in range(4):
                    S.mm(ps, ps[:, :], gw, gw[:, ft, f2 * 128:(f2 + 1) * 128], gT[ft], gT[ft][:, 512 * q4:512 * (q4 + 1)], start=(ft == 0), stop=(ft == 3))
                sg_ = sgm[(f2 * 4 + q4) % 2]
                S.i("act", "activation", sg_[:], ps[:, :], AF.Sigmoid, bias=gb[:, f2:f2 + 1], reads=[ps, gb], writes=[sg_])
                S.i("dve", "tensor_tensor", mixT[4 + f2][:, L0 + 512 * q4:L0 + 512 * (q4 + 1)], sg_[:], gT[f2][:, 512 * q4:512 * (q4 + 1)], ALU.mult, reads=[sg_, gT[f2]], writes=[mixT[4 + f2]])

    def mla(self, b, cqn, ckvn, KR, mixT, ph):
        S, I = self.S, self.I
        wuq = S.sbuf([128, 2, 768], BF16, "wuq", ph)
        self.load(wuq, I["o_w_uq"][:, :].rearrange("(k p) n -> p k n", p=128), I["o_w_uq"], q="pool")
        wukv = S.sbuf([128, 1024], BF16, "wukv", ph)
        self.load(wukv, I["o_w_ukv"][:, :], I["o_w_ukv"], q="pool")
        cos = S.sbuf([128, NLAT], F32, "cos", ph)
        sin = S.sbuf([128, NLAT], F32, "sin", ph)
        self.load(cos, I["cos_o"][:], I["cos_o"])
        self.load(sin, I["sin_o"][:], I["sin_o"])
        rotT = S.sbuf([128, 128], BF16, "rotT", ph)
        self.load(rotT, I["rotT_o"][:], I["rotT_o"])
        VA = S.sbuf([128, 18, 8, 128], BF16, "VA1", ph)
        S.i("pool", "memset", VA[:, :, :, 64:128], 1.0, writes=[VA])
        wv = wukv[:, :].rearrange("k (h t d) -> k h t d", h=8, t=2)
        for ti, (s, kind, _) in enumerate(TTILES):
            pv = self.ps()
            S.mm(pv, pv[:, :].rearrange("p (h d) -> p h d", h=8), ckvn, ckvn[:, s:s + 128], wukv, wv[:, :, 1, :])
            S.i("act", "activation", VA[:, ti, :, 0:64], pv[:, :].rearrange("p (h d) -> p h d", h=8), AF.Copy, reads=[pv], writes=[VA])
        Qh = [S.sbuf([128, NLAT], BF16, "Qh%d" % i, ph) for i in range(2)]
        Kh = [S.sbuf([128, W], BF16, "Kh%d" % i, ph) for i in range(2)]
        qn = S.sbuf([128, 512], BF16, "mqn", ph)
        t1 = S.sbuf([128, 512], F32, "mt1", ph)
        t2 = S.sbuf([128, 512], F32, "mt2", ph)
        T = {"pT": [S.sbuf([128, 512], BF16, "pT%d" % i, ph) for i in range(6)], "den": [S.sbuf([64, 512], F32, "den%d" % i, ph) for i in range(2)]}

        def kcol(kt):
            return (C0 + 128 * kt) if kt < 2 else (L0 + 128 * (kt - 2))

        for hp in range(4):
          for h in (2 * hp, 2 * hp + 1):
            Q, K = Qh[h % 2], Kh[h % 2]
            for (s, n, kind) in BLKS:
                pk = self.ps()
                S.mm(pk, pk[0:64, :n], wukv, wukv[:, h * 128:h * 128 + 64], ckvn, ckvn[:, s:s + n])
                S.i("act", "activation", K[0:64, s:s + n], pk[0:64, :n], AF.Copy, reads=[pk], writes=[K])
                S.i("pool", "tensor_copy", K[64:96, s:s + n], KR[64:96, s:s + n], reads=[KR], writes=[K])
            for qb in range(4):
                s = L0 + 512 * qb
                pq = self.ps()
                for j in range(2):
                    S.mm(pq, pq[0:96, :], wuq, wuq[:, j, h * 96:(h + 1) * 96], cqn, cqn[:, j, s:s + 512], start=(j == 0), stop=(j == 1))
                S.i("act", "activation", qn[0:96, :], pq[0:96, :], AF.Copy, reads=[pq], writes=[qn])
                p3 = self.ps()
                S.mm(p3, p3[0:96, :], rotT, rotT[0:96, 0:96], qn, qn[0:96, :])
                S.i("pool", "tensor_copy", Q[0:64, 512 * qb:512 * (qb + 1)], qn[0:64, :], reads=[qn], writes=[Q])
                S.i("pool", "tensor_tensor", t1[64:96, :], qn[64:96, :], cos[64:96, 512 * qb:512 * (qb + 1)], ALU.mult, reads=[qn, cos], writes=[t1])
                S.i("dve", "tensor_tensor", t2[64:96, :], p3[64:96, :], sin[64:96, 512 * qb:512 * (qb + 1)], ALU.mult, reads=[p3, sin], writes=[t2])
                S.i("dve", "tensor_tensor", Q[64:96, 512 * qb:512 * (qb + 1)], t1[64:96, :], t2[64:96, :], ALU.add, reads=[t1, t2], writes=[Q])
          out_t = mixT[hp]
          for qb in range(4):
            s = L0 + 512 * qb
            streams = []
            for h in (2 * hp, 2 * hp + 1):
                Q, K = Qh[h % 2], Kh[h % 2]
                po = (h % 2) * 64
                streams.append({"QTt": Q, "Qap": Q[0:96, 512 * qb:512 * (qb + 1)], "KTt": K,
                                "Kap": (lambda kt, K=K: K[0:96, kcol(kt):kcol(kt) + 128]),
                                "Vt": VA, "Vap": (lambda kt, h=h: VA[:, kt, h, :]), "out_t": out_t, "out_ap": out_t[po:po + 64, s:s + 512]})
            self.attn_multi(streams, 18, 512, 96 ** -0.5, T)


_HC = None


def get_hc():
    global _HC
    if _HC is None:
        _HC = host_consts()
    return _HC


def _dt_of(a):
    return BF16 if a.dtype == ml_dtypes.bfloat16 else F32


HC_SHAPES = {k: (list(v.shape), _dt_of(v)) for k, v in get_hc().items()}
DBG_SHAPES = {
    "MV": ([128, 2, 6, 8, 4], F32),
    "hT": ([8, 128, W], BF16),
    "HY": ([3, NCTX + NLAT, 512], BF16),
    "QT": ([4, 128, W], BF16),
    "KK": ([2, 128, W], BF16),
    "VA": ([128, 18, 2, 128], BF16),
    "mixT": ([8, 128, W], BF16),
    "Hf_l": ([2, 16, 128, 2, 512], F32),
    "Hf_c": ([2, 2, 128, 2, 512], F32),
    "XT": ([D, W], F32),
    "G": ([32, W], BF16),
}


def fm(v):
    return np.ascontiguousarray(np.asarray(v, np.float32).reshape(8, 128).T)


def host_prep(inputs, core):
    b0 = 2 * core
    P = {}
    x, ctx, c = inputs["x"], inputs["ctx"], inputs["c"]
    P["xT"] = _f(np.stack([np.concatenate([ctx[b].T, x[b].T], axis=1) for b in (b0, b0 + 1)]))
    P["cT"] = _f(np.stack([fm(c[b0]), fm(c[b0 + 1]), fm(inputs["c_ctx"]), fm(inputs["c_ctx"])], axis=-1))
    return P


def host_shared(inputs):
    Sh = {}
    Sh["w_mod"] = _f(inputs["w_mod"])
    Sh["bmodT"] = _f(inputs["b_mod"].reshape(2, 48, 128).transpose(2, 0, 1))
    Sh["ngT"] = _f(inputs["norm_g"].reshape(2, 2, 8, 128).transpose(3, 0, 1, 2))
    Sh["e_w_in"] = _f(inputs["e_w_in"][0])
    Sh["convw"] = _f(inputs["e_hy_conv_w"][0])
    Sh["convb"] = _f(inputs["e_hy_conv_b"][0][None])
    Sh["hy_w1"] = _f(inputs["e_hy_w1"][0])
    Sh["hy_w2"] = _f(inputs["e_hy_w2"][0])
    Sh["hy_w3"] = _f(inputs["e_hy_w3"][0])
    Sh["hy_vec"] = _f(np.stack([inputs["e_hy_b1"][0], inputs["e_hy_b2"][0], inputs["e_hy_freq"][0]], -1))
    Sh["fbias"] = _f(inputs["e_hy_fbias"][0])
    qkg = inputs["e_qk_g"][0]
    Sh["qkg"] = _f(np.stack([np.tile(qkg[0], 2), np.tile(qkg[1], 2)], -1))
    Sh["e_w_out"] = _f(inputs["e_w_out"][0])
    wr = np.concatenate([inputs["moe_w_rg"], inputs["moe_w_re"]], -1)
    Sh["wr"] = _f(wr.reshape(2, 8, 128, 36).transpose(2, 0, 1, 3))
    Sh["moe_w_gate"] = _f(inputs["moe_w_gate"])
    Sh["moe_w_up"] = _f(inputs["moe_w_up"])
    Sh["moe_w_down"] = _f(inputs["moe_w_down"])
    Sh["finalgT"] = fm(inputs["final_g"])
    Sh["o_w_in"] = _f(inputs["o_w_in"][0])
    qg = inputs["o_q_norm_g"][0]
    Sh["o_ng"] = _f(np.stack([qg[:128], qg[128:], inputs["o_kv_norm_g"][0]], -1))
    Sh["o_w_uq"] = _f(inputs["o_w_uq"][0])
    Sh["o_w_ukv"] = _f(inputs["o_w_ukv"][0])
    Sh["o_w_out"] = _f(inputs["o_w_out"][0])
    def st_layout(a):
        return a.reshape(2, 16, 2, 64).transpose(2, 3, 0, 1).reshape(128, 32)
    ldt = np.broadcast_to(inputs["o_s5_log_dt"][0][:, :, None], (2, 32, 64))
    Sh["s5p"] = _f(np.stack([st_layout(inputs["o_s5_a_re"][0]), st_layout(inputs["o_s5_a_im"][0]), st_layout(ldt)], 1))
    BD = np.zeros((2, 2, 16, 128, 128), np.float32)
    CD = np.zeros((2, 2, 16, 128, 128), np.float32)
    for ri, (bb, cc) in enumerate(((inputs["o_s5_b_re"][0], inputs["o_s5_c_re"][0]), (inputs["o_s5_b_im"][0], inputs["o_s5_c_im"][0]))):
        for d_ in range(2):
            for st in range(16):
                for gg in range(2):
                    g = 2 * st + gg
                    r0 = (g % 8) * 16
                    BD[ri, d_, st, r0:r0 + 16, gg * 64:(gg + 1) * 64] = bb[d_, g].T
                    CD[ri, d_, st, gg * 64:(gg + 1) * 64, r0:r0 + 16] = cc[d_, g].T
    Sh["s5BD"] = BD
    Sh["s5CD"] = CD
    Sh["dskT"] = _f(inputs["o_s5_d"][0].reshape(4, 128).T)
    Sh["glu_w"] = _f(inputs["o_glu_w"][0])
    Sh["glu_bT"] = _f(inputs["o_glu_b"][0].reshape(4, 128).T)
    Sh.update(get_hc())
    return Sh


def run(inputs, stop="all", dbg=(), cores=8):
    prog = Prog(stop, dbg)
    nc = prog.build()
    sh = host_shared(inputs)
    in_maps = []
    for core in range(cores):
        m = dict(sh)
        m.update(host_prep(inputs, core))
        in_maps.append({k: m[k] for k in prog.in_names})
    res = run_bass_kernel_spmd(nc, in_maps, core_ids=list(range(cores)))
    return prog, res


def kernel(**inputs):
    inputs = {k: np.asarray(v) for k, v in inputs.items()}
    prog, res = run(inputs)
    out = np.empty((16, NLAT, D), np.float32)
    for core in range(8):
        o = res.results[core]["outT"]
        for j in range(2):
            out[2 * core + j] = o[j].T
    return out
```
